# Optimizing a Trainium2 kernel written in Bass

```python
import jax, jax.numpy as jnp
from jax import lax
import numpy as np

D_MODEL = 1024
BATCH = 4
SEQ = 4096
DEPTH = 1

MEM_LEN = 256
EPS = 1e-6
M_HEADS = 4
M_DH = 128
M_W = M_HEADS * M_DH
CONV_K = 4
M_CHUNK = 128
DIL_GROUPS = ((128, 1), (512, 4), (2048, 16))
N_DIL = len(DIL_GROUPS)
DIL_HEADS = 4
DIL_DH = 64
DIL_W = DIL_HEADS * DIL_DH
DIL_BLOCK = 128
X_HEADS = 4
X_DH = 128
X_W = X_HEADS * X_DH
N_BRANCH = 3
IN_SIZES = (M_W, M_W, M_W, M_W, M_HEADS, M_HEADS, N_DIL * DIL_W, N_DIL * DIL_W, N_DIL * DIL_W, X_W)
IN_TOTAL = sum(IN_SIZES)
N_GROUPS = 4
EXP_PER_GROUP = 8
TOP_K = 2
D_FF_EXP = 256

kernel_name = 'hybrid_mlstm_dilated_memory_hmoe_block'


def rms_norm(x, g):
    x32 = x.astype(jnp.float32)
    y = x32 * lax.rsqrt(jnp.mean(x32 * x32, axis=-1, keepdims=True) + EPS)
    return (y * g.astype(jnp.float32)).astype(x.dtype)


def causal_depthwise_conv(x, w):
    K = w.shape[0]
    S = x.shape[1]
    xp = jnp.pad(x, ((0, 0), (K - 1, 0), (0, 0)))
    y = xp[:, 0:S] * w[0]
    for j in range(1, K):
        y = y + xp[:, j:j + S] * w[j]
    return y


def mlstm_chunkwise(q, k, v, i_pre, f_pre):
    B, S, H, Dh = q.shape
    f32 = jnp.float32
    L = M_CHUNK
    nc = S // L
    q = q.astype(f32)
    k = k.astype(f32) * (Dh ** -0.5)
    v = v.astype(f32)
    log_f = jax.nn.log_sigmoid(f_pre.astype(f32))
    log_i = i_pre.astype(f32)

    def chunks(a):
        return jnp.moveaxis(a.reshape((B, nc, L) + a.shape[2:]), 1, 0)

    causal = jnp.tril(jnp.ones((L, L), dtype=bool))

    def step(carry, inp):
        C, n, m = carry
        qb, kb, vb, lf, li = inp
        b = jnp.swapaxes(jnp.cumsum(lf, axis=1), 1, 2)
        li = jnp.swapaxes(li, 1, 2)
        log_w = jnp.where(causal, b[..., :, None] - b[..., None, :] + li[..., None, :], -jnp.inf)
        log_inter = b + m[..., None]
        m_t = jnp.maximum(log_inter, jnp.max(log_w, axis=-1))
        w_intra = jnp.exp(log_w - m_t[..., None])
        w_inter = jnp.swapaxes(jnp.exp(log_inter - m_t), 1, 2)[..., None]
        s = jnp.einsum('bthd,bshd->bhts', qb, kb) * w_intra
        num = jnp.einsum('bhts,bshe->bthe', s, vb) + w_inter * jnp.einsum('bthd,bhde->bthe', qb, C)
        den = jnp.swapaxes(jnp.sum(s, axis=-1), 1, 2) + w_inter[..., 0] * jnp.einsum('bthd,bhd->bth', qb, n)
        floor = jnp.swapaxes(jnp.exp(-m_t), 1, 2)
        h = num / jnp.maximum(jnp.abs(den), floor)[..., None]
        b_last = b[..., -1]
        log_end = b_last[..., None] - b + li
        m_new = jnp.maximum(b_last + m, jnp.max(log_end, axis=-1))
        w_end = jnp.exp(log_end - m_new[..., None])
        decay = jnp.exp(b_last + m - m_new)
        C = decay[..., None, None] * C + jnp.einsum('bhs,bshd,bshe->bhde', w_end, kb, vb)
        n = decay[..., None] * n + jnp.einsum('bhs,bshd->bhd', w_end, kb)
        return (C, n, m_new), h

    init = (jnp.zeros((B, H, Dh, Dh), f32), jnp.zeros((B, H, Dh), f32), jnp.zeros((B, H), f32))
    _, h = lax.scan(step, init, (chunks(q), chunks(k), chunks(v), chunks(log_f), chunks(log_i)))
    return jnp.moveaxis(h, 0, 1).reshape(B, S, H, Dh)


def dilated_window_attention(q, k, v, dilation, steps):
    B, S, H, Dh = q.shape
    f32 = jnp.float32
    r = dilation
    N = S // r
    Qb = DIL_BLOCK
    nb = -(-N // Qb)
    pad = nb * Qb - N

    def subseq(a):
        a = a.reshape(B, N, r, H, Dh).transpose(0, 2, 1, 3, 4)
        a = jnp.pad(a, ((0, 0), (0, 0), (0, pad), (0, 0), (0, 0)))
        return a.reshape(B, r, nb, Qb, H, Dh)

    def with_prev(a):
        prev = jnp.pad(a, ((0, 0), (0, 0), (1, 0), (0, 0), (0, 0), (0, 0)))[:, :, :-1]
        return jnp.concatenate([prev, a], axis=3)

    qb = subseq(q)
    kk = with_prev(subseq(k))
    vv = with_prev(subseq(v)).astype(f32)
    scores = jnp.einsum('brnqhd,brnkhd->brnhqk', qb, kk).astype(f32) * (Dh ** -0.5)
    blk = jnp.arange(nb)[:, None] * Qb
    qpos = blk + jnp.arange(Qb)[None]
    kpos = blk - Qb + jnp.arange(2 * Qb)[None]
    dist = qpos[:, :, None] - kpos[:, None, :]
    valid = (dist >= 0) & (dist <= steps) & (kpos[:, None, :] >= 0)
    scores = jnp.where(valid[:, None], scores, -jnp.inf)
    mx = jnp.max(scores, axis=-1, keepdims=True)
    p = jnp.exp(scores - mx)
    den = jnp.sum(p, axis=-1)
    out = jnp.einsum('brnhqk,brnkhd->brnqhd', p, vv) / jnp.swapaxes(den, -1, -2)[..., None]
    lse = jnp.swapaxes(mx[..., 0] + jnp.log(den), -1, -2)

    def unsub(a):
        a = a.reshape((B, r, nb * Qb) + a.shape[4:])[:, :, :N]
        a = jnp.swapaxes(a, 1, 2)
        return a.reshape((B, S) + a.shape[3:])

    return unsub(out), unsub(lse)


def memory_cross_attention(xq, mem, mem_norm_g, w_mem_kv, q_g, k_g):
    B, S, _ = xq.shape
    M = mem.shape[1]
    q = rms_norm(xq.reshape(B, S, X_HEADS, X_DH), q_g)
    kv = rms_norm(mem, mem_norm_g) @ w_mem_kv
    km, vm = jnp.split(kv, 2, axis=-1)
    km = rms_norm(km.reshape(B, M, X_HEADS, X_DH), k_g)
    vm = vm.reshape(B, M, X_HEADS, X_DH).astype(jnp.float32)
    s = jnp.einsum('bshd,bmhd->bhsm', q, km).astype(jnp.float32) * (X_DH ** -0.5)
    p = jax.nn.softmax(s, axis=-1)
    return jnp.einsum('bhsm,bmhd->bshd', p, vm)


def hier_moe(x, w_grp, b_grp, w_er, b_er, w_eg, w_eu, w_ed):
    B, S, D = x.shape
    T = B * S
    f32 = jnp.float32
    xt = x.reshape(T, D)
    grp_prob = jax.nn.softmax((xt @ w_grp + b_grp).astype(f32), axis=-1)
    g_w, g_idx = lax.top_k(grp_prob, 1)
    exp_logits = (jnp.einsum('td,gde->tge', xt, w_er) + b_er).astype(f32)
    exp_logits = jnp.take_along_axis(exp_logits, g_idx[:, :, None], axis=1)[:, 0]
    top_v, top_i = lax.top_k(exp_logits, TOP_K)
    top_w = jax.nn.softmax(top_v, axis=-1) * g_w
    within = jnp.sum(jax.nn.one_hot(top_i, EXP_PER_GROUP, dtype=f32) * top_w[..., None], axis=1)
    y = jnp.zeros((T, D), f32)
    for g in range(N_GROUPS):
        comb = within * (g_idx == g)
        hid = jax.nn.silu(jnp.einsum('td,edf->tef', xt, w_eg[g])) * jnp.einsum('td,edf->tef', xt, w_eu[g])
        y = y + jnp.einsum('tef,efd->td', hid * comb[..., None].astype(hid.dtype), w_ed[g]).astype(f32)
    return y.reshape(B, S, D)


def setup_inputs(seed: int = 0) -> dict:
    key = jax.random.key(seed)
    ks = jax.random.split(key, 32)
    f32 = jnp.float32

    def nrm(k, shape, scale):
        return jax.random.normal(k, shape, f32) * scale

    L = DEPTH
    G, E, F = N_GROUPS, EXP_PER_GROUP, D_FF_EXP
    return {
        'x': nrm(ks[0], (BATCH, SEQ, D_MODEL), 1.0),
        'mem': nrm(ks[1], (BATCH, MEM_LEN, D_MODEL), 1.0),
        'ln1_g': 1.0 + nrm(ks[2], (L, D_MODEL), 0.02),
        'w_in': nrm(ks[3], (L, D_MODEL, IN_TOTAL), D_MODEL ** -0.5),
        'm_conv': nrm(ks[4], (L, CONV_K, 2 * M_W), CONV_K ** -0.5),
        'm_i_b': nrm(ks[5], (L, M_HEADS), 0.1),
        'm_f_b': jnp.linspace(3.0, 6.0, M_HEADS, dtype=f32)[None] + nrm(ks[6], (L, M_HEADS), 0.1),
        'm_norm_g': 1.0 + nrm(ks[7], (L, M_HEADS, M_DH), 0.02),
        'dil_q_g': 1.0 + nrm(ks[8], (L, N_DIL, DIL_DH), 0.02),
        'dil_k_g': 1.0 + nrm(ks[9], (L, N_DIL, DIL_DH), 0.02),
        'mem_norm_g': 1.0 + nrm(ks[10], (L, D_MODEL), 0.02),
        'w_mem_kv': nrm(ks[11], (L, D_MODEL, 2 * X_W), D_MODEL ** -0.5),
        'x_q_g': 1.0 + nrm(ks[12], (L, X_DH), 0.02),
        'x_k_g': 1.0 + nrm(ks[13], (L, X_DH), 0.02),
        'w_a': nrm(ks[14], (L, M_W, D_MODEL), M_W ** -0.5),
        'w_b': nrm(ks[15], (L, DIL_W, D_MODEL), DIL_W ** -0.5),
        'w_c': nrm(ks[16], (L, X_W, D_MODEL), X_W ** -0.5),
        'w_gate': nrm(ks[17], (L, D_MODEL, N_BRANCH * D_MODEL), D_MODEL ** -0.5),
        'b_gate': nrm(ks[18], (L, N_BRANCH * D_MODEL), 0.01),
        'w_out': nrm(ks[19], (L, D_MODEL, D_MODEL), D_MODEL ** -0.5),
        'ln2_g': 1.0 + nrm(ks[20], (L, D_MODEL), 0.02),
        'w_grp': nrm(ks[21], (L, D_MODEL, G), D_MODEL ** -0.5),
        'b_grp': nrm(ks[22], (L, G), 0.01),
        'w_er': nrm(ks[23], (L, G, D_MODEL, E), D_MODEL ** -0.5),
        'b_er': nrm(ks[24], (L, G, E), 0.01),
        'w_eg': nrm(ks[25], (L, G, E, D_MODEL, F), D_MODEL ** -0.5),
        'w_eu': nrm(ks[26], (L, G, E, D_MODEL, F), D_MODEL ** -0.5),
        'w_ed': nrm(ks[27], (L, G, E, F, D_MODEL), F ** -0.5),
    }


def reference(x, mem, ln1_g, w_in, m_conv, m_i_b, m_f_b, m_norm_g, dil_q_g, dil_k_g,
              mem_norm_g, w_mem_kv, x_q_g, x_k_g, w_a, w_b, w_c, w_gate, b_gate, w_out,
              ln2_g, w_grp, b_grp, w_er, b_er, w_eg, w_eu, w_ed):
    B, S, D = x.shape
    splits = np.cumsum(IN_SIZES)[:-1].tolist()
    h = x
    for l in range(DEPTH):
        xn = rms_norm(h, ln1_g[l])
        z = xn @ w_in[l]
        mq, mk, mv, mo, mi, mf, dq, dk, dv, xq = jnp.split(z, splits, axis=-1)

        qk = jax.nn.silu(causal_depthwise_conv(jnp.concatenate([mq, mk], axis=-1), m_conv[l]))
        mq, mk = jnp.split(qk, 2, axis=-1)
        hA = mlstm_chunkwise(mq.reshape(B, S, M_HEADS, M_DH), mk.reshape(B, S, M_HEADS, M_DH),
                             mv.reshape(B, S, M_HEADS, M_DH), mi + m_i_b[l], mf + m_f_b[l])
        hA = rms_norm(hA, m_norm_g[l]).reshape(B, S, M_W) * jax.nn.sigmoid(mo)
        branch_a = hA @ w_a[l]

        dq = rms_norm(dq.reshape(B, S, N_DIL, DIL_HEADS, DIL_DH), dil_q_g[l][:, None, :])
        dk = rms_norm(dk.reshape(B, S, N_DIL, DIL_HEADS, DIL_DH), dil_k_g[l][:, None, :])
        dv = dv.reshape(B, S, N_DIL, DIL_HEADS, DIL_DH)
        outs, lses = [], []
        for g, (window, dilation) in enumerate(DIL_GROUPS):
            o_g, lse_g = dilated_window_attention(dq[:, :, g], dk[:, :, g], dv[:, :, g],
                                                  dilation, window // dilation)
            outs.append(o_g)
            lses.append(lse_g)
        alpha = jax.nn.softmax(jnp.stack(lses, axis=0), axis=0)[..., None]
        hB = jnp.sum(alpha * jnp.stack(outs, axis=0), axis=0).reshape(B, S, DIL_W)
        branch_b = hB @ w_b[l]

        hC = memory_cross_attention(xq, mem, mem_norm_g[l], w_mem_kv[l], x_q_g[l], x_k_g[l])
        branch_c = hC.reshape(B, S, X_W) @ w_c[l]

        gates = jax.nn.sigmoid(xn @ w_gate[l] + b_gate[l]).reshape(B, S, N_BRANCH, D)
        mixed = gates[:, :, 0] * branch_a + gates[:, :, 1] * branch_b + gates[:, :, 2] * branch_c
        h = h + (mixed @ w_out[l]).astype(h.dtype)

        hn = rms_norm(h, ln2_g[l])
        h = h + hier_moe(hn, w_grp[l], b_grp[l], w_er[l], b_er[l], w_eg[l], w_eu[l], w_ed[l]).astype(h.dtype)
    return h.astype(x.dtype)
```

```python
import contextlib
import numpy as np
import concourse.bass as bass
import concourse.mybir as mybir
from concourse.bass_utils import run_bass_kernel_spmd

F32 = mybir.dt.float32
BF16 = mybir.dt.bfloat16
AF = mybir.ActivationFunctionType
ALU = mybir.AluOpType
AX = mybir.AxisListType

NEG = -30000.0
EPS = 1e-6
T_OWN = 2048
T_ALL = 4096


class Buf:
    def __init__(self, name, t, excl=False):
        self.name = name
        self.t = t
        self.w = None
        self.r = {}
        self.excl = excl
        self.ds = None
        self.ds_sw = None

    def __getitem__(self, key):
        return self.t[key]


class DSem:
    def __init__(self, h):
        self.h = h
        self.cnt = 0


class KB:
    EPOCH = 30000

    def __init__(self, nc):
        self.nc = nc
        self.root = contextlib.ExitStack()
        self.es = self.root
        self.stack = []
        self.eng = {'pe': nc.tensor, 'act': nc.scalar, 'dve': nc.vector,
                    'pool': nc.gpsimd, 'sp': nc.sync}
        self.sem = {}
        self.cnt = {}
        self.nsem = 0
        self.seen = {e: {} for e in self.eng}
        self.ninst = {e: 0 for e in self.eng}
        self.nwait = {e: 0 for e in self.eng}
        self.dfree = []
        self.dfree_sw = []
        self.scope_bufs = [[]]
        self.used = 16512
        self.peak = 0
        self.limit = 229344
        self.used_stack = []
        for e in ('pe', 'act', 'dve', 'pool'):
            self._newsem(e)

    def _alloc_sem(self, name):
        self.nsem += 1
        return self.root.enter_context(self.nc.semaphore(f"{name}_{self.nsem}"))

    def _newsem(self, e):
        self.sem[e] = self._alloc_sem("s" + e)
        self.cnt[e] = 0

    def _dsem(self, b, sw=False):
        if sw:
            if b.ds_sw is None:
                b.ds_sw = self.dfree_sw.pop() if self.dfree_sw else DSem(self._alloc_sem("w"))
            return b.ds_sw
        if b.ds is None:
            b.ds = self.dfree.pop() if self.dfree else DSem(self._alloc_sem("d"))
        return b.ds

    def sb(self, name, shape, dtype):
        t = self.es.enter_context(self.nc.sbuf_tensor(name, list(shape), dtype))
        nbytes = int(np.prod(shape[1:])) * (2 if dtype == BF16 else 4)
        self.used += (nbytes + 31) // 32 * 32
        assert self.used <= self.limit, (name, self.used, self.limit)
        self.peak = max(self.peak, self.used)
        b = Buf(name, t)
        self.scope_bufs[-1].append(b)
        return b

    def sb_at(self, name, shape, dtype, offset):
        t = self.nc.alloc_sbuf_tensor_at(name, list(shape), dtype, offset=offset)
        return Buf(name, t)

    def ps(self, name, shape, dtype=F32):
        t = self.es.enter_context(self.nc.psum_tensor(name, list(shape), dtype))
        return Buf(name, t, excl=True)

    def view(self, b, name=None):
        nb = Buf(name or b.name, b.t, b.excl)
        self.scope_bufs[-1].append(nb)
        return nb

    def push(self):
        self.stack.append(self.es)
        self.es = contextlib.ExitStack()
        self.scope_bufs.append([])
        self.used_stack.append(self.used)

    def pop(self):
        bufs = self.scope_bufs.pop()
        for e in self.eng:
            self.wait_all(e, bufs)
        self.barrier()
        for b in bufs:
            if b.ds is not None:
                self.dfree.append(b.ds)
                b.ds = None
            if b.ds_sw is not None:
                self.dfree_sw.append(b.ds_sw)
                b.ds_sw = None
        self.es.close()
        self.es = self.stack.pop()
        self.used = self.used_stack.pop()

    def barrier(self):
        for e in self.eng:
            for e2 in ('pe', 'act', 'dve', 'pool'):
                if e2 != e and self.cnt[e2] > 0:
                    self._wait(e, (self.sem[e2], self.cnt[e2], e2))

    def _wait(self, e, ev):
        sem, val, eng = ev
        key = sem.name
        if self.seen[e].get(key, 0) >= val:
            return
        self.eng[e].wait_ge(sem, val)
        self.seen[e][key] = val
        self.nwait[e] += 1

    STRICT = True

    def _deps(self, e, reads, writes):
        same_ok = (e == 'pe') or not self.STRICT
        for b in reads:
            if b.w is not None:
                if not (b.w[2] == e and e == 'pe'):
                    self._wait(e, b.w)
            if b.excl:
                for ev in list(b.r.values()):
                    if ev[2] != e:
                        self._wait(e, ev)
        for b in writes:
            if b.w is not None and (b.w[2] != e or not same_ok):
                self._wait(e, b.w)
            for ev in list(b.r.values()):
                if ev[2] != e or not same_ok:
                    self._wait(e, ev)

    def _record(self, ev, reads, writes):
        for b in reads:
            old = b.r.get(ev[0].name)
            if old is None or old[1] < ev[1]:
                b.r[ev[0].name] = ev
        for b in writes:
            b.w = ev
            b.r = {}

    def op(self, e, fn, reads=(), writes=()):
        self._deps(e, reads, writes)
        ins = fn(self.eng[e])
        if self.cnt[e] >= self.EPOCH:
            self._newsem(e)
        self.cnt[e] += 1
        ins.then_inc(self.sem[e], 1)
        ev = (self.sem[e], self.cnt[e], e)
        self._record(ev, reads, writes)
        self.ninst[e] += 1
        return ev

    def dma(self, q, out, in_, reads=(), writes=(), sembuf=None, **kw):
        self._deps(q, reads, writes)
        ins = self.eng[q].dma_start(out=out, in_=in_, **kw)
        b = sembuf if sembuf is not None else (writes[0] if writes else reads[0])
        ds = self._dsem(b, sw=(q == 'pool'))
        ds.cnt += 16
        ins.then_inc(ds.h, 16)
        ev = (ds.h, ds.cnt, 'dma')
        self._record(ev, reads, writes)
        self.ninst[q] += 1
        return ev

    def wait_all(self, e, bufs):
        for b in bufs:
            if b.w is not None:
                self._wait(e, b.w)
            for ev in list(b.r.values()):
                self._wait(e, ev)

    def close(self):
        self.root.close()


def V(k, fn, r=(), w=()):
    return k.op('dve', fn, r, w)


def A(k, fn, r=(), w=()):
    return k.op('act', fn, r, w)


def P(k, fn, r=(), w=()):
    return k.op('pool', fn, r, w)


def M(k, fn, r=(), w=()):
    return k.op('pe', fn, r, w)


def pipe2(items):
    prev = None
    for s1, s2 in items:
        s1()
        if prev is not None:
            prev()
        prev = s2
    if prev is not None:
        prev()


def pipeN(items):
    if not items:
        return
    ns = len(items[0])
    for t in range(len(items) + ns - 1):
        for kk in range(ns):
            i = t - kk
            if 0 <= i < len(items):
                items[i][kk]()


O_MQ, O_MK, O_MV, O_MO, O_MI, O_MF, O_DQ, O_DK, O_DV, O_XQ = 0, 512, 1024, 1536, 2048, 2052, 2056, 2824, 3592, 4360
IN_TOTAL = 4872


def build_program(upto="all", dbg=()):
    nc = bass.Bass("TRN2", target_bir_lowering=False)

    def din(name, shape):
        return nc.dram_tensor(name, list(shape), F32, kind="ExternalInput").ap()

    x_own = din("x_own", [T_OWN, 1024])
    x_pre = din("x_pre", [T_OWN, 1024])
    mem = din("mem", [256, 1024])
    ln1_g = din("ln1_g", [1024])
    w_in = din("w_in", [1024, IN_TOTAL])
    m_conv = din("m_conv", [4, 1024])
    m_i_b = din("m_i_b", [4])
    m_f_b = din("m_f_b", [4])
    m_norm_g = din("m_norm_g", [512])
    dil_q_g = din("dil_q_g", [3, 64])
    dil_k_g = din("dil_k_g", [3, 64])
    mem_norm_g = din("mem_norm_g", [1024])
    w_mem_kv = din("w_mem_kv", [1024, 1024])
    x_q_g = din("x_q_g", [128])
    x_k_g = din("x_k_g", [128])
    w_a = din("w_a", [512, 1024])
    w_b = din("w_b", [256, 1024])
    w_c = din("w_c", [512, 1024])
    w_gate = din("w_gate", [1024, 3072])
    b_gate = din("b_gate", [3072])
    w_out = din("w_out", [1024, 1024])
    ln2_g = din("ln2_g", [1024])
    w_grp = din("w_grp", [1024, 4])
    b_grp = din("b_grp", [4])
    w_er = din("w_er", [4, 1024, 8])
    b_er = din("b_er", [32])
    w_eg = din("w_eg", [32, 1024, 256])
    w_eu = din("w_eu", [32, 1024, 256])
    w_ed = din("w_ed", [32, 256, 1024])
    pmul = din("pmul", [4, T_ALL])
    padd = din("padd", [4, T_ALL])
    mpfx = din("mpfx", [128, 128])
    out = nc.dram_tensor("out", [T_OWN, 1024], F32, kind="ExternalOutput").ap()
    dbg_out = {}

    def dout(name, shape):
        dbg_out[name] = nc.dram_tensor(name, list(shape), F32, kind="ExternalOutput").ap()
        return dbg_out[name]

    k = KB(nc)
    pb = [k.ps(f"pb{i}", [128, 512], F32) for i in range(8)]
    pbb = [b.t.bitcast(BF16) for b in pb]

    identf = k.sb("identf", [128, 128], F32)
    ident = k.sb("ident", [128, 128], BF16)
    P(k, lambda e: e.memset(identf[:], 0.0), w=[identf])
    P(k, lambda e: e.affine_select(identf[:], identf[:], pattern=[[-1, 128]], compare_op=ALU.not_equal,
                                   fill=1.0, base=0, channel_multiplier=1), r=[identf], w=[identf])
    V(k, lambda e: e.tensor_copy(ident[:], identf[:]), r=[identf], w=[ident])
    mtmp = k.sb("mtmp", [128, 128], F32)
    mask_cur = k.sb("mask_cur", [128, 128], BF16)
    mask_prev = k.sb("mask_prev", [128, 128], BF16)
    mask_pfx = k.sb("mask_pfx", [128, 128], BF16)
    P(k, lambda e: e.memset(mtmp[:], 0.0), w=[mtmp])
    P(k, lambda e: e.affine_select(mtmp[:], mtmp[:], pattern=[[1, 128]], compare_op=ALU.is_ge,
                                   fill=NEG, base=0, channel_multiplier=-1), r=[mtmp], w=[mtmp])
    V(k, lambda e: e.tensor_copy(mask_cur[:], mtmp[:]), r=[mtmp], w=[mask_cur])
    P(k, lambda e: e.memset(mtmp[:], 0.0), r=[mtmp], w=[mtmp])
    P(k, lambda e: e.affine_select(mtmp[:], mtmp[:], pattern=[[-1, 128]], compare_op=ALU.is_ge,
                                   fill=NEG, base=0, channel_multiplier=1), r=[mtmp], w=[mtmp])
    V(k, lambda e: e.tensor_copy(mask_prev[:], mtmp[:]), r=[mtmp], w=[mask_prev])
    k.dma('pool', mask_pfx[:], mpfx[:, :], writes=[mask_pfx])
    ones_bf = k.sb("ones_bf", [128, 128], BF16)
    P(k, lambda e: e.memset(ones_bf[:], 1.0), w=[ones_bf])
    blk64 = k.sb("blk64", [128, 128], BF16)
    P(k, lambda e: e.memset(blk64[:], 0.0), w=[blk64])
    P(k, lambda e: e.memset(blk64[0:64, 0:64], 1.0), w=[blk64])
    P(k, lambda e: e.memset(blk64[64:128, 64:128], 1.0), w=[blk64])
    gcol1 = k.sb("gcol1", [128, 8], F32)
    k.dma('sp', gcol1[:], ln1_g.rearrange("(c p) -> p c", p=128), writes=[gcol1], allow_slow_non_contiguous=True)
    mhalf = k.sb("mhalf", [128, 1], F32)
    P(k, lambda e: e.memset(mhalf[:], -0.5), w=[mhalf])

    k.push()
    xnT = k.sb("xnT", [128, 8, T_ALL], BF16)
    hAT = k.sb("hAT", [128, 4, T_OWN], BF16)

    k.push()
    wm = k.sb("wm", [128, 8, 2048], BF16)

    def norm_T(groups, dstT, gcol, tag):
        k.push()
        ntmax = max(nt for _, nt in groups)
        xgs = [k.sb(f"xg{tag}{i}", [128, ntmax, 1024], F32) for i in range(6)]
        xss = [k.sb(f"xs{tag}{i}", [128, 1024], BF16) for i in range(2)]
        junk = k.sb(f"junk{tag}", [128, 1024], BF16)
        items = []
        i = 0
        for gi, (src, nt) in enumerate(groups):
            xg = xgs[gi % 6]
            qn = ('sp', 'act', 'pool')[gi % 3]
            for t in range(nt):
                xs = xss[i % 2]
                bank = 6 + (i % 2)
                ss = k.sb(f"ss{tag}{i}", [128, 1], F32)
                rs = k.sb(f"rs{tag}{i}", [128, 1], F32)

                def s1(xg=xg, t=t, ss=ss, rs=rs, src=src, qn=qn, nt=nt):
                    if t == 0:
                        k.dma(qn, xg[:, 0:nt, :], src, writes=[xg])
                    A(k, lambda e: e.activation(junk[:], xg[:, t, :], AF.Square, accum_out=ss[:]), r=[xg], w=[junk, ss])
                    V(k, lambda e: e.tensor_scalar(ss[:], ss[:], 1.0 / 1024, EPS, ALU.mult, ALU.add), r=[ss], w=[ss])
                    P(k, lambda e: e.tensor_tensor(rs[:], ss[:], mhalf[:], ALU.pow), r=[ss, mhalf], w=[rs])

                def s2(xg=xg, t=t, xs=xs, rs=rs, bank=bank, i=i):
                    A(k, lambda e: e.activation(xs[:], xg[:, t, :], AF.Copy, scale=rs[:, 0:1]), r=[xg, rs], w=[xs])
                    for c in range(8):
                        M(k, lambda e, c=c: e.transpose(pbb[bank][:, c * 128:(c + 1) * 128], xs[:, c * 128:(c + 1) * 128], ident[:]),
                          r=[xs, ident], w=[pb[bank]])
                    V(k, lambda e: e.tensor_tensor(dstT[:, :, i * 128:(i + 1) * 128],
                                                   pbb[bank][:, :].rearrange("p (c t) -> p c t", c=8),
                                                   gcol[:].unsqueeze(2).broadcast_to([128, 8, 128]), ALU.mult),
                      r=[pb[bank], gcol], w=[dstT])
                items.append((s1, (lambda: None), s2))
                i += 1
        pipeN(items)
        k.pop()

    groups = [(xsrc[g2 * 256:(g2 + 1) * 256, :].rearrange("(t p) n -> p t n", p=128), 2)
              for xsrc in (x_pre, x_own) for g2 in range(8)]
    norm_T(groups, xnT, gcol1, "a")

    if "xnT" in dbg:
        o = dout("d_xnT", [128, 8, T_ALL])
        k.push()
        t32 = k.sb("dbg32", [128, 8, T_ALL // 2], F32)
        for hh in range(2):
            V(k, lambda e: e.tensor_copy(t32[:], xnT[:, :, hh * 2048:(hh + 1) * 2048]), r=[xnT], w=[t32])
            k.dma('sp', o[:, :, hh * 2048:(hh + 1) * 2048], t32[:], reads=[t32])
        k.pop()

    def finish():
        while k.stack:
            k.pop()
        for e in k.eng:
            k.wait_all(e, k.scope_bufs[0])
        k.barrier()
        k.close()
        return nc, dbg_out, k

    if upto == "A":
        return finish()

    COLS = k.sb("COLS", [128, 32, 3, 4], F32)
    MUB = k.sb("MUB", [128, 33, 4], F32)
    W_end = k.sb("W_end", [128, 32, 4], F32)
    DECAY = k.sb("DECAY", [128, 32, 4], F32)
    EA = k.sb("EA", [128, 32, 4], F32)
    EAb = k.sb("EAb", [128, 32, 4], BF16)
    FLOOR2 = k.sb("FLOOR2", [128, 32, 4], F32)
    k.push()
    RT = k.sb("RT", [128, T_ALL], F32)
    PMA = k.sb("PMA", [128, T_ALL], F32)
    wg = k.sb("wg", [128, 8, 8], BF16)
    gb = k.sb("gb", [128, 2], F32)
    tmpg = k.sb("tmpg", [128, 2, 512], F32)
    sel127 = k.sb("sel127", [128, 128], F32)
    k.dma('pool', wg[:], w_in[:, O_MI:O_MI + 8].rearrange("(c p) n -> p c n", p=128), writes=[wg])
    for c in range(8):
        k.dma('pool', wm[:, c, :], w_in[c * 128:(c + 1) * 128, 0:2048], writes=[wm])
    k.dma('sp', gb[0:4, 0:1], m_i_b.rearrange("(p o) -> p o", o=1), writes=[gb])
    k.dma('sp', gb[64:68, 1:2], m_f_b.rearrange("(p o) -> p o", o=1), writes=[gb])
    k.dma('sp', RT[32:36, :], padd[:, :], writes=[RT])
    k.dma('sp', PMA[64:68, :], pmul[:, :], writes=[PMA])
    V(k, lambda e: e.tensor_scalar(gb[64:68, 1:2], gb[64:68, 1:2], -1.0, None, ALU.mult), r=[gb], w=[gb])
    P(k, lambda e: e.memset(sel127[:], 0.0), w=[sel127])
    P(k, lambda e: e.memset(sel127[96:128, :], 1.0), w=[sel127])
    P(k, lambda e: e.affine_select(sel127[96:128, :], sel127[96:128, :], pattern=[[0, 128]], compare_op=ALU.is_ge,
                                   fill=0.0, base=-31, channel_multiplier=1), r=[sel127], w=[sel127])
    for tg in range(8):
        cs = slice(tg * 512, (tg + 1) * 512)
        bi, bf = pb[(2 * tg) % 6], pb[(2 * tg + 1) % 6]
        for c in range(8):
            M(k, lambda e, c=c: e.matmul(bi[0:4, :], wg[:, c, 0:4], xnT[:, c, cs], start=(c == 0), stop=(c == 7)),
              r=[wg, xnT], w=[bi])
        for c in range(8):
            M(k, lambda e, c=c: e.matmul(bf[64:68, :], wg[:, c, 4:8], xnT[:, c, cs], start=(c == 0), stop=(c == 7),
                                         tile_position=(0, 64)), r=[wg, xnT], w=[bf])
        V(k, lambda e: e.tensor_scalar(RT[0:4, cs], bi[0:4, :], gb[0:4, 0:1], None, ALU.add), r=[bi, gb], w=[RT])
        A(k, lambda e: e.activation(tmpg[64:68, 0, :], bf[64:68, :], AF.Exp, bias=gb[64:68, 1:2], scale=-1.0),
          r=[bf, gb], w=[tmpg])
        A(k, lambda e: e.activation(tmpg[64:68, 1, :], tmpg[64:68, 0, :], AF.Ln, bias=1.0), r=[tmpg], w=[tmpg])
        V(k, lambda e: e.scalar_tensor_tensor(RT[64:68, cs], tmpg[64:68, 1, :], -0.5, PMA[64:68, cs], ALU.mult, ALU.mult),
          r=[tmpg, PMA], w=[RT])
    V(k, lambda e: e.tensor_tensor_scan(PMA[0:4, :], RT[64:68, :], RT[64:68, :], 0.0, ALU.add, ALU.add),
      r=[RT, PMA], w=[PMA])
    V(k, lambda e: e.tensor_tensor(PMA[32:36, :], RT[0:4, :], PMA[0:4, :], ALU.subtract), r=[RT, PMA], w=[PMA])
    V(k, lambda e: e.tensor_tensor(RT[0:4, :], PMA[32:36, :], RT[32:36, :], ALU.add), r=[RT, PMA], w=[RT])
    V(k, lambda e: e.tensor_tensor_scan(RT[32:36, :], RT[0:4, :], RT[0:4, :], 0.0, ALU.max, ALU.max),
      r=[RT], w=[RT])
    V(k, lambda e: e.tensor_copy(RT[64:68, :], PMA[0:4, :]), r=[PMA, RT], w=[RT])
    for q in range(8):
        bank = pb[q % 4]
        for j in range(4):
            c = q * 4 + j
            M(k, lambda e, c=c, j=j: e.transpose(bank[:, j * 128:(j + 1) * 128], RT[:, c * 128:(c + 1) * 128], identf[:]),
              r=[RT, identf], w=[bank])
        V(k, lambda e: e.tensor_copy(COLS[:, q * 4:(q + 1) * 4, :, :],
                                     bank[:, :].rearrange("p (c q r) -> p c q r", c=4, q=4)[:, :, 0:3, 0:4]),
          r=[bank], w=[COLS])
    M(k, lambda e: e.matmul(pb[4][:, 0:384], sel127[:], COLS[:].rearrange("p c q r -> p (c q r)"), start=True, stop=True),
      r=[sel127, COLS], w=[pb[4]])
    P(k, lambda e: e.memset(MUB[:, 0, :], 0.0), w=[MUB])
    V(k, lambda e: e.tensor_copy(MUB[:, 1:33, :], pb[4][:, 0:384].rearrange("p (c q r) -> p c q r", c=32, q=3)[:, :, 1, :]),
      r=[pb[4]], w=[MUB])
    gt = k.sb("gt", [128, 4, 32, 4], F32)
    V(k, lambda e: e.tensor_tensor(gt[:, 0], COLS[:, :, 0, :], MUB[:, 0:32, :], ALU.subtract), r=[MUB, COLS], w=[gt])
    V(k, lambda e: e.tensor_tensor(gt[:, 1], COLS[:, :, 0, :], MUB[:, 1:33, :], ALU.subtract), r=[MUB, COLS], w=[gt])
    V(k, lambda e: e.tensor_tensor(gt[:, 2], MUB[:, 0:32, :], MUB[:, 1:33, :], ALU.subtract), r=[MUB], w=[gt])
    V(k, lambda e: e.tensor_tensor(gt[:, 3], COLS[:, :, 2, :], MUB[:, 0:32, :], ALU.add), r=[COLS, MUB], w=[gt])
    A(k, lambda e: e.activation(EA[:], gt[:, 0], AF.Exp), r=[gt], w=[EA])
    A(k, lambda e: e.activation(W_end[:], gt[:, 1], AF.Exp), r=[gt], w=[W_end])
    A(k, lambda e: e.activation(DECAY[:], gt[:, 2], AF.Exp), r=[gt], w=[DECAY])
    A(k, lambda e: e.activation(FLOOR2[:], gt[:, 3], AF.Exp, scale=-1.0), r=[gt], w=[FLOOR2])
    V(k, lambda e: e.tensor_scalar(W_end[:], W_end[:], 128.0 ** -0.5, None, ALU.mult), r=[W_end], w=[W_end])
    V(k, lambda e: e.tensor_copy(EAb[:], EA[:]), r=[EA], w=[EAb])
    k.pop()

    if "gates" in dbg:
        o = dout("d_cols", [128, 32 * 12])
        k.dma('sp', o[:, :], COLS[:].rearrange("p c q r -> p (c q r)"), reads=[COLS])
        o = dout("d_mub", [128, 33 * 4])
        k.dma('sp', o[:, :], MUB[:].rearrange("p c r -> p (c r)"), reads=[MUB])
        o = dout("d_wend", [128, 32 * 4])
        k.dma('sp', o[:, :], W_end[:].rearrange("p c r -> p (c r)"), reads=[W_end])
    if upto == "B":
        return finish()

    k.push()
    cw = k.sb("cw", [128, 4, 8], F32)
    for tap in range(4):
        k.dma('sp', cw[:, tap, :], m_conv[tap, :].rearrange("(j p) -> p j", p=128), writes=[cw],
              allow_slow_non_contiguous=True)
    dg = k.sb("dg", [128, 8, 4, 128], BF16)
    for j in range(8):
        for tap in range(4):
            V(k, lambda e, j=j, tap=tap: e.tensor_scalar(dg[:, j, tap, :], ident[:], cw[:, tap, j:j + 1], None, ALU.mult),
              r=[ident, cw], w=[dg])
    mask01 = k.sb("mask01", [128, 128], BF16)
    P(k, lambda e: e.memset(mtmp[:], 128.0 ** -0.5), r=[mtmp], w=[mtmp])
    P(k, lambda e: e.affine_select(mtmp[:], mtmp[:], pattern=[[1, 128]], compare_op=ALU.is_ge,
                                   fill=0.0, base=0, channel_multiplier=-1), r=[mtmp], w=[mtmp])
    V(k, lambda e: e.tensor_copy(mask01[:], mtmp[:]), r=[mtmp], w=[mask01])
    mng = k.sb("mng", [128, 512], F32)
    k.dma('sp', mng[:], m_norm_g.partition_broadcast(128), writes=[mng])
    zbs = [k.sb(f"zb{i}", [128, 515], BF16) for i in range(3)]
    carry = k.sb("carry", [128, 8, 3], BF16)
    P(k, lambda e: e.memset(carry[:], 0.0), w=[carry])
    qkT = [k.sb(f"qkT{i}", [128, 8, 512], BF16) for i in range(2)]
    vaug = [k.sb(f"vaug{i}", [128, 4, 128], BF16) for i in range(3)]
    vsc = [k.sb(f"vsc{i}", [128, 4, 128], BF16) for i in range(3)]
    gmo = [k.sb(f"gmo{i}", [128, 512], BF16) for i in range(3)]
    sgt = [k.sb(f"sgt{i}", [128, 512], F32) for i in range(2)]
    kw = [k.sb(f"kw{i}", [128, 4, 128], BF16) for i in range(3)]
    Cst = k.sb("Cst", [128, 4, 128], F32)
    nst = k.sb("nst", [128, 4], F32)
    Cbs = [k.sb(f"Cb{i}", [128, 4, 128], BF16) for i in range(2)]
    nbs = [k.sb(f"nb{i}", [128, 4], BF16) for i in range(2)]
    P(k, lambda e: e.memset(Cst[:], 0.0), w=[Cst])
    P(k, lambda e: e.memset(nst[:], 0.0), w=[nst])
    for i in range(2):
        P(k, lambda e, i=i: e.memset(Cbs[i][:], 0.0), w=[Cbs[i]])
        P(k, lambda e, i=i: e.memset(nbs[i][:], 0.0), w=[nbs[i]])
    DT = [k.sb(f"DT{i}", [128, 4, 128], BF16) for i in range(3)]
    hns = [k.sb(f"hns{i}", [128, 4, 128], F32) for i in range(2)]
    hsq = [k.sb(f"hsq{i}", [128, 4, 128], BF16) for i in range(2)]
    hA = [k.sb(f"hA{i}", [128, 4, 128], BF16) for i in range(2)]
    rr = 0
    Sb, ABb, XB_ = [pb[3], pb[4]], [pb[5], pb[6]], pb[7]

    def gbank():
        nonlocal rr
        b = rr % 3
        rr += 1
        return b

    nz = [0]

    def supertile(st):
        own = st >= 4
        cs = slice(st * 512, (st + 1) * 512)
        qk = qkT[st % 2]
        jlist = list(range(8)) if st >= 3 else [4, 5, 6, 7]
        items = []
        for j in jlist:
            zb = zbs[nz[0] % 3]
            nz[0] += 1

            def s1(j=j, zb=zb):
                b = gbank()
                for c in range(8):
                    M(k, lambda e, c=c: e.matmul(pb[b][:, :], wm[:, c, j * 128:(j + 1) * 128], xnT[:, c, cs],
                                                 start=(c == 0), stop=(c == 7)), r=[wm, xnT], w=[pb[b]])
                P(k, lambda e: e.tensor_copy(zb[:, 0:3], carry[:, j, :]), r=[carry], w=[zb])
                A(k, lambda e: e.activation(zb[:, 3:515], pb[b][:, :], AF.Copy), r=[pb[b]], w=[zb])
                P(k, lambda e: e.tensor_copy(carry[:, j, :], zb[:, 512:515]), r=[zb], w=[carry])

            def s2(j=j, zb=zb):
                if not (own or j >= 4):
                    return
                b2 = gbank()
                for tap in range(4):
                    M(k, lambda e, tap=tap: e.matmul(pb[b2][:, :], dg[:, j, tap, :], zb[:, tap:tap + 512],
                                                     start=(tap == 0), stop=(tap == 3)), r=[dg, zb], w=[pb[b2]])
                A(k, lambda e: e.activation(qk[:, j, :], pb[b2][:, :], AF.Silu), r=[pb[b2]], w=[qk])
            items.append((s1, s2))
        pipe2(items)

    def stageA(c):
        st, ci = c // 4, c % 4
        own = st >= 4
        qk = qkT[st % 2]
        tsl = slice(c * 128, (c + 1) * 128)
        lsl = slice(ci * 128, (ci + 1) * 128)
        va, vs_, kwt = vaug[c % 3], vsc[c % 3], kw[c % 3]
        b = gbank()
        for kk in range(8):
            M(k, lambda e, kk=kk: e.matmul(pb[b][:, :], xnT[:, kk, tsl], wm[:, kk, O_MV:O_MV + 512],
                                           start=(kk == 0), stop=(kk == 7)), r=[wm, xnT], w=[pb[b]])
        A(k, lambda e: e.activation(va[:].rearrange("p h d -> p (h d)"), pb[b][:, :], AF.Copy), r=[pb[b]], w=[va])
        if own:
            V(k, lambda e: e.tensor_tensor(vs_[:], pb[b][:, :].rearrange("p (h d) -> p h d", h=4),
                                           EA[:, c, :].unsqueeze(2).broadcast_to([128, 4, 128]), ALU.mult),
              r=[pb[b], EA], w=[vs_])
        b = gbank()
        for h in range(4):
            M(k, lambda e, h=h: e.transpose(pbb[b][:, h * 128:(h + 1) * 128], qk[:, 4 + h, lsl], ident[:]),
              r=[qk, ident], w=[pb[b]])
        V(k, lambda e: e.tensor_tensor(kwt[:], pbb[b][:, 0:512].rearrange("p (h d) -> p h d", h=4),
                                       W_end[:, c, :].unsqueeze(2).broadcast_to([128, 4, 128]), ALU.mult),
          r=[pb[b], W_end], w=[kwt])
        if own:
            g_, sg = gmo[c % 3], sgt[c % 2]
            b = gbank()
            for kk in range(8):
                M(k, lambda e, kk=kk: e.matmul(pb[b][:, :], xnT[:, kk, tsl], wm[:, kk, O_MO:O_MO + 512],
                                               start=(kk == 0), stop=(kk == 7)), r=[wm, xnT], w=[pb[b]])
            A(k, lambda e: e.activation(sg[:], pb[b][:, :], AF.Sigmoid), r=[pb[b]], w=[sg])
            P(k, lambda e: e.tensor_tensor(g_[:], sg[:], mng[:], ALU.mult), r=[sg, mng], w=[g_])
            S_ = Sb[c % 2]
            for h in range(4):
                M(k, lambda e, h=h: e.matmul(S_[:, h * 128:(h + 1) * 128], qk[:, 4 + h, lsl], qk[:, h, lsl],
                                             start=True, stop=True), r=[qk], w=[S_])
            dt = DT[c % 3]
            V(k, lambda e: e.tensor_tensor(dt[:], S_[:, :].rearrange("p (h d) -> p h d", h=4),
                                           mask01[:].unsqueeze(1).broadcast_to([128, 4, 128]), ALU.mult),
              r=[S_, mask01], w=[dt])

    def stageB(c):
        st, ci = c // 4, c % 4
        own = st >= 4
        qk = qkT[st % 2]
        lsl = slice(ci * 128, (ci + 1) * 128)
        va, vs_, kwt = vaug[c % 3], vsc[c % 3], kw[c % 3]
        Cb, nb_ = Cbs[c % 2], nbs[c % 2]
        b = gbank()
        for h in range(4):
            M(k, lambda e, h=h: e.matmul(pb[b][:, h * 128:(h + 1) * 128], kwt[:, h, :], va[:, h, :],
                                         start=True, stop=True), r=[kwt, va], w=[pb[b]])
        for h in range(4):
            M(k, lambda e, h=h: e.matmul(XB_[:, 8 + h:9 + h], kwt[:, h, :], ones_bf[:, 0:1],
                                         start=True, stop=True), r=[kwt, ones_bf], w=[XB_])
        if own:
            dt = DT[c % 3]
            AB_ = ABb[c % 2]
            xo = (c % 2) * 4
            for h in range(4):
                M(k, lambda e, h=h: e.matmul(AB_[:, h * 128:(h + 1) * 128], dt[:, h, :], vs_[:, h, :],
                                             start=True, stop=False), r=[dt, vs_], w=[AB_])
                M(k, lambda e, h=h: e.matmul(AB_[:, h * 128:(h + 1) * 128], qk[:, h, lsl], Cb[:, h, :],
                                             start=False, stop=True), r=[qk, Cb], w=[AB_])
            for h in range(4):
                M(k, lambda e, h=h: e.matmul(XB_[:, xo + h:xo + h + 1], dt[:, h, :], EAb[:, c, h:h + 1],
                                             start=True, stop=False), r=[dt, EAb], w=[XB_])
                M(k, lambda e, h=h: e.matmul(XB_[:, xo + h:xo + h + 1], qk[:, h, lsl], nb_[:, h:h + 1],
                                             start=False, stop=True), r=[qk, nb_], w=[XB_])
        V(k, lambda e: e.tensor_tensor(Cst[:], Cst[:], DECAY[:, c, :].unsqueeze(2).broadcast_to([128, 4, 128]), ALU.mult),
          r=[Cst, DECAY], w=[Cst])
        V(k, lambda e: e.tensor_tensor(Cst[:].rearrange("p h d -> p (h d)"), Cst[:].rearrange("p h d -> p (h d)"),
                                       pb[b][:, :], ALU.add), r=[Cst, pb[b]], w=[Cst])
        V(k, lambda e: e.tensor_tensor(nst[:], nst[:], DECAY[:, c, :], ALU.mult), r=[nst, DECAY], w=[nst])
        V(k, lambda e: e.tensor_tensor(nst[:], nst[:], XB_[:, 8:12], ALU.add), r=[nst, XB_], w=[nst])
        if c >= 15 and c < 31:
            Cn, nn = Cbs[(c + 1) % 2], nbs[(c + 1) % 2]
            A(k, lambda e: e.activation(Cn[:], Cst[:], AF.Copy), r=[Cst], w=[Cn])
            A(k, lambda e: e.activation(nn[:], nst[:], AF.Copy), r=[nst], w=[nn])
        if own:
            sm = k.sb(f"sm{c}", [128, 8, 4], F32)
            V(k, lambda e: e.tensor_copy(sm[:, 0, :], XB_[:, xo:xo + 4]), r=[XB_], w=[sm])
            return sm
        return None

    def stageN(c, sm):
        oc = c - 16
        g_ = gmo[c % 3]
        AB_ = ABb[c % 2]
        V(k, lambda e: e.scalar_tensor_tensor(sm[:, 1, :], sm[:, 0, :], -1.0, sm[:, 0, :], ALU.mult, ALU.max), r=[sm], w=[sm])
        V(k, lambda e: e.tensor_tensor(sm[:, 1, :], sm[:, 1, :], FLOOR2[:, c, :], ALU.max), r=[sm, FLOOR2], w=[sm])
        V(k, lambda e: e.reciprocal(sm[:, 2, :], sm[:, 1, :]), r=[sm], w=[sm])
        hq = hsq[c % 2]
        A(k, lambda e: e.activation(hq[:].rearrange("p h d -> p (h d)"), AB_[:, :], AF.Square), r=[AB_], w=[hq])
        V(k, lambda e: e.tensor_reduce(sm[:, 3, :], hq[:], AX.X, ALU.add), r=[hq], w=[sm])
        V(k, lambda e: e.tensor_tensor(sm[:, 4, :], sm[:, 2, :], sm[:, 2, :], ALU.mult), r=[sm], w=[sm])
        V(k, lambda e: e.tensor_tensor(sm[:, 4, :], sm[:, 4, :], sm[:, 3, :], ALU.mult), r=[sm], w=[sm])
        V(k, lambda e: e.tensor_scalar(sm[:, 4, :], sm[:, 4, :], 1.0 / 128, EPS, ALU.mult, ALU.add), r=[sm], w=[sm])
        P(k, lambda e: e.tensor_tensor(sm[:, 5, :], sm[:, 4, :], mhalf[:].broadcast_to([128, 4]), ALU.pow),
          r=[sm, mhalf], w=[sm])
        V(k, lambda e: e.tensor_tensor(sm[:, 6, :], sm[:, 5, :], sm[:, 2, :], ALU.mult), r=[sm], w=[sm])
        hn, ha = hns[c % 2], hA[c % 2]
        V(k, lambda e: e.tensor_tensor(hn[:], AB_[:, :].rearrange("p (h d) -> p h d", h=4),
                                       sm[:, 6, :].unsqueeze(2).broadcast_to([128, 4, 128]), ALU.mult),
          r=[AB_, sm], w=[hn])
        P(k, lambda e: e.tensor_tensor(ha[:].rearrange("p h d -> p (h d)"), hn[:].rearrange("p h d -> p (h d)"),
                                       g_[:], ALU.mult), r=[hn, g_], w=[ha])

    def stageT(c):
        oc = c - 16
        ha = hA[c % 2]
        b = gbank()
        for h in range(4):
            M(k, lambda e, h=h: e.transpose(pbb[b][:, h * 128:(h + 1) * 128], ha[:, h, :], ident[:]),
              r=[ha, ident], w=[pb[b]])
        A(k, lambda e: e.activation(hAT[:, :, oc * 128:(oc + 1) * 128],
                                    pbb[b][:, 0:512].rearrange("p (h d) -> p h d", h=4), AF.Copy),
          r=[pb[b]], w=[hAT])

    supertile(0)
    stageA(0)
    pendN = None
    pendT = None
    for c in range(32):
        if c % 4 == 2 and c // 4 + 1 < 8:
            supertile(c // 4 + 1)
        if c + 1 < 32:
            stageA(c + 1)
        sm = stageB(c)
        if pendT is not None:
            stageT(pendT)
            pendT = None
        if pendN is not None:
            stageN(*pendN)
            pendT = pendN[0]
        pendN = (c, sm) if sm is not None else None
    stageT(pendT)
    stageN(*pendN)
    stageT(pendN[0])
    k.pop()
    k.pop()
    hBT = k.sb("hBT", [128, 2, T_OWN], BF16)

    if "hA" in dbg:
        o = dout("d_hAT", [128, 4, T_OWN])
        k.push()
        t32 = k.sb("dbg32b", [128, 4, T_OWN], F32)
        V(k, lambda e: e.tensor_copy(t32[:], hAT[:]), r=[hAT], w=[t32])
        k.dma('sp', o[:, :, :], t32[:], reads=[t32])
        k.pop()
    if upto == "C":
        return finish()

    rr2 = [0]

    def gb3():
        b = rr2[0] % 3
        rr2[0] += 1
        return b

    fm_ctr = [0]
    FM_ACC = [0, 1, 2, 4, 5, 6]
    FM_SSQ = [3, 7]

    def fm_norm(wsel, rhs_sel, n, summat, inv_dim, gcol, dst, dstbuf, rbufs, scr3, outs=None, vw=None, gbuf=None):
        idx = fm_ctr[0]
        fm_ctr[0] += 1
        b = FM_ACC[idx % 6]
        sb_ = FM_SSQ[idx % 2]
        sq, ms, rs = scr3[idx % 3]
        if outs is None:
            outs = [(dst, slice(0, 128))]
        if vw is None:
            vw = lambda ap: ap

        def s1():
            for c in range(8):
                M(k, lambda e, c=c: e.matmul(pb[b][:, 0:n], wsel(c), rhs_sel(c), start=(c == 0), stop=(c == 7)),
                  r=rbufs, w=[pb[b]])
            A(k, lambda e: e.activation(sq[:, 0:n], pb[b][:, 0:n], AF.Square), r=[pb[b]], w=[sq])

        def s2():
            M(k, lambda e: e.matmul(pb[sb_][:, 0:n], summat, sq[:, 0:n], start=True, stop=True), r=[sq], w=[pb[sb_]])
            A(k, lambda e: e.activation(ms[:, 0:n], pb[sb_][:, 0:n], AF.Ln, bias=EPS, scale=inv_dim), r=[pb[sb_]], w=[ms])
            A(k, lambda e: e.activation(rs[:, 0:n], ms[:, 0:n], AF.Exp, scale=-0.5), r=[ms], w=[rs])

        def s3():
            for (d_ap, psl) in outs:
                V(k, lambda e: e.scalar_tensor_tensor(d_ap, vw(pb[b][psl, 0:n]), gcol[psl], vw(rs[psl, 0:n]), ALU.mult, ALU.mult),
                  r=[pb[b], rs, gbuf], w=[dstbuf])
        return (s1, s2, s3)

    k.push()
    DIL = [1, 4, 16]
    wq3 = [k.sb(f"wdil{i}", [128, 8, 256], BF16) for i in range(3)]

    def load_dil(g, which):
        off = (O_DQ, O_DK, O_DV)[which]
        k.dma('pool', wq3[which][:], w_in[:, off + g * 256: off + (g + 1) * 256].rearrange("(c p) n -> p c n", p=128),
              writes=[wq3[which]])

    for which in range(3):
        load_dil(0, which)
    qpad = k.sb("qpad", [128, 2, 2, T_OWN], BF16)
    kTg = k.sb("kTg", [128, 2, T_ALL], BF16)
    vt = k.sb("vt", [128, 32, 256], BF16)
    accB = k.sb("accB", [128, 4, T_OWN], F32)
    gq2 = k.sb("gq2", [128, 3, 2], F32)
    P(k, lambda e: e.memset(qpad[64:128, :, 0, :], 0.0), w=[qpad])
    P(k, lambda e: e.memset(qpad[0:64, :, 1, :], 0.0), w=[qpad])
    mpfx01 = k.sb("mpfx01", [128, 128], BF16)
    k.dma('sp', mtmp[:], mpfx[:, :], writes=[mtmp])
    V(k, lambda e: e.tensor_scalar(mpfx01[:], mtmp[:], -1.0, None, ALU.is_ge), r=[mtmp], w=[mpfx01])
    for g in range(3):
        for hf in range(2):
            k.dma('sp', gq2[hf * 64:(hf + 1) * 64, g, 0:1], dil_q_g[g, :].rearrange("(p o) -> p o", o=1), writes=[gq2])
            k.dma('sp', gq2[hf * 64:(hf + 1) * 64, g, 1:2], dil_k_g[g, :].rearrange("(p o) -> p o", o=1), writes=[gq2])
    V(k, lambda e: e.tensor_scalar(gq2[:, :, 0:1], gq2[:, :, 0:1], 0.125, None, ALU.mult), r=[gq2], w=[gq2])
    scrs = [(k.sb(f"sq{i}", [128, 512], BF16), k.sb(f"ms{i}", [128, 512], F32), k.sb(f"rs{i}", [128, 512], F32))
            for i in range(3)]
    Pcs = [k.sb(f"Pc{i}", [128, 4, 128], BF16) for i in range(2)]
    Pps = [k.sb(f"Pp{i}", [128, 4, 128], BF16) for i in range(2)]
    nfm = 0
    ntile = 0
    for g in range(3):
        r_ = DIL[g]
        nblk = 16 // r_
        NK, NQ = T_ALL // r_, T_OWN // r_
        ktgs = [4, 5, 6, 7] + ([3] if g < 2 else [0, 1, 2, 3])
        if r_ == 1:
            vwn = (lambda ap: ap)
            pv = (lambda ap, a0: ap[:, a0:a0 + 512])
        else:
            vwn = (lambda ap, r_=r_: ap.rearrange("p (a r) -> p r a", r=r_))
            pv = (lambda ap, a0, r_=r_: ap.rearrange("p (r a) -> p r a", r=r_)[:, :, a0:a0 + 512 // r_])
        fitems = []
        for tg in ktgs:
            cs = slice(tg * 512, (tg + 1) * 512)
            for j in range(2):
                if tg >= 4:
                    a0 = (tg - 4) * 512 // r_
                    outs = [(pv(qpad[pp * 64:(pp + 1) * 64, j, pp, :], a0), slice(pp * 64, (pp + 1) * 64)) for pp in range(2)]
                    fitems.append(fm_norm(lambda c, j=j: wq3[0][:, c, j * 128:(j + 1) * 128], lambda c, cs=cs: xnT[:, c, cs], 512, blk64[:],
                                          1.0 / 64, gq2[:, g, 0:1], None, qpad, [xnT, wq3[0]], scrs, outs=outs, vw=vwn, gbuf=gq2))
                    nfm += 1
                a0 = tg * 512 // r_
                outs = [(pv(kTg[:, j, :], a0), slice(0, 128))]
                fitems.append(fm_norm(lambda c, j=j: wq3[1][:, c, j * 128:(j + 1) * 128], lambda c, cs=cs: xnT[:, c, cs], 512, blk64[:],
                                      1.0 / 64, gq2[:, g, 1:2], None, kTg, [xnT, wq3[1]], scrs, outs=outs, vw=vwn, gbuf=gq2))
                nfm += 1
        pipeN(fitems)
        if g + 1 < 3:
            load_dil(g + 1, 0)
            load_dil(g + 1, 1)
        vtiles = []
        for blk in range(nblk):
            for res in range(r_):
                lo = 2048 + blk * 128 * r_ + res
                vtiles.append((blk * r_ + res, slice(lo, lo + 127 * r_ + 1, r_)))
        for res in range(r_):
            lo = 2048 - 128 * r_ + res
            vtiles.append((16 + res, slice(lo, lo + 127 * r_ + 1, r_)))
        for vidx, tsl in vtiles:
            b = gb3()
            for c in range(8):
                M(k, lambda e, c=c: e.matmul(pb[b][:, 0:256], xnT[:, c, tsl], wq3[2][:, c, :], start=(c == 0), stop=(c == 7)),
                  r=[xnT, wq3[2]], w=[pb[b]])
            A(k, lambda e: e.activation(vt[:, vidx, :], pb[b][:, 0:256], AF.Copy), r=[pb[b]], w=[vt])
        if g + 1 < 3:
            load_dil(g + 1, 2)
        aitems = []
        for blk in range(nblk):
            for res in range(r_):
                own_lo = blk * 128 * r_ + res
                qs = slice(own_lo, own_lo + 127 * r_ + 1, r_)
                qp = res * NQ + blk * 128
                kc = res * NK + (2048 + blk * 128 * r_) // r_
                kp = kc - 128
                if blk > 0:
                    vprev = (blk - 1) * r_ + res
                else:
                    vprev = 16 + res
                vcur = blk * r_ + res
                SC, SP = pb[4 + ntile % 2], pb[2 + ntile % 2]
                Pc, Pp = Pcs[ntile % 2], Pps[ntile % 2]
                ob = pb[6 + ntile % 2]

                def s1(SC=SC, SP=SP, Pc=Pc, Pp=Pp, qp=qp, kc=kc, kp=kp, blk=blk):
                    for (bank, k0) in ((SC, kc), (SP, kp)):
                        for j in range(2):
                            for pp in range(2):
                                h = 2 * j + pp
                                M(k, lambda e, h=h, j=j, pp=pp: e.matmul(bank[:, h * 128:(h + 1) * 128], kTg[:, j, k0:k0 + 128],
                                                                        qpad[:, j, pp, qp:qp + 128], start=True, stop=True),
                                  r=[kTg, qpad], w=[bank])
                    A(k, lambda e: e.activation(Pc[:].rearrange("p h t -> p (h t)"), SC[:, :], AF.Exp), r=[SC], w=[Pc])
                    A(k, lambda e: e.activation(Pp[:].rearrange("p h t -> p (h t)"), SP[:, :], AF.Exp), r=[SP], w=[Pp])
                    P(k, lambda e: e.affine_select(Pc[:], Pc[:], pattern=[[0, 4], [1, 128]], compare_op=ALU.is_ge,
                                                   fill=0.0, base=0, channel_multiplier=-1), r=[Pc], w=[Pc])
                    if blk > 0:
                        P(k, lambda e: e.affine_select(Pp[:], Pp[:], pattern=[[0, 4], [-1, 128]], compare_op=ALU.is_ge,
                                                       fill=0.0, base=0, channel_multiplier=1), r=[Pp], w=[Pp])
                    else:
                        P(k, lambda e: e.tensor_tensor(Pp[:], Pp[:], mpfx01[:].unsqueeze(1).broadcast_to([128, 4, 128]), ALU.mult),
                          r=[Pp, mpfx01], w=[Pp])

                def s2(ob=ob, Pc=Pc, Pp=Pp, vprev=vprev, vcur=vcur, qs=qs, g=g):
                    for h in range(4):
                        j, pbs = h // 2, 64 * (h % 2)
                        M(k, lambda e, h=h, j=j, pbs=pbs: e.matmul(ob[pbs:pbs + 64, j * 128:(j + 1) * 128], vt[:, vprev, h * 64:(h + 1) * 64],
                                                                  Pp[:, h, :], start=True, stop=False,
                                                                  tile_position=(0, pbs)), r=[vt, Pp], w=[ob])
                        M(k, lambda e, h=h, j=j, pbs=pbs: e.matmul(ob[pbs:pbs + 64, j * 128:(j + 1) * 128], vt[:, vcur, h * 64:(h + 1) * 64],
                                                                  Pc[:, h, :], start=False, stop=True,
                                                                  tile_position=(0, pbs)), r=[vt, Pc], w=[ob])
                    for h in range(4):
                        j, pbs = h // 2, 64 * (h % 2)
                        M(k, lambda e, h=h, j=j, pbs=pbs: e.matmul(ob[pbs:pbs + 64, 256 + j * 128:256 + (j + 1) * 128], ones_bf[:, 0:64],
                                                                  Pp[:, h, :], start=True, stop=False,
                                                                  tile_position=(0, pbs)), r=[ones_bf, Pp], w=[ob])
                        M(k, lambda e, h=h, j=j, pbs=pbs: e.matmul(ob[pbs:pbs + 64, 256 + j * 128:256 + (j + 1) * 128], ones_bf[:, 0:64],
                                                                  Pc[:, h, :], start=False, stop=True,
                                                                  tile_position=(0, pbs)), r=[ones_bf, Pc], w=[ob])
                    obv = ob[:, :].rearrange("p (s t) -> p s t", s=4)
                    if g == 0:
                        V(k, lambda e: e.tensor_copy(accB[:, :, qs], obv), r=[ob], w=[accB])
                    else:
                        V(k, lambda e: e.tensor_tensor(accB[:, :, qs], obv, accB[:, :, qs], ALU.add), r=[ob, accB], w=[accB])
                aitems.append((s1, s2))
                ntile += 1
        pipe2(aitems)
    A(k, lambda e: e.activation(accB[:, 2:4, :], accB[:, 2:4, :], AF.Ln), r=[accB], w=[accB])
    A(k, lambda e: e.activation(accB[:, 2:4, :], accB[:, 2:4, :], AF.Exp, scale=-1.0), r=[accB], w=[accB])
    V(k, lambda e: e.tensor_tensor(hBT[:], accB[:, 0:2, :], accB[:, 2:4, :], ALU.mult), r=[accB], w=[hBT])
    k.pop()

    if "hB" in dbg:
        o = dout("d_hBT", [128, 2, T_OWN])
        k.push()
        t32 = k.sb("dbg32c", [128, 2, T_OWN], F32)
        V(k, lambda e: e.tensor_copy(t32[:], hBT[:]), r=[hBT], w=[t32])
        k.dma('sp', o[:, :, :], t32[:], reads=[t32])
        k.pop()
    if upto == "D":
        return finish()

    hCT = k.sb("hCT", [128, 4, T_OWN], BF16)
    k.push()
    gcolm = k.sb("gcolm", [128, 8], F32)
    k.dma('sp', gcolm[:], mem_norm_g.rearrange("(c p) -> p c", p=128), writes=[gcolm], allow_slow_non_contiguous=True)
    mnT = k.sb("mnT", [128, 8, 256], BF16)
    norm_T([(mem.rearrange("(t p) n -> p t n", p=128), 2)], mnT, gcolm, "m")
    wkv = k.sb("wkv", [128, 8, 1024], BF16)
    for c in range(8):
        k.dma('pool', wkv[:, c, :], w_mem_kv[c * 128:(c + 1) * 128, :], writes=[wkv])
    wxq = k.sb("wxq", [128, 8, 512], BF16)
    k.dma('pool', wxq[:], w_in[:, O_XQ:O_XQ + 512].rearrange("(c p) n -> p c n", p=128), writes=[wxq])
    gx = k.sb("gx", [128, 2], F32)
    k.dma('sp', gx[:, 0:1], x_q_g.rearrange("(p o) -> p o", o=1), writes=[gx])
    k.dma('sp', gx[:, 1:2], x_k_g.rearrange("(p o) -> p o", o=1), writes=[gx])
    V(k, lambda e: e.tensor_scalar(gx[:, 0:1], gx[:, 0:1], 128.0 ** -0.5, None, ALU.mult), r=[gx], w=[gx])
    kmT = k.sb("kmT", [128, 4, 256], BF16)
    vm = k.sb("vm", [128, 2, 512], BF16)
    xqT = k.sb("xqT", [128, 4, T_OWN], BF16)
    scrs = [(k.sb(f"sqe{i}", [128, 512], BF16), k.sb(f"mse{i}", [128, 512], F32), k.sb(f"rse{i}", [128, 512], F32))
            for i in range(3)]
    nfm = 0
    fitems = []
    for h in range(4):
        fitems.append(fm_norm(lambda c, h=h: wkv[:, c, h * 128:(h + 1) * 128], lambda c: mnT[:, c, :], 256, ones_bf[:], 1.0 / 128,
                              gx[:, 1:2], kmT[:, h, :], kmT, [mnT, wkv], scrs, gbuf=gx))
        nfm += 1
    pipeN(fitems)
    for mt in range(2):
        b = gb3()
        for c in range(8):
            M(k, lambda e, c=c: e.matmul(pb[b][:, :], mnT[:, c, mt * 128:(mt + 1) * 128], wkv[:, c, 512:1024],
                                         start=(c == 0), stop=(c == 7)), r=[mnT, wkv], w=[pb[b]])
        A(k, lambda e: e.activation(vm[:, mt, :], pb[b][:, :], AF.Copy), r=[pb[b]], w=[vm])
    fitems = []
    for tg in range(4):
        cs = slice(2048 + tg * 512, 2048 + (tg + 1) * 512)
        for h in range(4):
            fitems.append(fm_norm(lambda c, h=h: wxq[:, c, h * 128:(h + 1) * 128], lambda c, cs=cs: xnT[:, c, cs], 512, ones_bf[:], 1.0 / 128,
                                  gx[:, 0:1], xqT[:, h, tg * 512:(tg + 1) * 512], xqT, [xnT, wxq], scrs, gbuf=gx))
            nfm += 1
    pipeN(fitems)
    Pm = [[k.sb(f"Pm{i}{mt}", [128, 512], BF16) for mt in range(2)] for i in range(2)]
    rdn = [k.sb(f"rdn{i}", [128, 512], F32) for i in range(2)]
    it = 0
    eitems = []
    for tg in range(4):
        ts_ = slice(tg * 512, (tg + 1) * 512)
        for h in range(4):
            sbs = (pb[4], pb[5]) if it % 2 == 0 else (pb[2], pb[3])
            Pmi = Pm[it % 2]
            rd = rdn[it % 2]

            def s1(h=h, ts_=ts_, sbs=sbs, Pmi=Pmi):
                for mt in range(2):
                    sb_ = sbs[mt]
                    M(k, lambda e: e.matmul(sb_[:, :], kmT[:, h, mt * 128:(mt + 1) * 128], xqT[:, h, ts_], start=True, stop=True),
                      r=[kmT, xqT], w=[sb_])
                    A(k, lambda e: e.activation(Pmi[mt][:], sb_[:, :], AF.Exp), r=[sb_], w=[Pmi[mt]])

            def s2(h=h, ts_=ts_, Pmi=Pmi, rd=rd):
                nb6, db7 = pb[6], pb[7]
                for mt in range(2):
                    M(k, lambda e: e.matmul(nb6[:, :], vm[:, mt, h * 128:(h + 1) * 128], Pmi[mt][:], start=(mt == 0), stop=(mt == 1)),
                      r=[vm, Pmi[mt]], w=[nb6])
                for mt in range(2):
                    M(k, lambda e: e.matmul(db7[:, :], ones_bf[:], Pmi[mt][:], start=(mt == 0), stop=(mt == 1)),
                      r=[ones_bf, Pmi[mt]], w=[db7])
                A(k, lambda e: e.activation(rd[:], db7[:, :], AF.Ln), r=[db7], w=[rd])
                A(k, lambda e: e.activation(rd[:], rd[:], AF.Exp, scale=-1.0), r=[rd], w=[rd])
                V(k, lambda e: e.tensor_tensor(hCT[:, h, ts_], nb6[:, :], rd[:], ALU.mult), r=[nb6, rd], w=[hCT])
            eitems.append((s1, s2))
            it += 1
    pipe2(eitems)
    k.pop()

    if "hC" in dbg:
        o = dout("d_hCT", [128, 4, T_OWN])
        k.push()
        t32 = k.sb("dbg32d", [128, 4, T_OWN], F32)
        V(k, lambda e: e.tensor_copy(t32[:], hCT[:]), r=[hCT], w=[t32])
        k.dma('sp', o[:, :, :], t32[:], reads=[t32])
        k.pop()
    if upto == "E":
        return finish()

    MIXT_OFF = 195584
    OUT_OFF = 130048
    mixT = k.sb_at("mixT", [128, 8, T_OWN], BF16, MIXT_OFF)
    k.limit = MIXT_OFF
    rr8 = [0]

    def nbk():
        b = pb[rr8[0] % 8]
        rr8[0] += 1
        return b

    k.push()
    bg = k.sb("bg", [128, 3, 8], F32)
    for b in range(3):
        k.dma('sp', bg[:, b, :], b_gate[b * 1024:(b + 1) * 1024].rearrange("(j p) -> p j", p=128), writes=[bg],
              allow_slow_non_contiguous=True)
    wgj = [k.sb(f"wgj{i}", [128, 8, 3, 128], BF16) for i in range(3)]
    waj = [k.sb(f"waj{i}", [128, 4, 128], BF16) for i in range(3)]
    wbj = [k.sb(f"wbj{i}", [128, 2, 128], BF16) for i in range(3)]
    wcj = [k.sb(f"wcj{i}", [128, 4, 128], BF16) for i in range(3)]

    def loadF(j):
        w = j % 3
        for b in range(3):
            k.dma('pool', wgj[w][:, :, b, :], w_gate[:, b * 1024 + j * 128:b * 1024 + (j + 1) * 128].rearrange("(c p) n -> p c n", p=128),
                  writes=[wgj[w]])
        k.dma('pool', waj[w][:], w_a[:, j * 128:(j + 1) * 128].rearrange("(c p) n -> p c n", p=128), writes=[waj[w]])
        k.dma('pool', wbj[w][:], w_b[:, j * 128:(j + 1) * 128].rearrange("(c p) n -> p c n", p=128), writes=[wbj[w]])
        k.dma('pool', wcj[w][:], w_c[:, j * 128:(j + 1) * 128].rearrange("(c p) n -> p c n", p=128), writes=[wcj[w]])

    loadF(0)
    loadF(1)
    sig = [k.sb(f"sig{i}", [128, 3, 512], F32) for i in range(2)]
    mm_ = [k.sb(f"mm{i}", [128, 3, 512], F32) for i in range(2)]
    for j in range(8):
        w = j % 3
        wg_ = wgj[w]
        for tg in range(4):
            it = j * 4 + tg
            sg, m_ = sig[it % 2], mm_[it % 2]
            cs = slice(2048 + tg * 512, 2048 + (tg + 1) * 512)
            ts_ = slice(tg * 512, (tg + 1) * 512)
            for b in range(3):
                bank = nbk()
                for c in range(8):
                    M(k, lambda e, c=c: e.matmul(bank[:, :], wg_[:, c, b, :], xnT[:, c, cs],
                                                 start=(c == 0), stop=(c == 7)), r=[wg_, xnT], w=[bank])
                A(k, lambda e: e.activation(sg[:, b, :], bank[:, :], AF.Sigmoid, bias=bg[:, b, j:j + 1]), r=[bank, bg], w=[sg])
            for (b, wt, hT_, nk) in ((0, waj[w], hAT, 4), (1, wbj[w], hBT, 2), (2, wcj[w], hCT, 4)):
                bank = nbk()
                for c in range(nk):
                    M(k, lambda e, c=c: e.matmul(bank[:, :], wt[:, c, :], hT_[:, c, ts_], start=(c == 0), stop=(c == nk - 1)),
                      r=[wt, hT_], w=[bank])
                V(k, lambda e: e.tensor_tensor(m_[:, b, :], bank[:, :], sg[:, b, :], ALU.mult), r=[bank, sg], w=[m_])
            P(k, lambda e: e.tensor_tensor(m_[:, 0, :], m_[:, 0, :], m_[:, 1, :], ALU.add), r=[m_], w=[m_])
            P(k, lambda e: e.tensor_tensor(mixT[:, j, ts_], m_[:, 0, :], m_[:, 2, :], ALU.add), r=[m_], w=[mixT])
            if tg == 0 and j + 2 < 8:
                loadF(j + 2)
    k.pop()
    k.pop()

    out_acc = k.sb_at("out_acc", [128, 16, 1024], F32, OUT_OFF)
    k.limit = OUT_OFF
    k.push()
    oacc = [k.view(out_acc, f"oacc{i}") for i in range(16)]
    hnT = k.sb("hnT", [128, 8, T_OWN], BF16)
    combT = k.sb("combT", [32, T_OWN], BF16)
    k.push()
    wo = k.sb("wo", [128, 8, 1024], BF16)
    for c in range(8):
        k.dma('pool', wo[:, c, :], w_out[c * 128:(c + 1) * 128, :], writes=[wo])
    xrs = [k.sb(f"xr{i}", [128, 1024], F32) for i in range(3)]

    def outproj(i):
        xt = xrs[i % 3]
        k.dma('sp', xt[:], x_own[i * 128:(i + 1) * 128, :], writes=[xt])
        for hf in range(2):
            bank = nbk()
            for c in range(8):
                M(k, lambda e, c=c: e.matmul(bank[:, :], mixT[:, c, i * 128:(i + 1) * 128], wo[:, c, hf * 512:(hf + 1) * 512],
                                             start=(c == 0), stop=(c == 7)), r=[mixT, wo], w=[bank])
            V(k, lambda e: e.tensor_tensor(out_acc[:, i, hf * 512:(hf + 1) * 512], bank[:, :], xt[:, hf * 512:(hf + 1) * 512], ALU.add),
              r=[bank, xt], w=[oacc[i]])

    gcol2 = k.sb("gcol2", [128, 8], F32)
    k.dma('sp', gcol2[:], ln2_g.rearrange("(c p) -> p c", p=128), writes=[gcol2], allow_slow_non_contiguous=True)
    wr32 = k.sb("wr32", [128, 8, 36], F32)
    k.dma('sp', wr32[:, :, 0:4], w_grp.rearrange("(c p) n -> p c n", p=128), writes=[wr32])
    for g in range(4):
        k.dma('sp', wr32[:, :, 4 + 8 * g:12 + 8 * g], w_er[g, :, :].rearrange("(c p) n -> p c n", p=128), writes=[wr32])
    brow = k.sb("brow", [128, 36], F32)
    k.dma('sp', brow[:, 0:4], b_grp.partition_broadcast(128), writes=[brow])
    k.dma('sp', brow[:, 4:36], b_er.partition_broadcast(128), writes=[brow])
    LG = k.sb("LG", [128, 16, 36], F32)
    hss = [k.sb(f"hs{i}", [128, 1024], F32) for i in range(2)]
    hn32 = [k.sb(f"hn32T{i}", [128, 8, 128], F32) for i in range(2)]
    junk2 = k.sb("junk2", [128, 1024], BF16)
    gitems = []
    for i in range(16):
        hs, h32 = hss[i % 2], hn32[i % 2]
        ss = k.sb(f"ssg{i}", [128, 1], F32)
        rs = k.sb(f"rsg{i}", [128, 1], F32)

        def s1(i=i, hs=hs, ss=ss, rs=rs):
            A(k, lambda e: e.activation(junk2[:], out_acc[:, i, :], AF.Square, accum_out=ss[:]), r=[oacc[i]], w=[junk2, ss])
            V(k, lambda e: e.tensor_scalar(ss[:], ss[:], 1.0 / 1024, EPS, ALU.mult, ALU.add), r=[ss], w=[ss])
            P(k, lambda e: e.tensor_tensor(rs[:], ss[:], mhalf[:], ALU.pow), r=[ss, mhalf], w=[rs])
            A(k, lambda e: e.activation(hs[:], out_acc[:, i, :], AF.Copy, scale=rs[:, 0:1]), r=[oacc[i], rs], w=[hs])

        def s2(i=i, hs=hs, h32=h32):
            for q in range(2):
                bank = nbk()
                for c in range(4):
                    M(k, lambda e, c=c: e.transpose(bank[:, c * 128:(c + 1) * 128], hs[:, (4 * q + c) * 128:(4 * q + c + 1) * 128], identf[:]),
                      r=[hs, identf], w=[bank])
                bv = bank[:, :].rearrange("p (c t) -> p c t", c=4)
                gb_ = gcol2[:, 4 * q:4 * q + 4].unsqueeze(2).broadcast_to([128, 4, 128])
                V(k, lambda e: e.tensor_tensor(hnT[:, 4 * q:4 * q + 4, i * 128:(i + 1) * 128], bv, gb_, ALU.mult),
                  r=[bank, gcol2], w=[hnT])
                V(k, lambda e: e.tensor_tensor(h32[:, 4 * q:4 * q + 4, :], bv, gb_, ALU.mult), r=[bank, gcol2], w=[h32])
            bank = nbk()
            for c in range(8):
                M(k, lambda e, c=c: e.matmul(bank[:, 0:36], h32[:, c, :], wr32[:, c, :], start=(c == 0), stop=(c == 7)),
                  r=[h32, wr32], w=[bank])
            V(k, lambda e: e.tensor_tensor(LG[:, i, :], bank[:, 0:36], brow[:], ALU.add), r=[bank, brow], w=[LG])
        gitems.append((s1, s2))
    outproj(0)
    outproj(1)
    prevg = None
    for i in range(16):
        if i + 2 < 16:
            outproj(i + 2)
        gitems[i][0]()
        if prevg is not None:
            prevg()
        prevg = gitems[i][1]
    prevg()
    R = k.sb("R", [128, 16, 80], F32)
    T4 = k.sb("T4", [128, 16, 4, 8], F32)
    comb = k.sb("comb", [128, 16, 4, 8], F32)
    lg = LG[:, :, 0:4]
    le = LG[:, :, 4:36].rearrange("p t (g e) -> p t g e", g=4)
    gmax, gs, gw, v1, v2, e2, w1, w2 = (R[:, :, i] for i in range(8))
    oh = R[:, :, 8:12]
    ex = R[:, :, 12:16]
    sel = R[:, :, 16:24]
    m1 = R[:, :, 24:32]
    sel2 = R[:, :, 32:40]
    m2 = R[:, :, 40:48]
    wi = R[:, :, 48:56]
    wi2 = R[:, :, 56:64]

    def bc(ap, n):
        return ap.unsqueeze(2).broadcast_to([128, 16, n])

    def VR(fn):
        V(k, fn, r=[R, LG, T4], w=[R, T4])

    VR(lambda e: e.tensor_reduce(gmax, lg, AX.X, ALU.max))
    VR(lambda e: e.tensor_tensor(oh, lg, bc(gmax, 4), ALU.is_equal))
    VR(lambda e: e.tensor_tensor(ex, lg, bc(gmax, 4), ALU.subtract))
    A(k, lambda e: e.activation(ex, ex, AF.Exp), r=[R], w=[R])
    VR(lambda e: e.tensor_reduce(gs, ex, AX.X, ALU.add))
    VR(lambda e: e.reciprocal(gw, gs))
    VR(lambda e: e.tensor_tensor(T4[:], le, oh.unsqueeze(3).broadcast_to([128, 16, 4, 8]), ALU.mult))
    VR(lambda e: e.tensor_reduce(sel, T4[:].rearrange("p t g e -> p t e g"), AX.X, ALU.add))
    VR(lambda e: e.tensor_reduce(v1, sel, AX.X, ALU.max))
    VR(lambda e: e.tensor_tensor(m1, sel, bc(v1, 8), ALU.is_equal))
    VR(lambda e: e.scalar_tensor_tensor(sel2, m1, -1e30, sel, ALU.mult, ALU.add))
    VR(lambda e: e.tensor_reduce(v2, sel2, AX.X, ALU.max))
    VR(lambda e: e.tensor_tensor(m2, sel2, bc(v2, 8), ALU.is_equal))
    VR(lambda e: e.tensor_tensor(e2, v2, v1, ALU.subtract))
    A(k, lambda e: e.activation(e2, e2, AF.Exp), r=[R], w=[R])
    VR(lambda e: e.tensor_scalar(w2, e2, 1.0, None, ALU.add))
    VR(lambda e: e.reciprocal(w2, w2))
    VR(lambda e: e.tensor_tensor(w1, gw, w2, ALU.mult))
    VR(lambda e: e.tensor_tensor(w2, w1, e2, ALU.mult))
    VR(lambda e: e.tensor_tensor(wi, m1, bc(w1, 8), ALU.mult))
    VR(lambda e: e.tensor_tensor(wi2, m2, bc(w2, 8), ALU.mult))
    VR(lambda e: e.tensor_tensor(wi, wi, wi2, ALU.add))
    V(k, lambda e: e.tensor_tensor(comb[:], oh.unsqueeze(3).broadcast_to([128, 16, 4, 8]),
                                   wi.unsqueeze(2).broadcast_to([128, 16, 4, 8]), ALU.mult), r=[R], w=[comb])
    for i in range(16):
        bank = nbk()
        M(k, lambda e: e.transpose(bank[0:32, 0:128], comb[:, i, :, :].rearrange("p g e -> p (g e)"), identf[:]),
          r=[comb, identf], w=[bank])
        A(k, lambda e: e.activation(combT[0:32, i * 128:(i + 1) * 128], bank[0:32, 0:128], AF.Copy), r=[bank], w=[combT])
    if "comb" in dbg:
        o = dout("d_comb", [128, 16 * 32])
        k.dma('sp', o[:, :], comb[:].rearrange("p t g e -> p (t g e)"), reads=[comb])
    k.pop()
    if "h1" in dbg:
        o = dout("d_h1", [T_OWN, 1024])
        k.dma('sp', o.rearrange("(i p) n -> p i n", p=128), out_acc[:], reads=oacc)
    if upto in ("F", "G"):
        k.wait_all('sp', oacc)
        return finish()

    k.push()
    selall = k.sb("selall", [32, 32, 128], BF16)
    P(k, lambda e: e.memset(selall[:], 0.0), w=[selall])
    P(k, lambda e: e.affine_select(selall[:], selall[:], pattern=[[1, 32], [0, 128]], compare_op=ALU.not_equal,
                                   fill=1.0, base=0, channel_multiplier=-1), r=[selall], w=[selall])
    weg = [k.sb(f"weg{i}", [128, 8, 256], BF16) for i in range(3)]
    weu = [k.sb(f"weu{i}", [128, 8, 256], BF16) for i in range(3)]
    wed = [k.sb(f"wed{i}", [128, 2, 1024], BF16) for i in range(3)]

    def load_gu(ei):
        k.dma('pool', weg[ei % 3][:], w_eg[ei, :, :].rearrange("(c p) n -> p c n", p=128), writes=[weg[ei % 3]])
        k.dma('pool', weu[ei % 3][:], w_eu[ei, :, :].rearrange("(c p) n -> p c n", p=128), writes=[weu[ei % 3]])

    def load_d(ei):
        k.dma('pool', wed[ei % 3][:], w_ed[ei, :, :].rearrange("(c p) n -> p c n", p=128), writes=[wed[ei % 3]])

    load_gu(0)
    load_d(0)
    load_gu(1)
    load_d(1)
    sgs = [k.sb(f"sgs{i}", [128, 512], F32) for i in range(2)]
    cbs = [k.sb(f"cbs{i}", [128, 512], BF16) for i in range(2)]
    t1s = [k.sb(f"t1s{i}", [128, 512], BF16) for i in range(2)]
    hids = [k.sb(f"hid{i}", [128, 2, 512], BF16) for i in range(3)]
    evs = [k.sb(f"evs{i}", [128, 512], F32) for i in range(2)]
    pending = []
    nacc = [0]

    def emit_down(e_, tg, hid, w):
        for tt in range(4):
            ti = tg * 4 + tt
            for hf in range(2):
                bank = nbk()
                for f in range(2):
                    M(k, lambda e, f=f: e.matmul(bank[:, :], hid[:, f, tt * 128:(tt + 1) * 128], wed[w][:, f, hf * 512:(hf + 1) * 512],
                                                 start=(f == 0), stop=(f == 1)), r=[hid, wed[w]], w=[bank])
                dst = out_acc[:, ti, hf * 512:(hf + 1) * 512]
                if nacc[0] % 3 == 2:
                    ev = evs[(nacc[0] // 3) % 2]
                    A(k, lambda e: e.activation(ev[:], bank[:, :], AF.Copy), r=[bank], w=[ev])
                    P(k, lambda e: e.tensor_tensor(dst, dst, ev[:], ALU.add), r=[ev, oacc[ti]], w=[oacc[ti]])
                else:
                    V(k, lambda e: e.tensor_tensor(dst, bank[:, :], dst, ALU.add), r=[bank, oacc[ti]], w=[oacc[ti]])
                nacc[0] += 1
            if e_ == 31:
                k.dma('sp', out[ti * 128:(ti + 1) * 128, :], out_acc[:, ti, :], reads=[oacc[ti]])

    itn = 0
    for e_ in range(32):
        w = e_ % 3
        for tg in range(4):
            ts_ = slice(tg * 512, (tg + 1) * 512)
            hid = hids[itn % 3]
            cb = cbs[itn % 2]
            cbank = nbk()
            M(k, lambda e: e.matmul(cbank[:, :], selall[0:32, e_, :], combT[0:32, ts_], start=True, stop=True),
              r=[selall, combT], w=[cbank])
            A(k, lambda e: e.activation(cb[:], cbank[:, :], AF.Copy), r=[cbank], w=[cb])
            for f in range(2):
                gbank = nbk()
                for c in range(8):
                    M(k, lambda e, c=c: e.matmul(gbank[:, :], weg[w][:, c, f * 128:(f + 1) * 128], hnT[:, c, ts_],
                                                 start=(c == 0), stop=(c == 7)), r=[weg[w], hnT], w=[gbank])
                ubank = nbk()
                for c in range(8):
                    M(k, lambda e, c=c: e.matmul(ubank[:, :], weu[w][:, c, f * 128:(f + 1) * 128], hnT[:, c, ts_],
                                                 start=(c == 0), stop=(c == 7)), r=[weu[w], hnT], w=[ubank])
                sg = sgs[(itn * 2 + f) % 2]
                t1 = t1s[(itn * 2 + f) % 2]
                A(k, lambda e: e.activation(sg[:], gbank[:, :], AF.Silu), r=[gbank], w=[sg])
                V(k, lambda e: e.tensor_tensor(t1[:], ubank[:, :], sg[:], ALU.mult), r=[ubank, sg], w=[t1])
                P(k, lambda e: e.tensor_tensor(hid[:, f, :], t1[:], cb[:], ALU.mult), r=[t1, cb], w=[hid])
            if pending:
                emit_down(*pending.pop())
            pending.append((e_, tg, hid, w))
            if e_ + 2 < 32 and tg == 1:
                load_gu(e_ + 2)
            if e_ + 2 < 32 and tg == 2:
                load_d(e_ + 2)
            itn += 1
    emit_down(*pending.pop())
    k.wait_all('sp', oacc)
    k.pop()
    k.pop()
    return finish()


def make_in_maps(inputs):
    f = lambda a: np.ascontiguousarray(np.asarray(a, dtype=np.float32))
    x = f(inputs["x"])
    mem = f(inputs["mem"])
    shared = {
        "ln1_g": f(inputs["ln1_g"][0]), "w_in": f(inputs["w_in"][0]), "m_conv": f(inputs["m_conv"][0]),
        "m_i_b": f(inputs["m_i_b"][0]), "m_f_b": f(inputs["m_f_b"][0]), "m_norm_g": f(inputs["m_norm_g"][0]).reshape(512),
        "dil_q_g": f(inputs["dil_q_g"][0]), "dil_k_g": f(inputs["dil_k_g"][0]), "mem_norm_g": f(inputs["mem_norm_g"][0]),
        "w_mem_kv": f(inputs["w_mem_kv"][0]), "x_q_g": f(inputs["x_q_g"][0]), "x_k_g": f(inputs["x_k_g"][0]),
        "w_a": f(inputs["w_a"][0]), "w_b": f(inputs["w_b"][0]), "w_c": f(inputs["w_c"][0]),
        "w_gate": f(inputs["w_gate"][0]), "b_gate": f(inputs["b_gate"][0]), "w_out": f(inputs["w_out"][0]),
        "ln2_g": f(inputs["ln2_g"][0]), "w_grp": f(inputs["w_grp"][0]), "b_grp": f(inputs["b_grp"][0]),
        "w_er": f(inputs["w_er"][0]), "b_er": f(inputs["b_er"][0]).reshape(32),
        "w_eg": f(inputs["w_eg"][0]).reshape(32, 1024, 256), "w_eu": f(inputs["w_eu"][0]).reshape(32, 1024, 256),
        "w_ed": f(inputs["w_ed"][0]).reshape(32, 256, 1024),
    }
    s, t = np.meshgrid(np.arange(128), np.arange(128), indexing="ij")
    tri_prev = np.where(s >= t, 0.0, NEG).astype(np.float32)
    in_maps = []
    for core in range(8):
        b, half = core // 2, core % 2
        m = dict(shared)
        m["x_own"] = np.ascontiguousarray(x[b, half * T_OWN:(half + 1) * T_OWN])
        m["mem"] = np.ascontiguousarray(mem[b])
        pm = np.ones((4, T_ALL), np.float32)
        pa = np.zeros((4, T_ALL), np.float32)
        if half == 0:
            m["x_pre"] = np.zeros((T_OWN, 1024), np.float32)
            pm[:, :T_OWN] = 0.0
            pa[:, :T_OWN] = NEG
            m["mpfx"] = np.full((128, 128), NEG, np.float32)
        else:
            m["x_pre"] = np.ascontiguousarray(x[b, 0:T_OWN])
            m["mpfx"] = tri_prev
        m["pmul"] = pm
        m["padd"] = pa
        in_maps.append(m)
    return in_maps


_CACHE = {}


def kernel(**inputs):
    if "nc" not in _CACHE:
        _CACHE["nc"] = build_program()[0]
    nc = _CACHE["nc"]
    in_maps = make_in_maps(inputs)
    res = run_bass_kernel_spmd(nc, in_maps, core_ids=list(range(8)))
    outp = np.zeros((4, 4096, 1024), np.float32)
    for core in range(8):
        b, half = core // 2, core % 2
        outp[b, half * T_OWN:(half + 1) * T_OWN] = res.results[core]["out"]
    return outp
```

```python
import contextlib
import numpy as np
import concourse.bass as bass
import concourse.mybir as mybir
from concourse.bass_utils import run_bass_kernel_spmd

F32 = mybir.dt.float32
BF16 = mybir.dt.bfloat16
AF = mybir.ActivationFunctionType
ALU = mybir.AluOpType
AX = mybir.AxisListType

NEG = -30000.0
EPS = 1e-6
T_OWN = 2048
T_ALL = 4096


class Buf:
    def __init__(self, name, t, excl=False):
        self.name = name
        self.t = t
        self.w = None
        self.r = {}
        self.excl = excl
        self.ds = None
        self.ds_sw = None

    def __getitem__(self, key):
        return self.t[key]


class DSem:
    def __init__(self, h):
        self.h = h
        self.cnt = 0


class KB:
    EPOCH = 30000

    def __init__(self, nc):
        self.nc = nc
        self.root = contextlib.ExitStack()
        self.es = self.root
        self.stack = []
        self.eng = {'pe': nc.tensor, 'act': nc.scalar, 'dve': nc.vector,
                    'pool': nc.gpsimd, 'sp': nc.sync}
        self.sem = {}
        self.cnt = {}
        self.nsem = 0
        self.seen = {e: {} for e in self.eng}
        self.ninst = {e: 0 for e in self.eng}
        self.nwait = {e: 0 for e in self.eng}
        self.dfree = []
        self.dfree_sw = []
        self.scope_bufs = [[]]
        self.used = 16512
        self.peak = 0
        self.limit = 229344
        self.used_stack = []
        for e in ('pe', 'act', 'dve', 'pool'):
            self._newsem(e)

    def _alloc_sem(self, name):
        self.nsem += 1
        return self.root.enter_context(self.nc.semaphore(f"{name}_{self.nsem}"))

    def _newsem(self, e):
        self.sem[e] = self._alloc_sem("s" + e)
        self.cnt[e] = 0

    def _dsem(self, b, sw=False):
        if sw:
            if b.ds_sw is None:
                b.ds_sw = self.dfree_sw.pop() if self.dfree_sw else DSem(self._alloc_sem("w"))
            return b.ds_sw
        if b.ds is None:
            b.ds = self.dfree.pop() if self.dfree else DSem(self._alloc_sem("d"))
        return b.ds

    def sb(self, name, shape, dtype):
        t = self.es.enter_context(self.nc.sbuf_tensor(name, list(shape), dtype))
        nbytes = int(np.prod(shape[1:])) * (2 if dtype == BF16 else 4)
        self.used += (nbytes + 31) // 32 * 32
        assert self.used <= self.limit, (name, self.used, self.limit)
        self.peak = max(self.peak, self.used)
        b = Buf(name, t)
        self.scope_bufs[-1].append(b)
        return b

    def sb_at(self, name, shape, dtype, offset):
        t = self.nc.alloc_sbuf_tensor_at(name, list(shape), dtype, offset=offset)
        return Buf(name, t)

    def ps(self, name, shape, dtype=F32):
        t = self.es.enter_context(self.nc.psum_tensor(name, list(shape), dtype))
        return Buf(name, t, excl=True)

    def view(self, b, name=None):
        nb = Buf(name or b.name, b.t, b.excl)
        self.scope_bufs[-1].append(nb)
        return nb

    def push(self):
        self.stack.append(self.es)
        self.es = contextlib.ExitStack()
        self.scope_bufs.append([])
        self.used_stack.append(self.used)

    def pop(self):
        bufs = self.scope_bufs.pop()
        for e in self.eng:
            self.wait_all(e, bufs)
        self.barrier()
        for b in bufs:
            if b.ds is not None:
                self.dfree.append(b.ds)
                b.ds = None
            if b.ds_sw is not None:
                self.dfree_sw.append(b.ds_sw)
                b.ds_sw = None
        self.es.close()
        self.es = self.stack.pop()
        self.used = self.used_stack.pop()

    def barrier(self):
        for e in self.eng:
            for e2 in ('pe', 'act', 'dve', 'pool'):
                if e2 != e and self.cnt[e2] > 0:
                    self._wait(e, (self.sem[e2], self.cnt[e2], e2))

    def _wait(self, e, ev):
        sem, val, eng = ev
        key = sem.name
        if self.seen[e].get(key, 0) >= val:
            return
        self.eng[e].wait_ge(sem, val)
        self.seen[e][key] = val
        self.nwait[e] += 1

    STRICT = True

    def _deps(self, e, reads, writes):
        same_ok = (e == 'pe') or not self.STRICT
        for b in reads:
            if b.w is not None:
                if not (b.w[2] == e and e == 'pe'):
                    self._wait(e, b.w)
            if b.excl:
                for ev in list(b.r.values()):
                    if ev[2] != e:
                        self._wait(e, ev)
        for b in writes:
            if b.w is not None and (b.w[2] != e or not same_ok):
                self._wait(e, b.w)
            for ev in list(b.r.values()):
                if ev[2] != e or not same_ok:
                    self._wait(e, ev)

    def _record(self, ev, reads, writes):
        for b in reads:
            old = b.r.get(ev[0].name)
            if old is None or old[1] < ev[1]:
                b.r[ev[0].name] = ev
        for b in writes:
            b.w = ev
            b.r = {}

    def op(self, e, fn, reads=(), writes=()):
        self._deps(e, reads, writes)
        ins = fn(self.eng[e])
        if self.cnt[e] >= self.EPOCH:
            self._newsem(e)
        self.cnt[e] += 1
        ins.then_inc(self.sem[e], 1)
        ev = (self.sem[e], self.cnt[e], e)
        self._record(ev, reads, writes)
        self.ninst[e] += 1
        return ev

    def dma(self, q, out, in_, reads=(), writes=(), sembuf=None, **kw):
        self._deps(q, reads, writes)
        ins = self.eng[q].dma_start(out=out, in_=in_, **kw)
        b = sembuf if sembuf is not None else (writes[0] if writes else reads[0])
        ds = self._dsem(b, sw=(q == 'pool'))
        ds.cnt += 16
        ins.then_inc(ds.h, 16)
        ev = (ds.h, ds.cnt, 'dma')
        self._record(ev, reads, writes)
        self.ninst[q] += 1
        return ev

    def wait_all(self, e, bufs):
        for b in bufs:
            if b.w is not None:
                self._wait(e, b.w)
            for ev in list(b.r.values()):
                self._wait(e, ev)

    def close(self):
        self.root.close()


def V(k, fn, r=(), w=()):
    return k.op('dve', fn, r, w)


def A(k, fn, r=(), w=()):
    return k.op('act', fn, r, w)


def P(k, fn, r=(), w=()):
    return k.op('pool', fn, r, w)


def M(k, fn, r=(), w=()):
    return k.op('pe', fn, r, w)


def pipe2(items):
    prev = None
    for s1, s2 in items:
        s1()
        if prev is not None:
            prev()
        prev = s2
    if prev is not None:
        prev()


def pipeN(items):
    if not items:
        return
    ns = len(items[0])
    for t in range(len(items) + ns - 1):
        for kk in range(ns):
            i = t - kk
            if 0 <= i < len(items):
                items[i][kk]()


O_MQ, O_MK, O_MV, O_MO, O_MI, O_MF, O_DQ, O_DK, O_DV, O_XQ = 0, 512, 1024, 1536, 2048, 2052, 2056, 2824, 3592, 4360
IN_TOTAL = 4872


def build_program(upto="all", dbg=()):
    nc = bass.Bass("TRN2", target_bir_lowering=False)

    def din(name, shape):
        return nc.dram_tensor(name, list(shape), F32, kind="ExternalInput").ap()

    x_own = din("x_own", [T_OWN, 1024])
    x_pre = din("x_pre", [T_OWN, 1024])
    mem = din("mem", [256, 1024])
    ln1_g = din("ln1_g", [1024])
    w_in = din("w_in", [1024, IN_TOTAL])
    m_conv = din("m_conv", [4, 1024])
    m_i_b = din("m_i_b", [4])
    m_f_b = din("m_f_b", [4])
    m_norm_g = din("m_norm_g", [512])
    dil_q_g = din("dil_q_g", [3, 64])
    dil_k_g = din("dil_k_g", [3, 64])
    mem_norm_g = din("mem_norm_g", [1024])
    w_mem_kv = din("w_mem_kv", [1024, 1024])
    x_q_g = din("x_q_g", [128])
    x_k_g = din("x_k_g", [128])
    w_a = din("w_a", [512, 1024])
    w_b = din("w_b", [256, 1024])
    w_c = din("w_c", [512, 1024])
    w_gate = din("w_gate", [1024, 3072])
    b_gate = din("b_gate", [3072])
    w_out = din("w_out", [1024, 1024])
    ln2_g = din("ln2_g", [1024])
    w_grp = din("w_grp", [1024, 4])
    b_grp = din("b_grp", [4])
    w_er = din("w_er", [4, 1024, 8])
    b_er = din("b_er", [32])
    w_eg = din("w_eg", [32, 1024, 256])
    w_eu = din("w_eu", [32, 1024, 256])
    w_ed = din("w_ed", [32, 256, 1024])
    pmul = din("pmul", [4, T_ALL])
    padd = din("padd", [4, T_ALL])
    mpfx = din("mpfx", [128, 128])
    out = nc.dram_tensor("out", [T_OWN, 1024], F32, kind="ExternalOutput").ap()
    dbg_out = {}

    def dout(name, shape):
        dbg_out[name] = nc.dram_tensor(name, list(shape), F32, kind="ExternalOutput").ap()
        return dbg_out[name]

    k = KB(nc)
    pb = [k.ps(f"pb{i}", [128, 512], F32) for i in range(8)]
    pbb = [b.t.bitcast(BF16) for b in pb]

    identf = k.sb("identf", [128, 128], F32)
    ident = k.sb("ident", [128, 128], BF16)
    P(k, lambda e: e.memset(identf[:], 0.0), w=[identf])
    P(k, lambda e: e.affine_select(identf[:], identf[:], pattern=[[-1, 128]], compare_op=ALU.not_equal,
                                   fill=1.0, base=0, channel_multiplier=1), r=[identf], w=[identf])
    V(k, lambda e: e.tensor_copy(ident[:], identf[:]), r=[identf], w=[ident])
    mtmp = k.sb("mtmp", [128, 128], F32)
    mask_cur = k.sb("mask_cur", [128, 128], BF16)
    mask_prev = k.sb("mask_prev", [128, 128], BF16)
    mask_pfx = k.sb("mask_pfx", [128, 128], BF16)
    P(k, lambda e: e.memset(mtmp[:], 0.0), w=[mtmp])
    P(k, lambda e: e.affine_select(mtmp[:], mtmp[:], pattern=[[1, 128]], compare_op=ALU.is_ge,
                                   fill=NEG, base=0, channel_multiplier=-1), r=[mtmp], w=[mtmp])
    V(k, lambda e: e.tensor_copy(mask_cur[:], mtmp[:]), r=[mtmp], w=[mask_cur])
    P(k, lambda e: e.memset(mtmp[:], 0.0), r=[mtmp], w=[mtmp])
    P(k, lambda e: e.affine_select(mtmp[:], mtmp[:], pattern=[[-1, 128]], compare_op=ALU.is_ge,
                                   fill=NEG, base=0, channel_multiplier=1), r=[mtmp], w=[mtmp])
    V(k, lambda e: e.tensor_copy(mask_prev[:], mtmp[:]), r=[mtmp], w=[mask_prev])
    k.dma('pool', mask_pfx[:], mpfx[:, :], writes=[mask_pfx])
    ones_bf = k.sb("ones_bf", [128, 128], BF16)
    P(k, lambda e: e.memset(ones_bf[:], 1.0), w=[ones_bf])
    blk64 = k.sb("blk64", [128, 128], BF16)
    P(k, lambda e: e.memset(blk64[:], 0.0), w=[blk64])
    P(k, lambda e: e.memset(blk64[0:64, 0:64], 1.0), w=[blk64])
    P(k, lambda e: e.memset(blk64[64:128, 64:128], 1.0), w=[blk64])
    gcol1 = k.sb("gcol1", [128, 8], F32)
    k.dma('sp', gcol1[:], ln1_g.rearrange("(c p) -> p c", p=128), writes=[gcol1], allow_slow_non_contiguous=True)
    mhalf = k.sb("mhalf", [128, 1], F32)
    P(k, lambda e: e.memset(mhalf[:], -0.5), w=[mhalf])

    k.push()
    xnT = k.sb("xnT", [128, 8, T_ALL], BF16)
    hAT = k.sb("hAT", [128, 4, T_OWN], BF16)

    k.push()
    wm = k.sb("wm", [128, 8, 2048], BF16)

    def norm_T(groups, dstT, gcol, tag):
        k.push()
        ntmax = max(nt for _, nt in groups)
        xgs = [k.sb(f"xg{tag}{i}", [128, ntmax, 1024], F32) for i in range(3)]
        xss = [k.sb(f"xs{tag}{i}", [128, 1024], BF16) for i in range(2)]
        junk = k.sb(f"junk{tag}", [128, 1024], BF16)
        items = []
        i = 0
        for gi, (src, nt) in enumerate(groups):
            xg = xgs[gi % 3]
            qn = 'sp' if gi % 2 == 0 else 'act'
            for t in range(nt):
                xs = xss[i % 2]
                bank = 6 + (i % 2)
                ss = k.sb(f"ss{tag}{i}", [128, 1], F32)
                rs = k.sb(f"rs{tag}{i}", [128, 1], F32)

                def s1(xg=xg, t=t, ss=ss, rs=rs, src=src, qn=qn, nt=nt):
                    if t == 0:
                        k.dma(qn, xg[:, 0:nt, :], src, writes=[xg])
                    A(k, lambda e: e.activation(junk[:], xg[:, t, :], AF.Square, accum_out=ss[:]), r=[xg], w=[junk, ss])
                    V(k, lambda e: e.tensor_scalar(ss[:], ss[:], 1.0 / 1024, EPS, ALU.mult, ALU.add), r=[ss], w=[ss])
                    P(k, lambda e: e.tensor_tensor(rs[:], ss[:], mhalf[:], ALU.pow), r=[ss, mhalf], w=[rs])

                def s2(xg=xg, t=t, xs=xs, rs=rs, bank=bank, i=i):
                    A(k, lambda e: e.activation(xs[:], xg[:, t, :], AF.Copy, scale=rs[:, 0:1]), r=[xg, rs], w=[xs])
                    for c in range(8):
                        M(k, lambda e, c=c: e.transpose(pbb[bank][:, c * 128:(c + 1) * 128], xs[:, c * 128:(c + 1) * 128], ident[:]),
                          r=[xs, ident], w=[pb[bank]])
                    V(k, lambda e: e.tensor_tensor(dstT[:, :, i * 128:(i + 1) * 128],
                                                   pbb[bank][:, :].rearrange("p (c t) -> p c t", c=8),
                                                   gcol[:].unsqueeze(2).broadcast_to([128, 8, 128]), ALU.mult),
                      r=[pb[bank], gcol], w=[dstT])
                items.append((s1, s2))
                i += 1
        pipe2(items)
        k.pop()

    groups = [(xsrc[g4 * 512:(g4 + 1) * 512, :].rearrange("(t p) n -> p t n", p=128), 4)
              for xsrc in (x_pre, x_own) for g4 in range(4)]
    norm_T(groups, xnT, gcol1, "a")

    if "xnT" in dbg:
        o = dout("d_xnT", [128, 8, T_ALL])
        k.push()
        t32 = k.sb("dbg32", [128, 8, T_ALL // 2], F32)
        for hh in range(2):
            V(k, lambda e: e.tensor_copy(t32[:], xnT[:, :, hh * 2048:(hh + 1) * 2048]), r=[xnT], w=[t32])
            k.dma('sp', o[:, :, hh * 2048:(hh + 1) * 2048], t32[:], reads=[t32])
        k.pop()

    def finish():
        while k.stack:
            k.pop()
        for e in k.eng:
            k.wait_all(e, k.scope_bufs[0])
        k.barrier()
        k.close()
        return nc, dbg_out, k

    if upto == "A":
        return finish()

    COLS = k.sb("COLS", [128, 32, 3, 4], F32)
    MUB = k.sb("MUB", [128, 33, 4], F32)
    W_end = k.sb("W_end", [128, 32, 4], F32)
    DECAY = k.sb("DECAY", [128, 32, 4], F32)
    EA = k.sb("EA", [128, 32, 4], F32)
    EAb = k.sb("EAb", [128, 32, 4], BF16)
    FLOOR2 = k.sb("FLOOR2", [128, 32, 4], F32)
    k.push()
    RT = k.sb("RT", [128, T_ALL], F32)
    PMA = k.sb("PMA", [128, T_ALL], F32)
    wg = k.sb("wg", [128, 8, 8], BF16)
    gb = k.sb("gb", [128, 2], F32)
    tmpg = k.sb("tmpg", [128, 2, 512], F32)
    sel127 = k.sb("sel127", [128, 128], F32)
    k.dma('pool', wg[:], w_in[:, O_MI:O_MI + 8].rearrange("(c p) n -> p c n", p=128), writes=[wg])
    for c in range(8):
        k.dma('pool', wm[:, c, :], w_in[c * 128:(c + 1) * 128, 0:2048], writes=[wm])
    k.dma('sp', gb[0:4, 0:1], m_i_b.rearrange("(p o) -> p o", o=1), writes=[gb])
    k.dma('sp', gb[64:68, 1:2], m_f_b.rearrange("(p o) -> p o", o=1), writes=[gb])
    k.dma('sp', RT[32:36, :], padd[:, :], writes=[RT])
    k.dma('sp', PMA[64:68, :], pmul[:, :], writes=[PMA])
    V(k, lambda e: e.tensor_scalar(gb[64:68, 1:2], gb[64:68, 1:2], -1.0, None, ALU.mult), r=[gb], w=[gb])
    P(k, lambda e: e.memset(sel127[:], 0.0), w=[sel127])
    P(k, lambda e: e.memset(sel127[96:128, :], 1.0), w=[sel127])
    P(k, lambda e: e.affine_select(sel127[96:128, :], sel127[96:128, :], pattern=[[0, 128]], compare_op=ALU.is_ge,
                                   fill=0.0, base=-31, channel_multiplier=1), r=[sel127], w=[sel127])
    for tg in range(8):
        cs = slice(tg * 512, (tg + 1) * 512)
        bi, bf = pb[(2 * tg) % 6], pb[(2 * tg + 1) % 6]
        for c in range(8):
            M(k, lambda e, c=c: e.matmul(bi[0:4, :], wg[:, c, 0:4], xnT[:, c, cs], start=(c == 0), stop=(c == 7)),
              r=[wg, xnT], w=[bi])
        for c in range(8):
            M(k, lambda e, c=c: e.matmul(bf[64:68, :], wg[:, c, 4:8], xnT[:, c, cs], start=(c == 0), stop=(c == 7),
                                         tile_position=(0, 64)), r=[wg, xnT], w=[bf])
        V(k, lambda e: e.tensor_scalar(RT[0:4, cs], bi[0:4, :], gb[0:4, 0:1], None, ALU.add), r=[bi, gb], w=[RT])
        A(k, lambda e: e.activation(tmpg[64:68, 0, :], bf[64:68, :], AF.Exp, bias=gb[64:68, 1:2], scale=-1.0),
          r=[bf, gb], w=[tmpg])
        A(k, lambda e: e.activation(tmpg[64:68, 1, :], tmpg[64:68, 0, :], AF.Ln, bias=1.0), r=[tmpg], w=[tmpg])
        V(k, lambda e: e.scalar_tensor_tensor(RT[64:68, cs], tmpg[64:68, 1, :], -0.5, PMA[64:68, cs], ALU.mult, ALU.mult),
          r=[tmpg, PMA], w=[RT])
    V(k, lambda e: e.tensor_tensor_scan(PMA[0:4, :], RT[64:68, :], RT[64:68, :], 0.0, ALU.add, ALU.add),
      r=[RT, PMA], w=[PMA])
    V(k, lambda e: e.tensor_tensor(PMA[32:36, :], RT[0:4, :], PMA[0:4, :], ALU.subtract), r=[RT, PMA], w=[PMA])
    V(k, lambda e: e.tensor_tensor(RT[0:4, :], PMA[32:36, :], RT[32:36, :], ALU.add), r=[RT, PMA], w=[RT])
    V(k, lambda e: e.tensor_tensor_scan(RT[32:36, :], RT[0:4, :], RT[0:4, :], 0.0, ALU.max, ALU.max),
      r=[RT], w=[RT])
    V(k, lambda e: e.tensor_copy(RT[64:68, :], PMA[0:4, :]), r=[PMA, RT], w=[RT])
    for q in range(8):
        bank = pb[q % 4]
        for j in range(4):
            c = q * 4 + j
            M(k, lambda e, c=c, j=j: e.transpose(bank[:, j * 128:(j + 1) * 128], RT[:, c * 128:(c + 1) * 128], identf[:]),
              r=[RT, identf], w=[bank])
        V(k, lambda e: e.tensor_copy(COLS[:, q * 4:(q + 1) * 4, :, :],
                                     bank[:, :].rearrange("p (c q r) -> p c q r", c=4, q=4)[:, :, 0:3, 0:4]),
          r=[bank], w=[COLS])
    M(k, lambda e: e.matmul(pb[4][:, 0:384], sel127[:], COLS[:].rearrange("p c q r -> p (c q r)"), start=True, stop=True),
      r=[sel127, COLS], w=[pb[4]])
    P(k, lambda e: e.memset(MUB[:, 0, :], 0.0), w=[MUB])
    V(k, lambda e: e.tensor_copy(MUB[:, 1:33, :], pb[4][:, 0:384].rearrange("p (c q r) -> p c q r", c=32, q=3)[:, :, 1, :]),
      r=[pb[4]], w=[MUB])
    gt = k.sb("gt", [128, 4, 32, 4], F32)
    V(k, lambda e: e.tensor_tensor(gt[:, 0], COLS[:, :, 0, :], MUB[:, 0:32, :], ALU.subtract), r=[MUB, COLS], w=[gt])
    V(k, lambda e: e.tensor_tensor(gt[:, 1], COLS[:, :, 0, :], MUB[:, 1:33, :], ALU.subtract), r=[MUB, COLS], w=[gt])
    V(k, lambda e: e.tensor_tensor(gt[:, 2], MUB[:, 0:32, :], MUB[:, 1:33, :], ALU.subtract), r=[MUB], w=[gt])
    V(k, lambda e: e.tensor_tensor(gt[:, 3], COLS[:, :, 2, :], MUB[:, 0:32, :], ALU.add), r=[COLS, MUB], w=[gt])
    A(k, lambda e: e.activation(EA[:], gt[:, 0], AF.Exp), r=[gt], w=[EA])
    A(k, lambda e: e.activation(W_end[:], gt[:, 1], AF.Exp), r=[gt], w=[W_end])
    A(k, lambda e: e.activation(DECAY[:], gt[:, 2], AF.Exp), r=[gt], w=[DECAY])
    A(k, lambda e: e.activation(FLOOR2[:], gt[:, 3], AF.Exp, scale=-1.0), r=[gt], w=[FLOOR2])
    V(k, lambda e: e.tensor_scalar(W_end[:], W_end[:], 128.0 ** -0.5, None, ALU.mult), r=[W_end], w=[W_end])
    V(k, lambda e: e.tensor_copy(EAb[:], EA[:]), r=[EA], w=[EAb])
    k.pop()

    if "gates" in dbg:
        o = dout("d_cols", [128, 32 * 12])
        k.dma('sp', o[:, :], COLS[:].rearrange("p c q r -> p (c q r)"), reads=[COLS])
        o = dout("d_mub", [128, 33 * 4])
        k.dma('sp', o[:, :], MUB[:].rearrange("p c r -> p (c r)"), reads=[MUB])
        o = dout("d_wend", [128, 32 * 4])
        k.dma('sp', o[:, :], W_end[:].rearrange("p c r -> p (c r)"), reads=[W_end])
    if upto == "B":
        return finish()

    k.push()
    cw = k.sb("cw", [128, 4, 8], F32)
    for tap in range(4):
        k.dma('sp', cw[:, tap, :], m_conv[tap, :].rearrange("(j p) -> p j", p=128), writes=[cw],
              allow_slow_non_contiguous=True)
    dg = k.sb("dg", [128, 8, 4, 128], BF16)
    for j in range(8):
        for tap in range(4):
            V(k, lambda e, j=j, tap=tap: e.tensor_scalar(dg[:, j, tap, :], ident[:], cw[:, tap, j:j + 1], None, ALU.mult),
              r=[ident, cw], w=[dg])
    mask01 = k.sb("mask01", [128, 128], BF16)
    P(k, lambda e: e.memset(mtmp[:], 128.0 ** -0.5), r=[mtmp], w=[mtmp])
    P(k, lambda e: e.affine_select(mtmp[:], mtmp[:], pattern=[[1, 128]], compare_op=ALU.is_ge,
                                   fill=0.0, base=0, channel_multiplier=-1), r=[mtmp], w=[mtmp])
    V(k, lambda e: e.tensor_copy(mask01[:], mtmp[:]), r=[mtmp], w=[mask01])
    mng = k.sb("mng", [128, 512], F32)
    k.dma('sp', mng[:], m_norm_g.partition_broadcast(128), writes=[mng])
    zbs = [k.sb(f"zb{i}", [128, 515], BF16) for i in range(3)]
    carry = k.sb("carry", [128, 8, 3], BF16)
    P(k, lambda e: e.memset(carry[:], 0.0), w=[carry])
    qkT = [k.sb(f"qkT{i}", [128, 8, 512], BF16) for i in range(2)]
    vaug = [k.sb(f"vaug{i}", [128, 4, 128], BF16) for i in range(3)]
    vsc = [k.sb(f"vsc{i}", [128, 4, 128], BF16) for i in range(3)]
    gmo = [k.sb(f"gmo{i}", [128, 512], BF16) for i in range(3)]
    sgt = [k.sb(f"sgt{i}", [128, 512], F32) for i in range(2)]
    kw = [k.sb(f"kw{i}", [128, 4, 128], BF16) for i in range(3)]
    Cst = k.sb("Cst", [128, 4, 128], F32)
    nst = k.sb("nst", [128, 4], F32)
    Cbs = [k.sb(f"Cb{i}", [128, 4, 128], BF16) for i in range(2)]
    nbs = [k.sb(f"nb{i}", [128, 4], BF16) for i in range(2)]
    P(k, lambda e: e.memset(Cst[:], 0.0), w=[Cst])
    P(k, lambda e: e.memset(nst[:], 0.0), w=[nst])
    for i in range(2):
        P(k, lambda e, i=i: e.memset(Cbs[i][:], 0.0), w=[Cbs[i]])
        P(k, lambda e, i=i: e.memset(nbs[i][:], 0.0), w=[nbs[i]])
    DT = [k.sb(f"DT{i}", [128, 4, 128], BF16) for i in range(3)]
    hns = [k.sb(f"hns{i}", [128, 4, 128], F32) for i in range(2)]
    hsq = [k.sb(f"hsq{i}", [128, 4, 128], BF16) for i in range(2)]
    hA = [k.sb(f"hA{i}", [128, 4, 128], BF16) for i in range(2)]
    rr = 0
    Sb, ABb, XB_ = [pb[3], pb[4]], [pb[5], pb[6]], pb[7]

    def gbank():
        nonlocal rr
        b = rr % 3
        rr += 1
        return b

    nz = [0]

    def supertile(st):
        own = st >= 4
        cs = slice(st * 512, (st + 1) * 512)
        qk = qkT[st % 2]
        jlist = list(range(8)) if st >= 3 else [4, 5, 6, 7]
        items = []
        for j in jlist:
            zb = zbs[nz[0] % 3]
            nz[0] += 1

            def s1(j=j, zb=zb):
                b = gbank()
                for c in range(8):
                    M(k, lambda e, c=c: e.matmul(pb[b][:, :], wm[:, c, j * 128:(j + 1) * 128], xnT[:, c, cs],
                                                 start=(c == 0), stop=(c == 7)), r=[wm, xnT], w=[pb[b]])
                P(k, lambda e: e.tensor_copy(zb[:, 0:3], carry[:, j, :]), r=[carry], w=[zb])
                A(k, lambda e: e.activation(zb[:, 3:515], pb[b][:, :], AF.Copy), r=[pb[b]], w=[zb])
                P(k, lambda e: e.tensor_copy(carry[:, j, :], zb[:, 512:515]), r=[zb], w=[carry])

            def s2(j=j, zb=zb):
                if not (own or j >= 4):
                    return
                b2 = gbank()
                for tap in range(4):
                    M(k, lambda e, tap=tap: e.matmul(pb[b2][:, :], dg[:, j, tap, :], zb[:, tap:tap + 512],
                                                     start=(tap == 0), stop=(tap == 3)), r=[dg, zb], w=[pb[b2]])
                A(k, lambda e: e.activation(qk[:, j, :], pb[b2][:, :], AF.Silu), r=[pb[b2]], w=[qk])
            items.append((s1, s2))
        pipe2(items)

    def stageA(c):
        st, ci = c // 4, c % 4
        own = st >= 4
        qk = qkT[st % 2]
        tsl = slice(c * 128, (c + 1) * 128)
        lsl = slice(ci * 128, (ci + 1) * 128)
        va, vs_, kwt = vaug[c % 3], vsc[c % 3], kw[c % 3]
        b = gbank()
        for kk in range(8):
            M(k, lambda e, kk=kk: e.matmul(pb[b][:, :], xnT[:, kk, tsl], wm[:, kk, O_MV:O_MV + 512],
                                           start=(kk == 0), stop=(kk == 7)), r=[wm, xnT], w=[pb[b]])
        A(k, lambda e: e.activation(va[:].rearrange("p h d -> p (h d)"), pb[b][:, :], AF.Copy), r=[pb[b]], w=[va])
        if own:
            V(k, lambda e: e.tensor_tensor(vs_[:], pb[b][:, :].rearrange("p (h d) -> p h d", h=4),
                                           EA[:, c, :].unsqueeze(2).broadcast_to([128, 4, 128]), ALU.mult),
              r=[pb[b], EA], w=[vs_])
        b = gbank()
        for h in range(4):
            M(k, lambda e, h=h: e.transpose(pbb[b][:, h * 128:(h + 1) * 128], qk[:, 4 + h, lsl], ident[:]),
              r=[qk, ident], w=[pb[b]])
        V(k, lambda e: e.tensor_tensor(kwt[:], pbb[b][:, 0:512].rearrange("p (h d) -> p h d", h=4),
                                       W_end[:, c, :].unsqueeze(2).broadcast_to([128, 4, 128]), ALU.mult),
          r=[pb[b], W_end], w=[kwt])
        if own:
            g_, sg = gmo[c % 3], sgt[c % 2]
            b = gbank()
            for kk in range(8):
                M(k, lambda e, kk=kk: e.matmul(pb[b][:, :], xnT[:, kk, tsl], wm[:, kk, O_MO:O_MO + 512],
                                               start=(kk == 0), stop=(kk == 7)), r=[wm, xnT], w=[pb[b]])
            A(k, lambda e: e.activation(sg[:], pb[b][:, :], AF.Sigmoid), r=[pb[b]], w=[sg])
            P(k, lambda e: e.tensor_tensor(g_[:], sg[:], mng[:], ALU.mult), r=[sg, mng], w=[g_])
            S_ = Sb[c % 2]
            for h in range(4):
                M(k, lambda e, h=h: e.matmul(S_[:, h * 128:(h + 1) * 128], qk[:, 4 + h, lsl], qk[:, h, lsl],
                                             start=True, stop=True), r=[qk], w=[S_])
            dt = DT[c % 3]
            V(k, lambda e: e.tensor_tensor(dt[:], S_[:, :].rearrange("p (h d) -> p h d", h=4),
                                           mask01[:].unsqueeze(1).broadcast_to([128, 4, 128]), ALU.mult),
              r=[S_, mask01], w=[dt])

    def stageB(c):
        st, ci = c // 4, c % 4
        own = st >= 4
        qk = qkT[st % 2]
        lsl = slice(ci * 128, (ci + 1) * 128)
        va, vs_, kwt = vaug[c % 3], vsc[c % 3], kw[c % 3]
        Cb, nb_ = Cbs[c % 2], nbs[c % 2]
        b = gbank()
        for h in range(4):
            M(k, lambda e, h=h: e.matmul(pb[b][:, h * 128:(h + 1) * 128], kwt[:, h, :], va[:, h, :],
                                         start=True, stop=True), r=[kwt, va], w=[pb[b]])
        for h in range(4):
            M(k, lambda e, h=h: e.matmul(XB_[:, 8 + h:9 + h], kwt[:, h, :], ones_bf[:, 0:1],
                                         start=True, stop=True), r=[kwt, ones_bf], w=[XB_])
        if own:
            dt = DT[c % 3]
            AB_ = ABb[c % 2]
            xo = (c % 2) * 4
            for h in range(4):
                M(k, lambda e, h=h: e.matmul(AB_[:, h * 128:(h + 1) * 128], dt[:, h, :], vs_[:, h, :],
                                             start=True, stop=False), r=[dt, vs_], w=[AB_])
                M(k, lambda e, h=h: e.matmul(AB_[:, h * 128:(h + 1) * 128], qk[:, h, lsl], Cb[:, h, :],
                                             start=False, stop=True), r=[qk, Cb], w=[AB_])
            for h in range(4):
                M(k, lambda e, h=h: e.matmul(XB_[:, xo + h:xo + h + 1], dt[:, h, :], EAb[:, c, h:h + 1],
                                             start=True, stop=False), r=[dt, EAb], w=[XB_])
                M(k, lambda e, h=h: e.matmul(XB_[:, xo + h:xo + h + 1], qk[:, h, lsl], nb_[:, h:h + 1],
                                             start=False, stop=True), r=[qk, nb_], w=[XB_])
        V(k, lambda e: e.tensor_tensor(Cst[:], Cst[:], DECAY[:, c, :].unsqueeze(2).broadcast_to([128, 4, 128]), ALU.mult),
          r=[Cst, DECAY], w=[Cst])
        V(k, lambda e: e.tensor_tensor(Cst[:].rearrange("p h d -> p (h d)"), Cst[:].rearrange("p h d -> p (h d)"),
                                       pb[b][:, :], ALU.add), r=[Cst, pb[b]], w=[Cst])
        V(k, lambda e: e.tensor_tensor(nst[:], nst[:], DECAY[:, c, :], ALU.mult), r=[nst, DECAY], w=[nst])
        V(k, lambda e: e.tensor_tensor(nst[:], nst[:], XB_[:, 8:12], ALU.add), r=[nst, XB_], w=[nst])
        if c >= 15 and c < 31:
            Cn, nn = Cbs[(c + 1) % 2], nbs[(c + 1) % 2]
            A(k, lambda e: e.activation(Cn[:], Cst[:], AF.Copy), r=[Cst], w=[Cn])
            A(k, lambda e: e.activation(nn[:], nst[:], AF.Copy), r=[nst], w=[nn])
        if own:
            sm = k.sb(f"sm{c}", [128, 8, 4], F32)
            V(k, lambda e: e.tensor_copy(sm[:, 0, :], XB_[:, xo:xo + 4]), r=[XB_], w=[sm])
            return sm
        return None

    def stageN(c, sm):
        oc = c - 16
        g_ = gmo[c % 3]
        AB_ = ABb[c % 2]
        V(k, lambda e: e.scalar_tensor_tensor(sm[:, 1, :], sm[:, 0, :], -1.0, sm[:, 0, :], ALU.mult, ALU.max), r=[sm], w=[sm])
        V(k, lambda e: e.tensor_tensor(sm[:, 1, :], sm[:, 1, :], FLOOR2[:, c, :], ALU.max), r=[sm, FLOOR2], w=[sm])
        V(k, lambda e: e.reciprocal(sm[:, 2, :], sm[:, 1, :]), r=[sm], w=[sm])
        hq = hsq[c % 2]
        A(k, lambda e: e.activation(hq[:].rearrange("p h d -> p (h d)"), AB_[:, :], AF.Square), r=[AB_], w=[hq])
        V(k, lambda e: e.tensor_reduce(sm[:, 3, :], hq[:], AX.X, ALU.add), r=[hq], w=[sm])
        V(k, lambda e: e.tensor_tensor(sm[:, 4, :], sm[:, 2, :], sm[:, 2, :], ALU.mult), r=[sm], w=[sm])
        V(k, lambda e: e.tensor_tensor(sm[:, 4, :], sm[:, 4, :], sm[:, 3, :], ALU.mult), r=[sm], w=[sm])
        V(k, lambda e: e.tensor_scalar(sm[:, 4, :], sm[:, 4, :], 1.0 / 128, EPS, ALU.mult, ALU.add), r=[sm], w=[sm])
        P(k, lambda e: e.tensor_tensor(sm[:, 5, :], sm[:, 4, :], mhalf[:].broadcast_to([128, 4]), ALU.pow),
          r=[sm, mhalf], w=[sm])
        V(k, lambda e: e.tensor_tensor(sm[:, 6, :], sm[:, 5, :], sm[:, 2, :], ALU.mult), r=[sm], w=[sm])
        hn, ha = hns[c % 2], hA[c % 2]
        V(k, lambda e: e.tensor_tensor(hn[:], AB_[:, :].rearrange("p (h d) -> p h d", h=4),
                                       sm[:, 6, :].unsqueeze(2).broadcast_to([128, 4, 128]), ALU.mult),
          r=[AB_, sm], w=[hn])
        P(k, lambda e: e.tensor_tensor(ha[:].rearrange("p h d -> p (h d)"), hn[:].rearrange("p h d -> p (h d)"),
                                       g_[:], ALU.mult), r=[hn, g_], w=[ha])

    def stageT(c):
        oc = c - 16
        ha = hA[c % 2]
        b = gbank()
        for h in range(4):
            M(k, lambda e, h=h: e.transpose(pbb[b][:, h * 128:(h + 1) * 128], ha[:, h, :], ident[:]),
              r=[ha, ident], w=[pb[b]])
        A(k, lambda e: e.activation(hAT[:, :, oc * 128:(oc + 1) * 128],
                                    pbb[b][:, 0:512].rearrange("p (h d) -> p h d", h=4), AF.Copy),
          r=[pb[b]], w=[hAT])

    supertile(0)
    stageA(0)
    pendN = None
    pendT = None
    for c in range(32):
        if c % 4 == 2 and c // 4 + 1 < 8:
            supertile(c // 4 + 1)
        if c + 1 < 32:
            stageA(c + 1)
        sm = stageB(c)
        if pendT is not None:
            stageT(pendT)
            pendT = None
        if pendN is not None:
            stageN(*pendN)
            pendT = pendN[0]
        pendN = (c, sm) if sm is not None else None
    stageT(pendT)
    stageN(*pendN)
    stageT(pendN[0])
    k.pop()
    k.pop()
    hBT = k.sb("hBT", [128, 2, T_OWN], BF16)

    if "hA" in dbg:
        o = dout("d_hAT", [128, 4, T_OWN])
        k.push()
        t32 = k.sb("dbg32b", [128, 4, T_OWN], F32)
        V(k, lambda e: e.tensor_copy(t32[:], hAT[:]), r=[hAT], w=[t32])
        k.dma('sp', o[:, :, :], t32[:], reads=[t32])
        k.pop()
    if upto == "C":
        return finish()

    rr2 = [0]

    def gb3():
        b = rr2[0] % 3
        rr2[0] += 1
        return b

    fm_ctr = [0]
    FM_ACC = [0, 1, 2, 4, 5, 6]
    FM_SSQ = [3, 7]

    def fm_norm(wsel, rhs_sel, n, summat, inv_dim, gcol, dst, dstbuf, rbufs, scr3, outs=None, vw=None, gbuf=None):
        idx = fm_ctr[0]
        fm_ctr[0] += 1
        b = FM_ACC[idx % 6]
        sb_ = FM_SSQ[idx % 2]
        sq, ms, rs = scr3[idx % 3]
        if outs is None:
            outs = [(dst, slice(0, 128))]
        if vw is None:
            vw = lambda ap: ap

        def s1():
            for c in range(8):
                M(k, lambda e, c=c: e.matmul(pb[b][:, 0:n], wsel(c), rhs_sel(c), start=(c == 0), stop=(c == 7)),
                  r=rbufs, w=[pb[b]])
            A(k, lambda e: e.activation(sq[:, 0:n], pb[b][:, 0:n], AF.Square), r=[pb[b]], w=[sq])

        def s2():
            M(k, lambda e: e.matmul(pb[sb_][:, 0:n], summat, sq[:, 0:n], start=True, stop=True), r=[sq], w=[pb[sb_]])
            A(k, lambda e: e.activation(ms[:, 0:n], pb[sb_][:, 0:n], AF.Ln, bias=EPS, scale=inv_dim), r=[pb[sb_]], w=[ms])
            A(k, lambda e: e.activation(rs[:, 0:n], ms[:, 0:n], AF.Exp, scale=-0.5), r=[ms], w=[rs])

        def s3():
            for (d_ap, psl) in outs:
                V(k, lambda e: e.scalar_tensor_tensor(d_ap, vw(pb[b][psl, 0:n]), gcol[psl], vw(rs[psl, 0:n]), ALU.mult, ALU.mult),
                  r=[pb[b], rs, gbuf], w=[dstbuf])
        return (s1, s2, s3)

    k.push()
    DIL = [1, 4, 16]
    wq3 = [k.sb(f"wdil{i}", [128, 8, 256], BF16) for i in range(3)]

    def load_dil(g, which):
        off = (O_DQ, O_DK, O_DV)[which]
        k.dma('pool', wq3[which][:], w_in[:, off + g * 256: off + (g + 1) * 256].rearrange("(c p) n -> p c n", p=128),
              writes=[wq3[which]])

    for which in range(3):
        load_dil(0, which)
    qpad = k.sb("qpad", [128, 2, 2, T_OWN], BF16)
    kTg = k.sb("kTg", [128, 2, T_ALL], BF16)
    vt = k.sb("vt", [128, 32, 256], BF16)
    accB = k.sb("accB", [128, 4, T_OWN], F32)
    gq2 = k.sb("gq2", [128, 3, 2], F32)
    P(k, lambda e: e.memset(qpad[64:128, :, 0, :], 0.0), w=[qpad])
    P(k, lambda e: e.memset(qpad[0:64, :, 1, :], 0.0), w=[qpad])
    mpfx01 = k.sb("mpfx01", [128, 128], BF16)
    k.dma('sp', mtmp[:], mpfx[:, :], writes=[mtmp])
    V(k, lambda e: e.tensor_scalar(mpfx01[:], mtmp[:], -1.0, None, ALU.is_ge), r=[mtmp], w=[mpfx01])
    for g in range(3):
        for hf in range(2):
            k.dma('sp', gq2[hf * 64:(hf + 1) * 64, g, 0:1], dil_q_g[g, :].rearrange("(p o) -> p o", o=1), writes=[gq2])
            k.dma('sp', gq2[hf * 64:(hf + 1) * 64, g, 1:2], dil_k_g[g, :].rearrange("(p o) -> p o", o=1), writes=[gq2])
    V(k, lambda e: e.tensor_scalar(gq2[:, :, 0:1], gq2[:, :, 0:1], 0.125, None, ALU.mult), r=[gq2], w=[gq2])
    scrs = [(k.sb(f"sq{i}", [128, 512], BF16), k.sb(f"ms{i}", [128, 512], F32), k.sb(f"rs{i}", [128, 512], F32))
            for i in range(3)]
    Pcs = [k.sb(f"Pc{i}", [128, 4, 128], BF16) for i in range(2)]
    Pps = [k.sb(f"Pp{i}", [128, 4, 128], BF16) for i in range(2)]
    nfm = 0
    ntile = 0
    for g in range(3):
        r_ = DIL[g]
        nblk = 16 // r_
        NK, NQ = T_ALL // r_, T_OWN // r_
        ktgs = [4, 5, 6, 7] + ([3] if g < 2 else [0, 1, 2, 3])
        if r_ == 1:
            vwn = (lambda ap: ap)
            pv = (lambda ap, a0: ap[:, a0:a0 + 512])
        else:
            vwn = (lambda ap, r_=r_: ap.rearrange("p (a r) -> p r a", r=r_))
            pv = (lambda ap, a0, r_=r_: ap.rearrange("p (r a) -> p r a", r=r_)[:, :, a0:a0 + 512 // r_])
        fitems = []
        for tg in ktgs:
            cs = slice(tg * 512, (tg + 1) * 512)
            for j in range(2):
                if tg >= 4:
                    a0 = (tg - 4) * 512 // r_
                    outs = [(pv(qpad[pp * 64:(pp + 1) * 64, j, pp, :], a0), slice(pp * 64, (pp + 1) * 64)) for pp in range(2)]
                    fitems.append(fm_norm(lambda c, j=j: wq3[0][:, c, j * 128:(j + 1) * 128], lambda c, cs=cs: xnT[:, c, cs], 512, blk64[:],
                                          1.0 / 64, gq2[:, g, 0:1], None, qpad, [xnT, wq3[0]], scrs, outs=outs, vw=vwn, gbuf=gq2))
                    nfm += 1
                a0 = tg * 512 // r_
                outs = [(pv(kTg[:, j, :], a0), slice(0, 128))]
                fitems.append(fm_norm(lambda c, j=j: wq3[1][:, c, j * 128:(j + 1) * 128], lambda c, cs=cs: xnT[:, c, cs], 512, blk64[:],
                                      1.0 / 64, gq2[:, g, 1:2], None, kTg, [xnT, wq3[1]], scrs, outs=outs, vw=vwn, gbuf=gq2))
                nfm += 1
        pipeN(fitems)
        if g + 1 < 3:
            load_dil(g + 1, 0)
            load_dil(g + 1, 1)
        vtiles = []
        for blk in range(nblk):
            for res in range(r_):
                lo = 2048 + blk * 128 * r_ + res
                vtiles.append((blk * r_ + res, slice(lo, lo + 127 * r_ + 1, r_)))
        for res in range(r_):
            lo = 2048 - 128 * r_ + res
            vtiles.append((16 + res, slice(lo, lo + 127 * r_ + 1, r_)))
        for vidx, tsl in vtiles:
            b = gb3()
            for c in range(8):
                M(k, lambda e, c=c: e.matmul(pb[b][:, 0:256], xnT[:, c, tsl], wq3[2][:, c, :], start=(c == 0), stop=(c == 7)),
                  r=[xnT, wq3[2]], w=[pb[b]])
            A(k, lambda e: e.activation(vt[:, vidx, :], pb[b][:, 0:256], AF.Copy), r=[pb[b]], w=[vt])
        if g + 1 < 3:
            load_dil(g + 1, 2)
        aitems = []
        for blk in range(nblk):
            for res in range(r_):
                own_lo = blk * 128 * r_ + res
                qs = slice(own_lo, own_lo + 127 * r_ + 1, r_)
                qp = res * NQ + blk * 128
                kc = res * NK + (2048 + blk * 128 * r_) // r_
                kp = kc - 128
                if blk > 0:
                    vprev = (blk - 1) * r_ + res
                else:
                    vprev = 16 + res
                vcur = blk * r_ + res
                SC, SP = pb[4 + ntile % 2], pb[2 + ntile % 2]
                Pc, Pp = Pcs[ntile % 2], Pps[ntile % 2]
                ob = pb[6 + ntile % 2]

                def s1(SC=SC, SP=SP, Pc=Pc, Pp=Pp, qp=qp, kc=kc, kp=kp, blk=blk):
                    for (bank, k0) in ((SC, kc), (SP, kp)):
                        for j in range(2):
                            for pp in range(2):
                                h = 2 * j + pp
                                M(k, lambda e, h=h, j=j, pp=pp: e.matmul(bank[:, h * 128:(h + 1) * 128], kTg[:, j, k0:k0 + 128],
                                                                        qpad[:, j, pp, qp:qp + 128], start=True, stop=True),
                                  r=[kTg, qpad], w=[bank])
                    A(k, lambda e: e.activation(Pc[:].rearrange("p h t -> p (h t)"), SC[:, :], AF.Exp), r=[SC], w=[Pc])
                    A(k, lambda e: e.activation(Pp[:].rearrange("p h t -> p (h t)"), SP[:, :], AF.Exp), r=[SP], w=[Pp])
                    P(k, lambda e: e.affine_select(Pc[:], Pc[:], pattern=[[0, 4], [1, 128]], compare_op=ALU.is_ge,
                                                   fill=0.0, base=0, channel_multiplier=-1), r=[Pc], w=[Pc])
                    if blk > 0:
                        P(k, lambda e: e.affine_select(Pp[:], Pp[:], pattern=[[0, 4], [-1, 128]], compare_op=ALU.is_ge,
                                                       fill=0.0, base=0, channel_multiplier=1), r=[Pp], w=[Pp])
                    else:
                        P(k, lambda e: e.tensor_tensor(Pp[:], Pp[:], mpfx01[:].unsqueeze(1).broadcast_to([128, 4, 128]), ALU.mult),
                          r=[Pp, mpfx01], w=[Pp])

                def s2(ob=ob, Pc=Pc, Pp=Pp, vprev=vprev, vcur=vcur, qs=qs, g=g):
                    for h in range(4):
                        j, pbs = h // 2, 64 * (h % 2)
                        M(k, lambda e, h=h, j=j, pbs=pbs: e.matmul(ob[pbs:pbs + 64, j * 128:(j + 1) * 128], vt[:, vprev, h * 64:(h + 1) * 64],
                                                                  Pp[:, h, :], start=True, stop=False,
                                                                  tile_position=(0, pbs)), r=[vt, Pp], w=[ob])
                        M(k, lambda e, h=h, j=j, pbs=pbs: e.matmul(ob[pbs:pbs + 64, j * 128:(j + 1) * 128], vt[:, vcur, h * 64:(h + 1) * 64],
                                                                  Pc[:, h, :], start=False, stop=True,
                                                                  tile_position=(0, pbs)), r=[vt, Pc], w=[ob])
                    for h in range(4):
                        j, pbs = h // 2, 64 * (h % 2)
                        M(k, lambda e, h=h, j=j, pbs=pbs: e.matmul(ob[pbs:pbs + 64, 256 + j * 128:256 + (j + 1) * 128], ones_bf[:, 0:64],
                                                                  Pp[:, h, :], start=True, stop=False,
                                                                  tile_position=(0, pbs)), r=[ones_bf, Pp], w=[ob])
                        M(k, lambda e, h=h, j=j, pbs=pbs: e.matmul(ob[pbs:pbs + 64, 256 + j * 128:256 + (j + 1) * 128], ones_bf[:, 0:64],
                                                                  Pc[:, h, :], start=False, stop=True,
                                                                  tile_position=(0, pbs)), r=[ones_bf, Pc], w=[ob])
                    obv = ob[:, :].rearrange("p (s t) -> p s t", s=4)
                    if g == 0:
                        V(k, lambda e: e.tensor_copy(accB[:, :, qs], obv), r=[ob], w=[accB])
                    else:
                        V(k, lambda e: e.tensor_tensor(accB[:, :, qs], obv, accB[:, :, qs], ALU.add), r=[ob, accB], w=[accB])
                aitems.append((s1, s2))
                ntile += 1
        pipe2(aitems)
    A(k, lambda e: e.activation(accB[:, 2:4, :], accB[:, 2:4, :], AF.Ln), r=[accB], w=[accB])
    A(k, lambda e: e.activation(accB[:, 2:4, :], accB[:, 2:4, :], AF.Exp, scale=-1.0), r=[accB], w=[accB])
    V(k, lambda e: e.tensor_tensor(hBT[:], accB[:, 0:2, :], accB[:, 2:4, :], ALU.mult), r=[accB], w=[hBT])
    k.pop()

    if "hB" in dbg:
        o = dout("d_hBT", [128, 2, T_OWN])
        k.push()
        t32 = k.sb("dbg32c", [128, 2, T_OWN], F32)
        V(k, lambda e: e.tensor_copy(t32[:], hBT[:]), r=[hBT], w=[t32])
        k.dma('sp', o[:, :, :], t32[:], reads=[t32])
        k.pop()
    if upto == "D":
        return finish()

    hCT = k.sb("hCT", [128, 4, T_OWN], BF16)
    k.push()
    gcolm = k.sb("gcolm", [128, 8], F32)
    k.dma('sp', gcolm[:], mem_norm_g.rearrange("(c p) -> p c", p=128), writes=[gcolm], allow_slow_non_contiguous=True)
    mnT = k.sb("mnT", [128, 8, 256], BF16)
    norm_T([(mem.rearrange("(t p) n -> p t n", p=128), 2)], mnT, gcolm, "m")
    wkv = k.sb("wkv", [128, 8, 1024], BF16)
    for c in range(8):
        k.dma('pool', wkv[:, c, :], w_mem_kv[c * 128:(c + 1) * 128, :], writes=[wkv])
    wxq = k.sb("wxq", [128, 8, 512], BF16)
    k.dma('pool', wxq[:], w_in[:, O_XQ:O_XQ + 512].rearrange("(c p) n -> p c n", p=128), writes=[wxq])
    gx = k.sb("gx", [128, 2], F32)
    k.dma('sp', gx[:, 0:1], x_q_g.rearrange("(p o) -> p o", o=1), writes=[gx])
    k.dma('sp', gx[:, 1:2], x_k_g.rearrange("(p o) -> p o", o=1), writes=[gx])
    V(k, lambda e: e.tensor_scalar(gx[:, 0:1], gx[:, 0:1], 128.0 ** -0.5, None, ALU.mult), r=[gx], w=[gx])
    kmT = k.sb("kmT", [128, 4, 256], BF16)
    vm = k.sb("vm", [128, 2, 512], BF16)
    xqT = k.sb("xqT", [128, 4, T_OWN], BF16)
    scrs = [(k.sb(f"sqe{i}", [128, 512], BF16), k.sb(f"mse{i}", [128, 512], F32), k.sb(f"rse{i}", [128, 512], F32))
            for i in range(3)]
    nfm = 0
    fitems = []
    for h in range(4):
        fitems.append(fm_norm(lambda c, h=h: wkv[:, c, h * 128:(h + 1) * 128], lambda c: mnT[:, c, :], 256, ones_bf[:], 1.0 / 128,
                              gx[:, 1:2], kmT[:, h, :], kmT, [mnT, wkv], scrs, gbuf=gx))
        nfm += 1
    pipeN(fitems)
    for mt in range(2):
        b = gb3()
        for c in range(8):
            M(k, lambda e, c=c: e.matmul(pb[b][:, :], mnT[:, c, mt * 128:(mt + 1) * 128], wkv[:, c, 512:1024],
                                         start=(c == 0), stop=(c == 7)), r=[mnT, wkv], w=[pb[b]])
        A(k, lambda e: e.activation(vm[:, mt, :], pb[b][:, :], AF.Copy), r=[pb[b]], w=[vm])
    fitems = []
    for tg in range(4):
        cs = slice(2048 + tg * 512, 2048 + (tg + 1) * 512)
        for h in range(4):
            fitems.append(fm_norm(lambda c, h=h: wxq[:, c, h * 128:(h + 1) * 128], lambda c, cs=cs: xnT[:, c, cs], 512, ones_bf[:], 1.0 / 128,
                                  gx[:, 0:1], xqT[:, h, tg * 512:(tg + 1) * 512], xqT, [xnT, wxq], scrs, gbuf=gx))
            nfm += 1
    pipeN(fitems)
    Pm = [[k.sb(f"Pm{i}{mt}", [128, 512], BF16) for mt in range(2)] for i in range(2)]
    rdn = [k.sb(f"rdn{i}", [128, 512], F32) for i in range(2)]
    it = 0
    eitems = []
    for tg in range(4):
        ts_ = slice(tg * 512, (tg + 1) * 512)
        for h in range(4):
            sbs = (pb[4], pb[5]) if it % 2 == 0 else (pb[2], pb[3])
            Pmi = Pm[it % 2]
            rd = rdn[it % 2]

            def s1(h=h, ts_=ts_, sbs=sbs, Pmi=Pmi):
                for mt in range(2):
                    sb_ = sbs[mt]
                    M(k, lambda e: e.matmul(sb_[:, :], kmT[:, h, mt * 128:(mt + 1) * 128], xqT[:, h, ts_], start=True, stop=True),
                      r=[kmT, xqT], w=[sb_])
                    A(k, lambda e: e.activation(Pmi[mt][:], sb_[:, :], AF.Exp), r=[sb_], w=[Pmi[mt]])

            def s2(h=h, ts_=ts_, Pmi=Pmi, rd=rd):
                nb6, db7 = pb[6], pb[7]
                for mt in range(2):
                    M(k, lambda e: e.matmul(nb6[:, :], vm[:, mt, h * 128:(h + 1) * 128], Pmi[mt][:], start=(mt == 0), stop=(mt == 1)),
                      r=[vm, Pmi[mt]], w=[nb6])
                for mt in range(2):
                    M(k, lambda e: e.matmul(db7[:, :], ones_bf[:], Pmi[mt][:], start=(mt == 0), stop=(mt == 1)),
                      r=[ones_bf, Pmi[mt]], w=[db7])
                A(k, lambda e: e.activation(rd[:], db7[:, :], AF.Ln), r=[db7], w=[rd])
                A(k, lambda e: e.activation(rd[:], rd[:], AF.Exp, scale=-1.0), r=[rd], w=[rd])
                V(k, lambda e: e.tensor_tensor(hCT[:, h, ts_], nb6[:, :], rd[:], ALU.mult), r=[nb6, rd], w=[hCT])
            eitems.append((s1, s2))
            it += 1
    pipe2(eitems)
    k.pop()

    if "hC" in dbg:
        o = dout("d_hCT", [128, 4, T_OWN])
        k.push()
        t32 = k.sb("dbg32d", [128, 4, T_OWN], F32)
        V(k, lambda e: e.tensor_copy(t32[:], hCT[:]), r=[hCT], w=[t32])
        k.dma('sp', o[:, :, :], t32[:], reads=[t32])
        k.pop()
    if upto == "E":
        return finish()

    MIXT_OFF = 195584
    OUT_OFF = 130048
    mixT = k.sb_at("mixT", [128, 8, T_OWN], BF16, MIXT_OFF)
    k.limit = MIXT_OFF
    rr8 = [0]

    def nbk():
        b = pb[rr8[0] % 8]
        rr8[0] += 1
        return b

    k.push()
    bg = k.sb("bg", [128, 3, 8], F32)
    for b in range(3):
        k.dma('sp', bg[:, b, :], b_gate[b * 1024:(b + 1) * 1024].rearrange("(j p) -> p j", p=128), writes=[bg],
              allow_slow_non_contiguous=True)
    wgj = [k.sb(f"wgj{i}", [128, 8, 3, 128], BF16) for i in range(3)]
    waj = [k.sb(f"waj{i}", [128, 4, 128], BF16) for i in range(3)]
    wbj = [k.sb(f"wbj{i}", [128, 2, 128], BF16) for i in range(3)]
    wcj = [k.sb(f"wcj{i}", [128, 4, 128], BF16) for i in range(3)]

    def loadF(j):
        w = j % 3
        for b in range(3):
            k.dma('pool', wgj[w][:, :, b, :], w_gate[:, b * 1024 + j * 128:b * 1024 + (j + 1) * 128].rearrange("(c p) n -> p c n", p=128),
                  writes=[wgj[w]])
        k.dma('pool', waj[w][:], w_a[:, j * 128:(j + 1) * 128].rearrange("(c p) n -> p c n", p=128), writes=[waj[w]])
        k.dma('pool', wbj[w][:], w_b[:, j * 128:(j + 1) * 128].rearrange("(c p) n -> p c n", p=128), writes=[wbj[w]])
        k.dma('pool', wcj[w][:], w_c[:, j * 128:(j + 1) * 128].rearrange("(c p) n -> p c n", p=128), writes=[wcj[w]])

    loadF(0)
    loadF(1)
    sig = [k.sb(f"sig{i}", [128, 3, 512], F32) for i in range(2)]
    mm_ = [k.sb(f"mm{i}", [128, 3, 512], F32) for i in range(2)]
    for j in range(8):
        w = j % 3
        wg_ = wgj[w]
        for tg in range(4):
            it = j * 4 + tg
            sg, m_ = sig[it % 2], mm_[it % 2]
            cs = slice(2048 + tg * 512, 2048 + (tg + 1) * 512)
            ts_ = slice(tg * 512, (tg + 1) * 512)
            for b in range(3):
                bank = nbk()
                for c in range(8):
                    M(k, lambda e, c=c: e.matmul(bank[:, :], wg_[:, c, b, :], xnT[:, c, cs],
                                                 start=(c == 0), stop=(c == 7)), r=[wg_, xnT], w=[bank])
                A(k, lambda e: e.activation(sg[:, b, :], bank[:, :], AF.Sigmoid, bias=bg[:, b, j:j + 1]), r=[bank, bg], w=[sg])
            for (b, wt, hT_, nk) in ((0, waj[w], hAT, 4), (1, wbj[w], hBT, 2), (2, wcj[w], hCT, 4)):
                bank = nbk()
                for c in range(nk):
                    M(k, lambda e, c=c: e.matmul(bank[:, :], wt[:, c, :], hT_[:, c, ts_], start=(c == 0), stop=(c == nk - 1)),
                      r=[wt, hT_], w=[bank])
                V(k, lambda e: e.tensor_tensor(m_[:, b, :], bank[:, :], sg[:, b, :], ALU.mult), r=[bank, sg], w=[m_])
            P(k, lambda e: e.tensor_tensor(m_[:, 0, :], m_[:, 0, :], m_[:, 1, :], ALU.add), r=[m_], w=[m_])
            P(k, lambda e: e.tensor_tensor(mixT[:, j, ts_], m_[:, 0, :], m_[:, 2, :], ALU.add), r=[m_], w=[mixT])
            if tg == 0 and j + 2 < 8:
                loadF(j + 2)
    k.pop()
    k.pop()

    out_acc = k.sb_at("out_acc", [128, 16, 1024], F32, OUT_OFF)
    k.limit = OUT_OFF
    k.push()
    oacc = [k.view(out_acc, f"oacc{i}") for i in range(16)]
    hnT = k.sb("hnT", [128, 8, T_OWN], BF16)
    combT = k.sb("combT", [32, T_OWN], BF16)
    k.push()
    wo = k.sb("wo", [128, 8, 1024], BF16)
    for c in range(8):
        k.dma('pool', wo[:, c, :], w_out[c * 128:(c + 1) * 128, :], writes=[wo])
    xrs = [k.sb(f"xr{i}", [128, 1024], F32) for i in range(3)]

    def outproj(i):
        xt = xrs[i % 3]
        k.dma('sp', xt[:], x_own[i * 128:(i + 1) * 128, :], writes=[xt])
        for hf in range(2):
            bank = nbk()
            for c in range(8):
                M(k, lambda e, c=c: e.matmul(bank[:, :], mixT[:, c, i * 128:(i + 1) * 128], wo[:, c, hf * 512:(hf + 1) * 512],
                                             start=(c == 0), stop=(c == 7)), r=[mixT, wo], w=[bank])
            V(k, lambda e: e.tensor_tensor(out_acc[:, i, hf * 512:(hf + 1) * 512], bank[:, :], xt[:, hf * 512:(hf + 1) * 512], ALU.add),
              r=[bank, xt], w=[oacc[i]])

    gcol2 = k.sb("gcol2", [128, 8], F32)
    k.dma('sp', gcol2[:], ln2_g.rearrange("(c p) -> p c", p=128), writes=[gcol2], allow_slow_non_contiguous=True)
    wr32 = k.sb("wr32", [128, 8, 36], F32)
    k.dma('sp', wr32[:, :, 0:4], w_grp.rearrange("(c p) n -> p c n", p=128), writes=[wr32])
    for g in range(4):
        k.dma('sp', wr32[:, :, 4 + 8 * g:12 + 8 * g], w_er[g, :, :].rearrange("(c p) n -> p c n", p=128), writes=[wr32])
    brow = k.sb("brow", [128, 36], F32)
    k.dma('sp', brow[:, 0:4], b_grp.partition_broadcast(128), writes=[brow])
    k.dma('sp', brow[:, 4:36], b_er.partition_broadcast(128), writes=[brow])
    LG = k.sb("LG", [128, 16, 36], F32)
    hss = [k.sb(f"hs{i}", [128, 1024], F32) for i in range(2)]
    hn32 = [k.sb(f"hn32T{i}", [128, 8, 128], F32) for i in range(2)]
    junk2 = k.sb("junk2", [128, 1024], BF16)
    gitems = []
    for i in range(16):
        hs, h32 = hss[i % 2], hn32[i % 2]
        ss = k.sb(f"ssg{i}", [128, 1], F32)
        rs = k.sb(f"rsg{i}", [128, 1], F32)

        def s1(i=i, hs=hs, ss=ss, rs=rs):
            A(k, lambda e: e.activation(junk2[:], out_acc[:, i, :], AF.Square, accum_out=ss[:]), r=[oacc[i]], w=[junk2, ss])
            V(k, lambda e: e.tensor_scalar(ss[:], ss[:], 1.0 / 1024, EPS, ALU.mult, ALU.add), r=[ss], w=[ss])
            P(k, lambda e: e.tensor_tensor(rs[:], ss[:], mhalf[:], ALU.pow), r=[ss, mhalf], w=[rs])
            A(k, lambda e: e.activation(hs[:], out_acc[:, i, :], AF.Copy, scale=rs[:, 0:1]), r=[oacc[i], rs], w=[hs])

        def s2(i=i, hs=hs, h32=h32):
            for q in range(2):
                bank = nbk()
                for c in range(4):
                    M(k, lambda e, c=c: e.transpose(bank[:, c * 128:(c + 1) * 128], hs[:, (4 * q + c) * 128:(4 * q + c + 1) * 128], identf[:]),
                      r=[hs, identf], w=[bank])
                bv = bank[:, :].rearrange("p (c t) -> p c t", c=4)
                gb_ = gcol2[:, 4 * q:4 * q + 4].unsqueeze(2).broadcast_to([128, 4, 128])
                V(k, lambda e: e.tensor_tensor(hnT[:, 4 * q:4 * q + 4, i * 128:(i + 1) * 128], bv, gb_, ALU.mult),
                  r=[bank, gcol2], w=[hnT])
                V(k, lambda e: e.tensor_tensor(h32[:, 4 * q:4 * q + 4, :], bv, gb_, ALU.mult), r=[bank, gcol2], w=[h32])

        def s3(i=i, h32=h32):
            bank = nbk()
            for c in range(8):
                M(k, lambda e, c=c: e.matmul(bank[:, 0:36], h32[:, c, :], wr32[:, c, :], start=(c == 0), stop=(c == 7)),
                  r=[h32, wr32], w=[bank])
            V(k, lambda e: e.tensor_tensor(LG[:, i, :], bank[:, 0:36], brow[:], ALU.add), r=[bank, brow], w=[LG])
        gitems.append((s1, s2, s3))
    outproj(0)
    outproj(1)
    for i in range(16 + 2):
        if i + 2 < 16:
            outproj(i + 2)
        if i < 16:
            gitems[i][0]()
        if 0 <= i - 1 < 16:
            gitems[i - 1][1]()
        if 0 <= i - 2 < 16:
            gitems[i - 2][2]()
    R = k.sb("R", [128, 16, 80], F32)
    T4 = k.sb("T4", [128, 16, 4, 8], F32)
    comb = k.sb("comb", [128, 16, 4, 8], F32)
    lg = LG[:, :, 0:4]
    le = LG[:, :, 4:36].rearrange("p t (g e) -> p t g e", g=4)
    gmax, gs, gw, v1, v2, e2, w1, w2 = (R[:, :, i] for i in range(8))
    oh = R[:, :, 8:12]
    ex = R[:, :, 12:16]
    sel = R[:, :, 16:24]
    m1 = R[:, :, 24:32]
    sel2 = R[:, :, 32:40]
    m2 = R[:, :, 40:48]
    wi = R[:, :, 48:56]
    wi2 = R[:, :, 56:64]

    def bc(ap, n):
        return ap.unsqueeze(2).broadcast_to([128, 16, n])

    def VR(fn):
        V(k, fn, r=[R, LG, T4], w=[R, T4])

    VR(lambda e: e.tensor_reduce(gmax, lg, AX.X, ALU.max))
    VR(lambda e: e.tensor_tensor(oh, lg, bc(gmax, 4), ALU.is_equal))
    VR(lambda e: e.tensor_tensor(ex, lg, bc(gmax, 4), ALU.subtract))
    A(k, lambda e: e.activation(ex, ex, AF.Exp), r=[R], w=[R])
    VR(lambda e: e.tensor_reduce(gs, ex, AX.X, ALU.add))
    VR(lambda e: e.reciprocal(gw, gs))
    VR(lambda e: e.tensor_tensor(T4[:], le, oh.unsqueeze(3).broadcast_to([128, 16, 4, 8]), ALU.mult))
    VR(lambda e: e.tensor_reduce(sel, T4[:].rearrange("p t g e -> p t e g"), AX.X, ALU.add))
    VR(lambda e: e.tensor_reduce(v1, sel, AX.X, ALU.max))
    VR(lambda e: e.tensor_tensor(m1, sel, bc(v1, 8), ALU.is_equal))
    VR(lambda e: e.scalar_tensor_tensor(sel2, m1, -1e30, sel, ALU.mult, ALU.add))
    VR(lambda e: e.tensor_reduce(v2, sel2, AX.X, ALU.max))
    VR(lambda e: e.tensor_tensor(m2, sel2, bc(v2, 8), ALU.is_equal))
    VR(lambda e: e.tensor_tensor(e2, v2, v1, ALU.subtract))
    A(k, lambda e: e.activation(e2, e2, AF.Exp), r=[R], w=[R])
    VR(lambda e: e.tensor_scalar(w2, e2, 1.0, None, ALU.add))
    VR(lambda e: e.reciprocal(w2, w2))
    VR(lambda e: e.tensor_tensor(w1, gw, w2, ALU.mult))
    VR(lambda e: e.tensor_tensor(w2, w1, e2, ALU.mult))
    VR(lambda e: e.tensor_tensor(wi, m1, bc(w1, 8), ALU.mult))
    VR(lambda e: e.tensor_tensor(wi2, m2, bc(w2, 8), ALU.mult))
    VR(lambda e: e.tensor_tensor(wi, wi, wi2, ALU.add))
    V(k, lambda e: e.tensor_tensor(comb[:], oh.unsqueeze(3).broadcast_to([128, 16, 4, 8]),
                                   wi.unsqueeze(2).broadcast_to([128, 16, 4, 8]), ALU.mult), r=[R], w=[comb])
    for i in range(16):
        bank = nbk()
        M(k, lambda e: e.transpose(bank[0:32, 0:128], comb[:, i, :, :].rearrange("p g e -> p (g e)"), identf[:]),
          r=[comb, identf], w=[bank])
        A(k, lambda e: e.activation(combT[0:32, i * 128:(i + 1) * 128], bank[0:32, 0:128], AF.Copy), r=[bank], w=[combT])
    if "comb" in dbg:
        o = dout("d_comb", [128, 16 * 32])
        k.dma('sp', o[:, :], comb[:].rearrange("p t g e -> p (t g e)"), reads=[comb])
    k.pop()
    if "h1" in dbg:
        o = dout("d_h1", [T_OWN, 1024])
        k.dma('sp', o.rearrange("(i p) n -> p i n", p=128), out_acc[:], reads=oacc)
    if upto in ("F", "G"):
        k.wait_all('sp', oacc)
        return finish()

    k.push()
    selall = k.sb("selall", [32, 32, 128], BF16)
    P(k, lambda e: e.memset(selall[:], 0.0), w=[selall])
    P(k, lambda e: e.affine_select(selall[:], selall[:], pattern=[[1, 32], [0, 128]], compare_op=ALU.not_equal,
                                   fill=1.0, base=0, channel_multiplier=-1), r=[selall], w=[selall])
    weg = [k.sb(f"weg{i}", [128, 8, 256], BF16) for i in range(3)]
    weu = [k.sb(f"weu{i}", [128, 8, 256], BF16) for i in range(3)]
    wed = [k.sb(f"wed{i}", [128, 2, 1024], BF16) for i in range(3)]

    def load_gu(ei):
        k.dma('pool', weg[ei % 3][:], w_eg[ei, :, :].rearrange("(c p) n -> p c n", p=128), writes=[weg[ei % 3]])
        k.dma('pool', weu[ei % 3][:], w_eu[ei, :, :].rearrange("(c p) n -> p c n", p=128), writes=[weu[ei % 3]])

    def load_d(ei):
        k.dma('pool', wed[ei % 3][:], w_ed[ei, :, :].rearrange("(c p) n -> p c n", p=128), writes=[wed[ei % 3]])

    load_gu(0)
    load_d(0)
    load_gu(1)
    load_d(1)
    sgs = [k.sb(f"sgs{i}", [128, 512], F32) for i in range(2)]
    cbs = [k.sb(f"cbs{i}", [128, 512], BF16) for i in range(2)]
    t1s = [k.sb(f"t1s{i}", [128, 512], BF16) for i in range(2)]
    hids = [k.sb(f"hid{i}", [128, 2, 512], BF16) for i in range(3)]
    evs = [k.sb(f"evs{i}", [128, 512], F32) for i in range(2)]
    pending = []
    nacc = [0]

    def emit_down(e_, tg, hid, w):
        for tt in range(4):
            ti = tg * 4 + tt
            for hf in range(2):
                bank = nbk()
                for f in range(2):
                    M(k, lambda e, f=f: e.matmul(bank[:, :], hid[:, f, tt * 128:(tt + 1) * 128], wed[w][:, f, hf * 512:(hf + 1) * 512],
                                                 start=(f == 0), stop=(f == 1)), r=[hid, wed[w]], w=[bank])
                dst = out_acc[:, ti, hf * 512:(hf + 1) * 512]
                if nacc[0] % 3 == 2:
                    ev = evs[(nacc[0] // 3) % 2]
                    A(k, lambda e: e.activation(ev[:], bank[:, :], AF.Copy), r=[bank], w=[ev])
                    P(k, lambda e: e.tensor_tensor(dst, dst, ev[:], ALU.add), r=[ev, oacc[ti]], w=[oacc[ti]])
                else:
                    V(k, lambda e: e.tensor_tensor(dst, bank[:, :], dst, ALU.add), r=[bank, oacc[ti]], w=[oacc[ti]])
                nacc[0] += 1
            if e_ == 31:
                k.dma('sp', out[ti * 128:(ti + 1) * 128, :], out_acc[:, ti, :], reads=[oacc[ti]])

    itn = 0
    for e_ in range(32):
        w = e_ % 3
        for tg in range(4):
            ts_ = slice(tg * 512, (tg + 1) * 512)
            hid = hids[itn % 3]
            cb = cbs[itn % 2]
            cbank = nbk()
            M(k, lambda e: e.matmul(cbank[:, :], selall[0:32, e_, :], combT[0:32, ts_], start=True, stop=True),
              r=[selall, combT], w=[cbank])
            A(k, lambda e: e.activation(cb[:], cbank[:, :], AF.Copy), r=[cbank], w=[cb])
            for f in range(2):
                gbank = nbk()
                for c in range(8):
                    M(k, lambda e, c=c: e.matmul(gbank[:, :], weg[w][:, c, f * 128:(f + 1) * 128], hnT[:, c, ts_],
                                                 start=(c == 0), stop=(c == 7)), r=[weg[w], hnT], w=[gbank])
                ubank = nbk()
                for c in range(8):
                    M(k, lambda e, c=c: e.matmul(ubank[:, :], weu[w][:, c, f * 128:(f + 1) * 128], hnT[:, c, ts_],
                                                 start=(c == 0), stop=(c == 7)), r=[weu[w], hnT], w=[ubank])
                sg = sgs[(itn * 2 + f) % 2]
                t1 = t1s[(itn * 2 + f) % 2]
                A(k, lambda e: e.activation(sg[:], gbank[:, :], AF.Silu), r=[gbank], w=[sg])
                V(k, lambda e: e.tensor_tensor(t1[:], ubank[:, :], sg[:], ALU.mult), r=[ubank, sg], w=[t1])
                P(k, lambda e: e.tensor_tensor(hid[:, f, :], t1[:], cb[:], ALU.mult), r=[t1, cb], w=[hid])
            if pending:
                emit_down(*pending.pop())
            pending.append((e_, tg, hid, w))
            if e_ + 2 < 32 and tg == 1:
                load_gu(e_ + 2)
            if e_ + 2 < 32 and tg == 2:
                load_d(e_ + 2)
            itn += 1
    emit_down(*pending.pop())
    k.wait_all('sp', oacc)
    k.pop()
    k.pop()
    return finish()


def make_in_maps(inputs):
    f = lambda a: np.ascontiguousarray(np.asarray(a, dtype=np.float32))
    x = f(inputs["x"])
    mem = f(inputs["mem"])
    shared = {
        "ln1_g": f(inputs["ln1_g"][0]), "w_in": f(inputs["w_in"][0]), "m_conv": f(inputs["m_conv"][0]),
        "m_i_b": f(inputs["m_i_b"][0]), "m_f_b": f(inputs["m_f_b"][0]), "m_norm_g": f(inputs["m_norm_g"][0]).reshape(512),
        "dil_q_g": f(inputs["dil_q_g"][0]), "dil_k_g": f(inputs["dil_k_g"][0]), "mem_norm_g": f(inputs["mem_norm_g"][0]),
        "w_mem_kv": f(inputs["w_mem_kv"][0]), "x_q_g": f(inputs["x_q_g"][0]), "x_k_g": f(inputs["x_k_g"][0]),
        "w_a": f(inputs["w_a"][0]), "w_b": f(inputs["w_b"][0]), "w_c": f(inputs["w_c"][0]),
        "w_gate": f(inputs["w_gate"][0]), "b_gate": f(inputs["b_gate"][0]), "w_out": f(inputs["w_out"][0]),
        "ln2_g": f(inputs["ln2_g"][0]), "w_grp": f(inputs["w_grp"][0]), "b_grp": f(inputs["b_grp"][0]),
        "w_er": f(inputs["w_er"][0]), "b_er": f(inputs["b_er"][0]).reshape(32),
        "w_eg": f(inputs["w_eg"][0]).reshape(32, 1024, 256), "w_eu": f(inputs["w_eu"][0]).reshape(32, 1024, 256),
        "w_ed": f(inputs["w_ed"][0]).reshape(32, 256, 1024),
    }
    s, t = np.meshgrid(np.arange(128), np.arange(128), indexing="ij")
    tri_prev = np.where(s >= t, 0.0, NEG).astype(np.float32)
    in_maps = []
    for core in range(8):
        b, half = core // 2, core % 2
        m = dict(shared)
        m["x_own"] = np.ascontiguousarray(x[b, half * T_OWN:(half + 1) * T_OWN])
        m["mem"] = np.ascontiguousarray(mem[b])
        pm = np.ones((4, T_ALL), np.float32)
        pa = np.zeros((4, T_ALL), np.float32)
        if half == 0:
            m["x_pre"] = np.zeros((T_OWN, 1024), np.float32)
            pm[:, :T_OWN] = 0.0
            pa[:, :T_OWN] = NEG
            m["mpfx"] = np.full((128, 128), NEG, np.float32)
        else:
            m["x_pre"] = np.ascontiguousarray(x[b, 0:T_OWN])
            m["mpfx"] = tri_prev
        m["pmul"] = pm
        m["padd"] = pa
        in_maps.append(m)
    return in_maps


_CACHE = {}


def kernel(**inputs):
    if "nc" not in _CACHE:
        _CACHE["nc"] = build_program()[0]
    nc = _CACHE["nc"]
    in_maps = make_in_maps(inputs)
    res = run_bass_kernel_spmd(nc, in_maps, core_ids=list(range(8)))
    outp = np.zeros((4, 4096, 1024), np.float32)
    for core in range(8):
        b, half = core // 2, core % 2
        outp[b, half * T_OWN:(half + 1) * T_OWN] = res.results[core]["out"]
    return outp
```

```python
import contextlib
import numpy as np
import concourse.bass as bass
import concourse.mybir as mybir
from concourse.bass_utils import run_bass_kernel_spmd

F32 = mybir.dt.float32
BF16 = mybir.dt.bfloat16
AF = mybir.ActivationFunctionType
ALU = mybir.AluOpType
AX = mybir.AxisListType

NEG = -30000.0
EPS = 1e-6
T_OWN = 2048
T_ALL = 4096


class Buf:
    def __init__(self, name, t, excl=False):
        self.name = name
        self.t = t
        self.w = None
        self.r = {}
        self.excl = excl
        self.ds = None
        self.ds_sw = None

    def __getitem__(self, key):
        return self.t[key]


class DSem:
    def __init__(self, h):
        self.h = h
        self.cnt = 0


class KB:
    EPOCH = 30000

    def __init__(self, nc):
        self.nc = nc
        self.root = contextlib.ExitStack()
        self.es = self.root
        self.stack = []
        self.eng = {'pe': nc.tensor, 'act': nc.scalar, 'dve': nc.vector,
                    'pool': nc.gpsimd, 'sp': nc.sync}
        self.sem = {}
        self.cnt = {}
        self.nsem = 0
        self.seen = {e: {} for e in self.eng}
        self.ninst = {e: 0 for e in self.eng}
        self.nwait = {e: 0 for e in self.eng}
        self.dfree = []
        self.dfree_sw = []
        self.scope_bufs = [[]]
        self.used = 16512
        self.peak = 0
        self.limit = 229344
        self.used_stack = []
        for e in ('pe', 'act', 'dve', 'pool'):
            self._newsem(e)

    def _alloc_sem(self, name):
        self.nsem += 1
        return self.root.enter_context(self.nc.semaphore(f"{name}_{self.nsem}"))

    def _newsem(self, e):
        self.sem[e] = self._alloc_sem("s" + e)
        self.cnt[e] = 0

    def _dsem(self, b, sw=False):
        if sw:
            if b.ds_sw is None:
                b.ds_sw = self.dfree_sw.pop() if self.dfree_sw else DSem(self._alloc_sem("w"))
            return b.ds_sw
        if b.ds is None:
            b.ds = self.dfree.pop() if self.dfree else DSem(self._alloc_sem("d"))
        return b.ds

    def sb(self, name, shape, dtype):
        t = self.es.enter_context(self.nc.sbuf_tensor(name, list(shape), dtype))
        nbytes = int(np.prod(shape[1:])) * (2 if dtype == BF16 else 4)
        self.used += (nbytes + 31) // 32 * 32
        assert self.used <= self.limit, (name, self.used, self.limit)
        self.peak = max(self.peak, self.used)
        b = Buf(name, t)
        self.scope_bufs[-1].append(b)
        return b

    def sb_at(self, name, shape, dtype, offset):
        t = self.nc.alloc_sbuf_tensor_at(name, list(shape), dtype, offset=offset)
        return Buf(name, t)

    def ps(self, name, shape, dtype=F32):
        t = self.es.enter_context(self.nc.psum_tensor(name, list(shape), dtype))
        return Buf(name, t, excl=True)

    def view(self, b, name=None):
        nb = Buf(name or b.name, b.t, b.excl)
        self.scope_bufs[-1].append(nb)
        return nb

    def push(self):
        self.stack.append(self.es)
        self.es = contextlib.ExitStack()
        self.scope_bufs.append([])
        self.used_stack.append(self.used)

    def pop(self):
        bufs = self.scope_bufs.pop()
        for e in self.eng:
            self.wait_all(e, bufs)
        self.barrier()
        for b in bufs:
            if b.ds is not None:
                self.dfree.append(b.ds)
                b.ds = None
            if b.ds_sw is not None:
                self.dfree_sw.append(b.ds_sw)
                b.ds_sw = None
        self.es.close()
        self.es = self.stack.pop()
        self.used = self.used_stack.pop()

    def barrier(self):
        for e in self.eng:
            for e2 in ('pe', 'act', 'dve', 'pool'):
                if e2 != e and self.cnt[e2] > 0:
                    self._wait(e, (self.sem[e2], self.cnt[e2], e2))

    def _wait(self, e, ev):
        sem, val, eng = ev
        key = sem.name
        if self.seen[e].get(key, 0) >= val:
            return
        self.eng[e].wait_ge(sem, val)
        self.seen[e][key] = val
        self.nwait[e] += 1

    STRICT = True

    def _deps(self, e, reads, writes):
        same_ok = (e == 'pe') or not self.STRICT
        for b in reads:
            if b.w is not None:
                if not (b.w[2] == e and e == 'pe'):
                    self._wait(e, b.w)
            if b.excl:
                for ev in list(b.r.values()):
                    if ev[2] != e:
                        self._wait(e, ev)
        for b in writes:
            if b.w is not None and (b.w[2] != e or not same_ok):
                self._wait(e, b.w)
            for ev in list(b.r.values()):
                if ev[2] != e or not same_ok:
                    self._wait(e, ev)

    def _record(self, ev, reads, writes):
        for b in reads:
            old = b.r.get(ev[0].name)
            if old is None or old[1] < ev[1]:
                b.r[ev[0].name] = ev
        for b in writes:
            b.w = ev
            b.r = {}

    def op(self, e, fn, reads=(), writes=()):
        self._deps(e, reads, writes)
        ins = fn(self.eng[e])
        if self.cnt[e] >= self.EPOCH:
            self._newsem(e)
        self.cnt[e] += 1
        ins.then_inc(self.sem[e], 1)
        ev = (self.sem[e], self.cnt[e], e)
        self._record(ev, reads, writes)
        self.ninst[e] += 1
        return ev

    def dma(self, q, out, in_, reads=(), writes=(), sembuf=None, **kw):
        self._deps(q, reads, writes)
        ins = self.eng[q].dma_start(out=out, in_=in_, **kw)
        b = sembuf if sembuf is not None else (writes[0] if writes else reads[0])
        ds = self._dsem(b, sw=(q == 'pool'))
        ds.cnt += 16
        ins.then_inc(ds.h, 16)
        ev = (ds.h, ds.cnt, 'dma')
        self._record(ev, reads, writes)
        self.ninst[q] += 1
        return ev

    def wait_all(self, e, bufs):
        for b in bufs:
            if b.w is not None:
                self._wait(e, b.w)
            for ev in list(b.r.values()):
                self._wait(e, ev)

    def close(self):
        self.root.close()


def V(k, fn, r=(), w=()):
    return k.op('dve', fn, r, w)


def A(k, fn, r=(), w=()):
    return k.op('act', fn, r, w)


def P(k, fn, r=(), w=()):
    return k.op('pool', fn, r, w)


def M(k, fn, r=(), w=()):
    return k.op('pe', fn, r, w)


def pipe2(items):
    prev = None
    for s1, s2 in items:
        s1()
        if prev is not None:
            prev()
        prev = s2
    if prev is not None:
        prev()


def pipeN(items):
    if not items:
        return
    ns = len(items[0])
    for t in range(len(items) + ns - 1):
        for kk in range(ns):
            i = t - kk
            if 0 <= i < len(items):
                items[i][kk]()


O_MQ, O_MK, O_MV, O_MO, O_MI, O_MF, O_DQ, O_DK, O_DV, O_XQ = 0, 512, 1024, 1536, 2048, 2052, 2056, 2824, 3592, 4360
IN_TOTAL = 4872


def build_program(upto="all", dbg=()):
    nc = bass.Bass("TRN2", target_bir_lowering=False)

    def din(name, shape):
        return nc.dram_tensor(name, list(shape), F32, kind="ExternalInput").ap()

    x_own = din("x_own", [T_OWN, 1024])
    x_pre = din("x_pre", [T_OWN, 1024])
    mem = din("mem", [256, 1024])
    ln1_g = din("ln1_g", [1024])
    w_in = din("w_in", [1024, IN_TOTAL])
    m_conv = din("m_conv", [4, 1024])
    m_i_b = din("m_i_b", [4])
    m_f_b = din("m_f_b", [4])
    m_norm_g = din("m_norm_g", [512])
    dil_q_g = din("dil_q_g", [3, 64])
    dil_k_g = din("dil_k_g", [3, 64])
    mem_norm_g = din("mem_norm_g", [1024])
    w_mem_kv = din("w_mem_kv", [1024, 1024])
    x_q_g = din("x_q_g", [128])
    x_k_g = din("x_k_g", [128])
    w_a = din("w_a", [512, 1024])
    w_b = din("w_b", [256, 1024])
    w_c = din("w_c", [512, 1024])
    w_gate = din("w_gate", [1024, 3072])
    b_gate = din("b_gate", [3072])
    w_out = din("w_out", [1024, 1024])
    ln2_g = din("ln2_g", [1024])
    w_grp = din("w_grp", [1024, 4])
    b_grp = din("b_grp", [4])
    w_er = din("w_er", [4, 1024, 8])
    b_er = din("b_er", [32])
    w_eg = din("w_eg", [32, 1024, 256])
    w_eu = din("w_eu", [32, 1024, 256])
    w_ed = din("w_ed", [32, 256, 1024])
    pmul = din("pmul", [4, T_ALL])
    padd = din("padd", [4, T_ALL])
    mpfx = din("mpfx", [128, 128])
    out = nc.dram_tensor("out", [T_OWN, 1024], F32, kind="ExternalOutput").ap()
    dbg_out = {}

    def dout(name, shape):
        dbg_out[name] = nc.dram_tensor(name, list(shape), F32, kind="ExternalOutput").ap()
        return dbg_out[name]

    k = KB(nc)
    pb = [k.ps(f"pb{i}", [128, 512], F32) for i in range(8)]
    pbb = [b.t.bitcast(BF16) for b in pb]

    identf = k.sb("identf", [128, 128], F32)
    ident = k.sb("ident", [128, 128], BF16)
    P(k, lambda e: e.memset(identf[:], 0.0), w=[identf])
    P(k, lambda e: e.affine_select(identf[:], identf[:], pattern=[[-1, 128]], compare_op=ALU.not_equal,
                                   fill=1.0, base=0, channel_multiplier=1), r=[identf], w=[identf])
    V(k, lambda e: e.tensor_copy(ident[:], identf[:]), r=[identf], w=[ident])
    mtmp = k.sb("mtmp", [128, 128], F32)
    mask_cur = k.sb("mask_cur", [128, 128], BF16)
    mask_prev = k.sb("mask_prev", [128, 128], BF16)
    mask_pfx = k.sb("mask_pfx", [128, 128], BF16)
    P(k, lambda e: e.memset(mtmp[:], 0.0), w=[mtmp])
    P(k, lambda e: e.affine_select(mtmp[:], mtmp[:], pattern=[[1, 128]], compare_op=ALU.is_ge,
                                   fill=NEG, base=0, channel_multiplier=-1), r=[mtmp], w=[mtmp])
    V(k, lambda e: e.tensor_copy(mask_cur[:], mtmp[:]), r=[mtmp], w=[mask_cur])
    P(k, lambda e: e.memset(mtmp[:], 0.0), r=[mtmp], w=[mtmp])
    P(k, lambda e: e.affine_select(mtmp[:], mtmp[:], pattern=[[-1, 128]], compare_op=ALU.is_ge,
                                   fill=NEG, base=0, channel_multiplier=1), r=[mtmp], w=[mtmp])
    V(k, lambda e: e.tensor_copy(mask_prev[:], mtmp[:]), r=[mtmp], w=[mask_prev])
    k.dma('pool', mask_pfx[:], mpfx[:, :], writes=[mask_pfx])
    ones_bf = k.sb("ones_bf", [128, 128], BF16)
    P(k, lambda e: e.memset(ones_bf[:], 1.0), w=[ones_bf])
    blk64 = k.sb("blk64", [128, 128], BF16)
    P(k, lambda e: e.memset(blk64[:], 0.0), w=[blk64])
    P(k, lambda e: e.memset(blk64[0:64, 0:64], 1.0), w=[blk64])
    P(k, lambda e: e.memset(blk64[64:128, 64:128], 1.0), w=[blk64])
    gcol1 = k.sb("gcol1", [128, 8], F32)
    k.dma('sp', gcol1[:], ln1_g.rearrange("(c p) -> p c", p=128), writes=[gcol1], allow_slow_non_contiguous=True)
    mhalf = k.sb("mhalf", [128, 1], F32)
    P(k, lambda e: e.memset(mhalf[:], -0.5), w=[mhalf])

    k.push()
    xnT = k.sb("xnT", [128, 8, T_ALL], BF16)
    hAT = k.sb("hAT", [128, 4, T_OWN], BF16)

    k.push()
    wm = k.sb("wm", [128, 8, 2048], BF16)

    def norm_T(groups, dstT, gcol, tag):
        k.push()
        ntmax = max(nt for _, nt in groups)
        xgs = [k.sb(f"xg{tag}{i}", [128, ntmax, 1024], F32) for i in range(3)]
        xss = [k.sb(f"xs{tag}{i}", [128, 1024], BF16) for i in range(2)]
        junk = k.sb(f"junk{tag}", [128, 1024], BF16)
        items = []
        i = 0
        for gi, (src, nt) in enumerate(groups):
            xg = xgs[gi % 3]
            qn = 'sp' if gi % 2 == 0 else 'act'
            for t in range(nt):
                xs = xss[i % 2]
                bank = 6 + (i % 2)
                ss = k.sb(f"ss{tag}{i}", [128, 1], F32)
                rs = k.sb(f"rs{tag}{i}", [128, 1], F32)

                def s1(xg=xg, t=t, ss=ss, rs=rs, src=src, qn=qn, nt=nt):
                    if t == 0:
                        k.dma(qn, xg[:, 0:nt, :], src, writes=[xg])
                    A(k, lambda e: e.activation(junk[:], xg[:, t, :], AF.Square, accum_out=ss[:]), r=[xg], w=[junk, ss])
                    V(k, lambda e: e.tensor_scalar(ss[:], ss[:], 1.0 / 1024, EPS, ALU.mult, ALU.add), r=[ss], w=[ss])
                    P(k, lambda e: e.tensor_tensor(rs[:], ss[:], mhalf[:], ALU.pow), r=[ss, mhalf], w=[rs])

                def s2(xg=xg, t=t, xs=xs, rs=rs, bank=bank, i=i):
                    A(k, lambda e: e.activation(xs[:], xg[:, t, :], AF.Copy, scale=rs[:, 0:1]), r=[xg, rs], w=[xs])
                    for c in range(8):
                        M(k, lambda e, c=c: e.transpose(pbb[bank][:, c * 128:(c + 1) * 128], xs[:, c * 128:(c + 1) * 128], ident[:]),
                          r=[xs, ident], w=[pb[bank]])
                    V(k, lambda e: e.tensor_tensor(dstT[:, :, i * 128:(i + 1) * 128],
                                                   pbb[bank][:, :].rearrange("p (c t) -> p c t", c=8),
                                                   gcol[:].unsqueeze(2).broadcast_to([128, 8, 128]), ALU.mult),
                      r=[pb[bank], gcol], w=[dstT])
                items.append((s1, s2))
                i += 1
        pipe2(items)
        k.pop()

    groups = [(xsrc[g4 * 512:(g4 + 1) * 512, :].rearrange("(t p) n -> p t n", p=128), 4)
              for xsrc in (x_pre, x_own) for g4 in range(4)]
    norm_T(groups, xnT, gcol1, "a")

    if "xnT" in dbg:
        o = dout("d_xnT", [128, 8, T_ALL])
        k.push()
        t32 = k.sb("dbg32", [128, 8, T_ALL // 2], F32)
        for hh in range(2):
            V(k, lambda e: e.tensor_copy(t32[:], xnT[:, :, hh * 2048:(hh + 1) * 2048]), r=[xnT], w=[t32])
            k.dma('sp', o[:, :, hh * 2048:(hh + 1) * 2048], t32[:], reads=[t32])
        k.pop()

    def finish():
        while k.stack:
            k.pop()
        for e in k.eng:
            k.wait_all(e, k.scope_bufs[0])
        k.barrier()
        k.close()
        return nc, dbg_out, k

    if upto == "A":
        return finish()

    COLS = k.sb("COLS", [128, 32, 3, 4], F32)
    MUB = k.sb("MUB", [128, 33, 4], F32)
    W_end = k.sb("W_end", [128, 32, 4], F32)
    DECAY = k.sb("DECAY", [128, 32, 4], F32)
    EA = k.sb("EA", [128, 32, 4], F32)
    EAb = k.sb("EAb", [128, 32, 4], BF16)
    FLOOR2 = k.sb("FLOOR2", [128, 32, 4], F32)
    k.push()
    RT = k.sb("RT", [128, T_ALL], F32)
    PMA = k.sb("PMA", [128, T_ALL], F32)
    wg = k.sb("wg", [128, 8, 8], BF16)
    gb = k.sb("gb", [128, 2], F32)
    tmpg = k.sb("tmpg", [128, 2, 512], F32)
    sel127 = k.sb("sel127", [128, 128], F32)
    k.dma('pool', wg[:], w_in[:, O_MI:O_MI + 8].rearrange("(c p) n -> p c n", p=128), writes=[wg])
    for c in range(8):
        k.dma('pool', wm[:, c, :], w_in[c * 128:(c + 1) * 128, 0:2048], writes=[wm])
    k.dma('sp', gb[0:4, 0:1], m_i_b.rearrange("(p o) -> p o", o=1), writes=[gb])
    k.dma('sp', gb[64:68, 1:2], m_f_b.rearrange("(p o) -> p o", o=1), writes=[gb])
    k.dma('sp', RT[32:36, :], padd[:, :], writes=[RT])
    k.dma('sp', PMA[64:68, :], pmul[:, :], writes=[PMA])
    V(k, lambda e: e.tensor_scalar(gb[64:68, 1:2], gb[64:68, 1:2], -1.0, None, ALU.mult), r=[gb], w=[gb])
    P(k, lambda e: e.memset(sel127[:], 0.0), w=[sel127])
    P(k, lambda e: e.memset(sel127[96:128, :], 1.0), w=[sel127])
    P(k, lambda e: e.affine_select(sel127[96:128, :], sel127[96:128, :], pattern=[[0, 128]], compare_op=ALU.is_ge,
                                   fill=0.0, base=-31, channel_multiplier=1), r=[sel127], w=[sel127])
    for tg in range(8):
        cs = slice(tg * 512, (tg + 1) * 512)
        bi, bf = pb[(2 * tg) % 6], pb[(2 * tg + 1) % 6]
        for c in range(8):
            M(k, lambda e, c=c: e.matmul(bi[0:4, :], wg[:, c, 0:4], xnT[:, c, cs], start=(c == 0), stop=(c == 7)),
              r=[wg, xnT], w=[bi])
        for c in range(8):
            M(k, lambda e, c=c: e.matmul(bf[64:68, :], wg[:, c, 4:8], xnT[:, c, cs], start=(c == 0), stop=(c == 7),
                                         tile_position=(0, 64)), r=[wg, xnT], w=[bf])
        V(k, lambda e: e.tensor_scalar(RT[0:4, cs], bi[0:4, :], gb[0:4, 0:1], None, ALU.add), r=[bi, gb], w=[RT])
        A(k, lambda e: e.activation(tmpg[64:68, 0, :], bf[64:68, :], AF.Exp, bias=gb[64:68, 1:2], scale=-1.0),
          r=[bf, gb], w=[tmpg])
        A(k, lambda e: e.activation(tmpg[64:68, 1, :], tmpg[64:68, 0, :], AF.Ln, bias=1.0), r=[tmpg], w=[tmpg])
        V(k, lambda e: e.scalar_tensor_tensor(RT[64:68, cs], tmpg[64:68, 1, :], -0.5, PMA[64:68, cs], ALU.mult, ALU.mult),
          r=[tmpg, PMA], w=[RT])
    V(k, lambda e: e.tensor_tensor_scan(PMA[0:4, :], RT[64:68, :], RT[64:68, :], 0.0, ALU.add, ALU.add),
      r=[RT, PMA], w=[PMA])
    V(k, lambda e: e.tensor_tensor(PMA[32:36, :], RT[0:4, :], PMA[0:4, :], ALU.subtract), r=[RT, PMA], w=[PMA])
    V(k, lambda e: e.tensor_tensor(RT[0:4, :], PMA[32:36, :], RT[32:36, :], ALU.add), r=[RT, PMA], w=[RT])
    V(k, lambda e: e.tensor_tensor_scan(RT[32:36, :], RT[0:4, :], RT[0:4, :], 0.0, ALU.max, ALU.max),
      r=[RT], w=[RT])
    V(k, lambda e: e.tensor_copy(RT[64:68, :], PMA[0:4, :]), r=[PMA, RT], w=[RT])
    for q in range(8):
        bank = pb[q % 4]
        for j in range(4):
            c = q * 4 + j
            M(k, lambda e, c=c, j=j: e.transpose(bank[:, j * 128:(j + 1) * 128], RT[:, c * 128:(c + 1) * 128], identf[:]),
              r=[RT, identf], w=[bank])
        V(k, lambda e: e.tensor_copy(COLS[:, q * 4:(q + 1) * 4, :, :],
                                     bank[:, :].rearrange("p (c q r) -> p c q r", c=4, q=4)[:, :, 0:3, 0:4]),
          r=[bank], w=[COLS])
    M(k, lambda e: e.matmul(pb[4][:, 0:384], sel127[:], COLS[:].rearrange("p c q r -> p (c q r)"), start=True, stop=True),
      r=[sel127, COLS], w=[pb[4]])
    P(k, lambda e: e.memset(MUB[:, 0, :], 0.0), w=[MUB])
    V(k, lambda e: e.tensor_copy(MUB[:, 1:33, :], pb[4][:, 0:384].rearrange("p (c q r) -> p c q r", c=32, q=3)[:, :, 1, :]),
      r=[pb[4]], w=[MUB])
    gt = k.sb("gt", [128, 4, 32, 4], F32)
    V(k, lambda e: e.tensor_tensor(gt[:, 0], COLS[:, :, 0, :], MUB[:, 0:32, :], ALU.subtract), r=[MUB, COLS], w=[gt])
    V(k, lambda e: e.tensor_tensor(gt[:, 1], COLS[:, :, 0, :], MUB[:, 1:33, :], ALU.subtract), r=[MUB, COLS], w=[gt])
    V(k, lambda e: e.tensor_tensor(gt[:, 2], MUB[:, 0:32, :], MUB[:, 1:33, :], ALU.subtract), r=[MUB], w=[gt])
    V(k, lambda e: e.tensor_tensor(gt[:, 3], COLS[:, :, 2, :], MUB[:, 0:32, :], ALU.add), r=[COLS, MUB], w=[gt])
    A(k, lambda e: e.activation(EA[:], gt[:, 0], AF.Exp), r=[gt], w=[EA])
    A(k, lambda e: e.activation(W_end[:], gt[:, 1], AF.Exp), r=[gt], w=[W_end])
    A(k, lambda e: e.activation(DECAY[:], gt[:, 2], AF.Exp), r=[gt], w=[DECAY])
    A(k, lambda e: e.activation(FLOOR2[:], gt[:, 3], AF.Exp, scale=-1.0), r=[gt], w=[FLOOR2])
    V(k, lambda e: e.tensor_scalar(W_end[:], W_end[:], 128.0 ** -0.5, None, ALU.mult), r=[W_end], w=[W_end])
    V(k, lambda e: e.tensor_copy(EAb[:], EA[:]), r=[EA], w=[EAb])
    k.pop()

    if "gates" in dbg:
        o = dout("d_cols", [128, 32 * 12])
        k.dma('sp', o[:, :], COLS[:].rearrange("p c q r -> p (c q r)"), reads=[COLS])
        o = dout("d_mub", [128, 33 * 4])
        k.dma('sp', o[:, :], MUB[:].rearrange("p c r -> p (c r)"), reads=[MUB])
        o = dout("d_wend", [128, 32 * 4])
        k.dma('sp', o[:, :], W_end[:].rearrange("p c r -> p (c r)"), reads=[W_end])
    if upto == "B":
        return finish()

    k.push()
    cw = k.sb("cw", [128, 4, 8], F32)
    for tap in range(4):
        k.dma('sp', cw[:, tap, :], m_conv[tap, :].rearrange("(j p) -> p j", p=128), writes=[cw],
              allow_slow_non_contiguous=True)
    dg = k.sb("dg", [128, 8, 4, 128], BF16)
    for j in range(8):
        for tap in range(4):
            V(k, lambda e, j=j, tap=tap: e.tensor_scalar(dg[:, j, tap, :], ident[:], cw[:, tap, j:j + 1], None, ALU.mult),
              r=[ident, cw], w=[dg])
    mask01 = k.sb("mask01", [128, 128], BF16)
    P(k, lambda e: e.memset(mtmp[:], 128.0 ** -0.5), r=[mtmp], w=[mtmp])
    P(k, lambda e: e.affine_select(mtmp[:], mtmp[:], pattern=[[1, 128]], compare_op=ALU.is_ge,
                                   fill=0.0, base=0, channel_multiplier=-1), r=[mtmp], w=[mtmp])
    V(k, lambda e: e.tensor_copy(mask01[:], mtmp[:]), r=[mtmp], w=[mask01])
    mng = k.sb("mng", [128, 512], F32)
    k.dma('sp', mng[:], m_norm_g.partition_broadcast(128), writes=[mng])
    zbs = [k.sb(f"zb{i}", [128, 515], BF16) for i in range(3)]
    carry = k.sb("carry", [128, 8, 3], BF16)
    P(k, lambda e: e.memset(carry[:], 0.0), w=[carry])
    qkT = [k.sb(f"qkT{i}", [128, 8, 512], BF16) for i in range(2)]
    vaug = [k.sb(f"vaug{i}", [128, 4, 128], BF16) for i in range(3)]
    vsc = [k.sb(f"vsc{i}", [128, 4, 128], BF16) for i in range(3)]
    gmo = [k.sb(f"gmo{i}", [128, 512], BF16) for i in range(3)]
    sgt = [k.sb(f"sgt{i}", [128, 512], F32) for i in range(2)]
    kw = [k.sb(f"kw{i}", [128, 4, 128], BF16) for i in range(3)]
    Cst = k.sb("Cst", [128, 4, 128], F32)
    nst = k.sb("nst", [128, 4], F32)
    Cbs = [k.sb(f"Cb{i}", [128, 4, 128], BF16) for i in range(2)]
    nbs = [k.sb(f"nb{i}", [128, 4], BF16) for i in range(2)]
    P(k, lambda e: e.memset(Cst[:], 0.0), w=[Cst])
    P(k, lambda e: e.memset(nst[:], 0.0), w=[nst])
    for i in range(2):
        P(k, lambda e, i=i: e.memset(Cbs[i][:], 0.0), w=[Cbs[i]])
        P(k, lambda e, i=i: e.memset(nbs[i][:], 0.0), w=[nbs[i]])
    DT = [k.sb(f"DT{i}", [128, 4, 128], BF16) for i in range(3)]
    hns = [k.sb(f"hns{i}", [128, 4, 128], F32) for i in range(2)]
    hsq = [k.sb(f"hsq{i}", [128, 4, 128], BF16) for i in range(2)]
    hA = [k.sb(f"hA{i}", [128, 4, 128], BF16) for i in range(2)]
    rr = 0
    Sb, ABb, XB_ = [pb[3], pb[4]], [pb[5], pb[6]], pb[7]

    def gbank():
        nonlocal rr
        b = rr % 3
        rr += 1
        return b

    nz = [0]

    def supertile(st):
        own = st >= 4
        cs = slice(st * 512, (st + 1) * 512)
        qk = qkT[st % 2]
        jlist = list(range(8)) if st >= 3 else [4, 5, 6, 7]
        items = []
        for j in jlist:
            zb = zbs[nz[0] % 3]
            nz[0] += 1

            def s1(j=j, zb=zb):
                b = gbank()
                for c in range(8):
                    M(k, lambda e, c=c: e.matmul(pb[b][:, :], wm[:, c, j * 128:(j + 1) * 128], xnT[:, c, cs],
                                                 start=(c == 0), stop=(c == 7)), r=[wm, xnT], w=[pb[b]])
                P(k, lambda e: e.tensor_copy(zb[:, 0:3], carry[:, j, :]), r=[carry], w=[zb])
                A(k, lambda e: e.activation(zb[:, 3:515], pb[b][:, :], AF.Copy), r=[pb[b]], w=[zb])
                P(k, lambda e: e.tensor_copy(carry[:, j, :], zb[:, 512:515]), r=[zb], w=[carry])

            def s2(j=j, zb=zb):
                if not (own or j >= 4):
                    return
                b2 = gbank()
                for tap in range(4):
                    M(k, lambda e, tap=tap: e.matmul(pb[b2][:, :], dg[:, j, tap, :], zb[:, tap:tap + 512],
                                                     start=(tap == 0), stop=(tap == 3)), r=[dg, zb], w=[pb[b2]])
                A(k, lambda e: e.activation(qk[:, j, :], pb[b2][:, :], AF.Silu), r=[pb[b2]], w=[qk])
            items.append((s1, s2))
        pipe2(items)

    def stageA(c):
        st, ci = c // 4, c % 4
        own = st >= 4
        qk = qkT[st % 2]
        tsl = slice(c * 128, (c + 1) * 128)
        lsl = slice(ci * 128, (ci + 1) * 128)
        va, vs_, kwt = vaug[c % 3], vsc[c % 3], kw[c % 3]
        b = gbank()
        for kk in range(8):
            M(k, lambda e, kk=kk: e.matmul(pb[b][:, :], xnT[:, kk, tsl], wm[:, kk, O_MV:O_MV + 512],
                                           start=(kk == 0), stop=(kk == 7)), r=[wm, xnT], w=[pb[b]])
        A(k, lambda e: e.activation(va[:].rearrange("p h d -> p (h d)"), pb[b][:, :], AF.Copy), r=[pb[b]], w=[va])
        if own:
            V(k, lambda e: e.tensor_tensor(vs_[:], pb[b][:, :].rearrange("p (h d) -> p h d", h=4),
                                           EA[:, c, :].unsqueeze(2).broadcast_to([128, 4, 128]), ALU.mult),
              r=[pb[b], EA], w=[vs_])
        b = gbank()
        for h in range(4):
            M(k, lambda e, h=h: e.transpose(pbb[b][:, h * 128:(h + 1) * 128], qk[:, 4 + h, lsl], ident[:]),
              r=[qk, ident], w=[pb[b]])
        V(k, lambda e: e.tensor_tensor(kwt[:], pbb[b][:, 0:512].rearrange("p (h d) -> p h d", h=4),
                                       W_end[:, c, :].unsqueeze(2).broadcast_to([128, 4, 128]), ALU.mult),
          r=[pb[b], W_end], w=[kwt])
        if own:
            g_, sg = gmo[c % 3], sgt[c % 2]
            b = gbank()
            for kk in range(8):
                M(k, lambda e, kk=kk: e.matmul(pb[b][:, :], xnT[:, kk, tsl], wm[:, kk, O_MO:O_MO + 512],
                                               start=(kk == 0), stop=(kk == 7)), r=[wm, xnT], w=[pb[b]])
            A(k, lambda e: e.activation(sg[:], pb[b][:, :], AF.Sigmoid), r=[pb[b]], w=[sg])
            P(k, lambda e: e.tensor_tensor(g_[:], sg[:], mng[:], ALU.mult), r=[sg, mng], w=[g_])
            S_ = Sb[c % 2]
            for h in range(4):
                M(k, lambda e, h=h: e.matmul(S_[:, h * 128:(h + 1) * 128], qk[:, 4 + h, lsl], qk[:, h, lsl],
                                             start=True, stop=True), r=[qk], w=[S_])
            dt = DT[c % 3]
            V(k, lambda e: e.tensor_tensor(dt[:], S_[:, :].rearrange("p (h d) -> p h d", h=4),
                                           mask01[:].unsqueeze(1).broadcast_to([128, 4, 128]), ALU.mult),
              r=[S_, mask01], w=[dt])

    def stageB(c):
        st, ci = c // 4, c % 4
        own = st >= 4
        qk = qkT[st % 2]
        lsl = slice(ci * 128, (ci + 1) * 128)
        va, vs_, kwt = vaug[c % 3], vsc[c % 3], kw[c % 3]
        Cb, nb_ = Cbs[c % 2], nbs[c % 2]
        b = gbank()
        for h in range(4):
            M(k, lambda e, h=h: e.matmul(pb[b][:, h * 128:(h + 1) * 128], kwt[:, h, :], va[:, h, :],
                                         start=True, stop=True), r=[kwt, va], w=[pb[b]])
        for h in range(4):
            M(k, lambda e, h=h: e.matmul(XB_[:, 8 + h:9 + h], kwt[:, h, :], ones_bf[:, 0:1],
                                         start=True, stop=True), r=[kwt, ones_bf], w=[XB_])
        if own:
            dt = DT[c % 3]
            AB_ = ABb[c % 2]
            xo = (c % 2) * 4
            for h in range(4):
                M(k, lambda e, h=h: e.matmul(AB_[:, h * 128:(h + 1) * 128], dt[:, h, :], vs_[:, h, :],
                                             start=True, stop=False), r=[dt, vs_], w=[AB_])
                M(k, lambda e, h=h: e.matmul(AB_[:, h * 128:(h + 1) * 128], qk[:, h, lsl], Cb[:, h, :],
                                             start=False, stop=True), r=[qk, Cb], w=[AB_])
            for h in range(4):
                M(k, lambda e, h=h: e.matmul(XB_[:, xo + h:xo + h + 1], dt[:, h, :], EAb[:, c, h:h + 1],
                                             start=True, stop=False), r=[dt, EAb], w=[XB_])
                M(k, lambda e, h=h: e.matmul(XB_[:, xo + h:xo + h + 1], qk[:, h, lsl], nb_[:, h:h + 1],
                                             start=False, stop=True), r=[qk, nb_], w=[XB_])
        V(k, lambda e: e.tensor_tensor(Cst[:], Cst[:], DECAY[:, c, :].unsqueeze(2).broadcast_to([128, 4, 128]), ALU.mult),
          r=[Cst, DECAY], w=[Cst])
        V(k, lambda e: e.tensor_tensor(Cst[:].rearrange("p h d -> p (h d)"), Cst[:].rearrange("p h d -> p (h d)"),
                                       pb[b][:, :], ALU.add), r=[Cst, pb[b]], w=[Cst])
        V(k, lambda e: e.tensor_tensor(nst[:], nst[:], DECAY[:, c, :], ALU.mult), r=[nst, DECAY], w=[nst])
        V(k, lambda e: e.tensor_tensor(nst[:], nst[:], XB_[:, 8:12], ALU.add), r=[nst, XB_], w=[nst])
        if c >= 15 and c < 31:
            Cn, nn = Cbs[(c + 1) % 2], nbs[(c + 1) % 2]
            A(k, lambda e: e.activation(Cn[:], Cst[:], AF.Copy), r=[Cst], w=[Cn])
            A(k, lambda e: e.activation(nn[:], nst[:], AF.Copy), r=[nst], w=[nn])
        if own:
            sm = k.sb(f"sm{c}", [128, 8, 4], F32)
            V(k, lambda e: e.tensor_copy(sm[:, 0, :], XB_[:, xo:xo + 4]), r=[XB_], w=[sm])
            return sm
        return None

    def stageN(c, sm):
        oc = c - 16
        g_ = gmo[c % 3]
        AB_ = ABb[c % 2]
        V(k, lambda e: e.scalar_tensor_tensor(sm[:, 1, :], sm[:, 0, :], -1.0, sm[:, 0, :], ALU.mult, ALU.max), r=[sm], w=[sm])
        V(k, lambda e: e.tensor_tensor(sm[:, 1, :], sm[:, 1, :], FLOOR2[:, c, :], ALU.max), r=[sm, FLOOR2], w=[sm])
        V(k, lambda e: e.reciprocal(sm[:, 2, :], sm[:, 1, :]), r=[sm], w=[sm])
        hq = hsq[c % 2]
        A(k, lambda e: e.activation(hq[:].rearrange("p h d -> p (h d)"), AB_[:, :], AF.Square), r=[AB_], w=[hq])
        V(k, lambda e: e.tensor_reduce(sm[:, 3, :], hq[:], AX.X, ALU.add), r=[hq], w=[sm])
        V(k, lambda e: e.tensor_tensor(sm[:, 4, :], sm[:, 2, :], sm[:, 2, :], ALU.mult), r=[sm], w=[sm])
        V(k, lambda e: e.tensor_tensor(sm[:, 4, :], sm[:, 4, :], sm[:, 3, :], ALU.mult), r=[sm], w=[sm])
        V(k, lambda e: e.tensor_scalar(sm[:, 4, :], sm[:, 4, :], 1.0 / 128, EPS, ALU.mult, ALU.add), r=[sm], w=[sm])
        P(k, lambda e: e.tensor_tensor(sm[:, 5, :], sm[:, 4, :], mhalf[:].broadcast_to([128, 4]), ALU.pow),
          r=[sm, mhalf], w=[sm])
        V(k, lambda e: e.tensor_tensor(sm[:, 6, :], sm[:, 5, :], sm[:, 2, :], ALU.mult), r=[sm], w=[sm])
        hn, ha = hns[c % 2], hA[c % 2]
        V(k, lambda e: e.tensor_tensor(hn[:], AB_[:, :].rearrange("p (h d) -> p h d", h=4),
                                       sm[:, 6, :].unsqueeze(2).broadcast_to([128, 4, 128]), ALU.mult),
          r=[AB_, sm], w=[hn])
        P(k, lambda e: e.tensor_tensor(ha[:].rearrange("p h d -> p (h d)"), hn[:].rearrange("p h d -> p (h d)"),
                                       g_[:], ALU.mult), r=[hn, g_], w=[ha])

    def stageT(c):
        oc = c - 16
        ha = hA[c % 2]
        b = gbank()
        for h in range(4):
            M(k, lambda e, h=h: e.transpose(pbb[b][:, h * 128:(h + 1) * 128], ha[:, h, :], ident[:]),
              r=[ha, ident], w=[pb[b]])
        A(k, lambda e: e.activation(hAT[:, :, oc * 128:(oc + 1) * 128],
                                    pbb[b][:, 0:512].rearrange("p (h d) -> p h d", h=4), AF.Copy),
          r=[pb[b]], w=[hAT])

    supertile(0)
    stageA(0)
    pendN = None
    pendT = None
    for c in range(32):
        if c % 4 == 2 and c // 4 + 1 < 8:
            supertile(c // 4 + 1)
        if c + 1 < 32:
            stageA(c + 1)
        sm = stageB(c)
        if pendT is not None:
            stageT(pendT)
            pendT = None
        if pendN is not None:
            stageN(*pendN)
            pendT = pendN[0]
        pendN = (c, sm) if sm is not None else None
    stageT(pendT)
    stageN(*pendN)
    stageT(pendN[0])
    k.pop()
    k.pop()
    hBT = k.sb("hBT", [128, 2, T_OWN], BF16)

    if "hA" in dbg:
        o = dout("d_hAT", [128, 4, T_OWN])
        k.push()
        t32 = k.sb("dbg32b", [128, 4, T_OWN], F32)
        V(k, lambda e: e.tensor_copy(t32[:], hAT[:]), r=[hAT], w=[t32])
        k.dma('sp', o[:, :, :], t32[:], reads=[t32])
        k.pop()
    if upto == "C":
        return finish()

    rr2 = [0]

    def gb3():
        b = rr2[0] % 3
        rr2[0] += 1
        return b

    fm_ctr = [0]
    FM_ACC = [0, 1, 2, 4, 5, 6]
    FM_SSQ = [3, 7]

    def fm_norm(wsel, rhs_sel, n, summat, inv_dim, gcol, dst, dstbuf, rbufs, scr3, outs=None, vw=None, gbuf=None):
        idx = fm_ctr[0]
        fm_ctr[0] += 1
        b = FM_ACC[idx % 6]
        sb_ = FM_SSQ[idx % 2]
        sq, ms, rs = scr3[idx % 3]
        if outs is None:
            outs = [(dst, slice(0, 128))]
        if vw is None:
            vw = lambda ap: ap

        def s1():
            for c in range(8):
                M(k, lambda e, c=c: e.matmul(pb[b][:, 0:n], wsel(c), rhs_sel(c), start=(c == 0), stop=(c == 7)),
                  r=rbufs, w=[pb[b]])
            A(k, lambda e: e.activation(sq[:, 0:n], pb[b][:, 0:n], AF.Square), r=[pb[b]], w=[sq])

        def s2():
            M(k, lambda e: e.matmul(pb[sb_][:, 0:n], summat, sq[:, 0:n], start=True, stop=True), r=[sq], w=[pb[sb_]])
            A(k, lambda e: e.activation(ms[:, 0:n], pb[sb_][:, 0:n], AF.Ln, bias=EPS, scale=inv_dim), r=[pb[sb_]], w=[ms])
            A(k, lambda e: e.activation(rs[:, 0:n], ms[:, 0:n], AF.Exp, scale=-0.5), r=[ms], w=[rs])

        def s3():
            for (d_ap, psl) in outs:
                V(k, lambda e: e.scalar_tensor_tensor(d_ap, vw(pb[b][psl, 0:n]), gcol[psl], vw(rs[psl, 0:n]), ALU.mult, ALU.mult),
                  r=[pb[b], rs, gbuf], w=[dstbuf])
        return (s1, s2, s3)

    k.push()
    DIL = [1, 4, 16]
    wq3 = [k.sb(f"wdil{i}", [128, 8, 256], BF16) for i in range(3)]

    def load_dil(g, which):
        off = (O_DQ, O_DK, O_DV)[which]
        k.dma('pool', wq3[which][:], w_in[:, off + g * 256: off + (g + 1) * 256].rearrange("(c p) n -> p c n", p=128),
              writes=[wq3[which]])

    for which in range(3):
        load_dil(0, which)
    qpad = k.sb("qpad", [128, 2, 2, T_OWN], BF16)
    kTg = k.sb("kTg", [128, 2, T_ALL], BF16)
    vt = k.sb("vt", [128, 32, 256], BF16)
    accB = k.sb("accB", [128, 4, T_OWN], F32)
    gq2 = k.sb("gq2", [128, 3, 2], F32)
    P(k, lambda e: e.memset(qpad[64:128, :, 0, :], 0.0), w=[qpad])
    P(k, lambda e: e.memset(qpad[0:64, :, 1, :], 0.0), w=[qpad])
    mpfx01 = k.sb("mpfx01", [128, 128], BF16)
    k.dma('sp', mtmp[:], mpfx[:, :], writes=[mtmp])
    V(k, lambda e: e.tensor_scalar(mpfx01[:], mtmp[:], -1.0, None, ALU.is_ge), r=[mtmp], w=[mpfx01])
    for g in range(3):
        for hf in range(2):
            k.dma('sp', gq2[hf * 64:(hf + 1) * 64, g, 0:1], dil_q_g[g, :].rearrange("(p o) -> p o", o=1), writes=[gq2])
            k.dma('sp', gq2[hf * 64:(hf + 1) * 64, g, 1:2], dil_k_g[g, :].rearrange("(p o) -> p o", o=1), writes=[gq2])
    V(k, lambda e: e.tensor_scalar(gq2[:, :, 0:1], gq2[:, :, 0:1], 0.125, None, ALU.mult), r=[gq2], w=[gq2])
    scrs = [(k.sb(f"sq{i}", [128, 512], BF16), k.sb(f"ms{i}", [128, 512], F32), k.sb(f"rs{i}", [128, 512], F32))
            for i in range(3)]
    Pcs = [k.sb(f"Pc{i}", [128, 4, 128], BF16) for i in range(3)]
    Pps = [k.sb(f"Pp{i}", [128, 4, 128], BF16) for i in range(3)]
    nfm = 0
    ntile = 0
    for g in range(3):
        r_ = DIL[g]
        nblk = 16 // r_
        NK, NQ = T_ALL // r_, T_OWN // r_
        ktgs = [4, 5, 6, 7] + ([3] if g < 2 else [0, 1, 2, 3])
        if r_ == 1:
            vwn = (lambda ap: ap)
            pv = (lambda ap, a0: ap[:, a0:a0 + 512])
        else:
            vwn = (lambda ap, r_=r_: ap.rearrange("p (a r) -> p r a", r=r_))
            pv = (lambda ap, a0, r_=r_: ap.rearrange("p (r a) -> p r a", r=r_)[:, :, a0:a0 + 512 // r_])
        fitems = []
        for tg in ktgs:
            cs = slice(tg * 512, (tg + 1) * 512)
            for j in range(2):
                if tg >= 4:
                    a0 = (tg - 4) * 512 // r_
                    outs = [(pv(qpad[pp * 64:(pp + 1) * 64, j, pp, :], a0), slice(pp * 64, (pp + 1) * 64)) for pp in range(2)]
                    fitems.append(fm_norm(lambda c, j=j: wq3[0][:, c, j * 128:(j + 1) * 128], lambda c, cs=cs: xnT[:, c, cs], 512, blk64[:],
                                          1.0 / 64, gq2[:, g, 0:1], None, qpad, [xnT, wq3[0]], scrs, outs=outs, vw=vwn, gbuf=gq2))
                    nfm += 1
                a0 = tg * 512 // r_
                outs = [(pv(kTg[:, j, :], a0), slice(0, 128))]
                fitems.append(fm_norm(lambda c, j=j: wq3[1][:, c, j * 128:(j + 1) * 128], lambda c, cs=cs: xnT[:, c, cs], 512, blk64[:],
                                      1.0 / 64, gq2[:, g, 1:2], None, kTg, [xnT, wq3[1]], scrs, outs=outs, vw=vwn, gbuf=gq2))
                nfm += 1
        pipeN(fitems)
        if g + 1 < 3:
            load_dil(g + 1, 0)
            load_dil(g + 1, 1)
        vtiles = []
        for blk in range(nblk):
            for res in range(r_):
                lo = 2048 + blk * 128 * r_ + res
                vtiles.append((blk * r_ + res, slice(lo, lo + 127 * r_ + 1, r_)))
        for res in range(r_):
            lo = 2048 - 128 * r_ + res
            vtiles.append((16 + res, slice(lo, lo + 127 * r_ + 1, r_)))
        for vidx, tsl in vtiles:
            b = gb3()
            for c in range(8):
                M(k, lambda e, c=c: e.matmul(pb[b][:, 0:256], xnT[:, c, tsl], wq3[2][:, c, :], start=(c == 0), stop=(c == 7)),
                  r=[xnT, wq3[2]], w=[pb[b]])
            A(k, lambda e: e.activation(vt[:, vidx, :], pb[b][:, 0:256], AF.Copy), r=[pb[b]], w=[vt])
        if g + 1 < 3:
            load_dil(g + 1, 2)
        aitems = []
        for blk in range(nblk):
            for res in range(r_):
                own_lo = blk * 128 * r_ + res
                qs = slice(own_lo, own_lo + 127 * r_ + 1, r_)
                qp = res * NQ + blk * 128
                kc = res * NK + (2048 + blk * 128 * r_) // r_
                kp = kc - 128
                if blk > 0:
                    vprev = (blk - 1) * r_ + res
                else:
                    vprev = 16 + res
                vcur = blk * r_ + res
                SC, SP = pb[(4, 5, 0)[ntile % 3]], pb[(2, 3, 1)[ntile % 3]]
                Pc, Pp = Pcs[ntile % 3], Pps[ntile % 3]
                ob = pb[6 + ntile % 2]

                def s1(SC=SC, SP=SP, Pc=Pc, Pp=Pp, qp=qp, kc=kc, kp=kp, blk=blk):
                    for (bank, k0) in ((SC, kc), (SP, kp)):
                        for j in range(2):
                            for pp in range(2):
                                h = 2 * j + pp
                                M(k, lambda e, h=h, j=j, pp=pp: e.matmul(bank[:, h * 128:(h + 1) * 128], kTg[:, j, k0:k0 + 128],
                                                                        qpad[:, j, pp, qp:qp + 128], start=True, stop=True),
                                  r=[kTg, qpad], w=[bank])
                    A(k, lambda e: e.activation(Pc[:].rearrange("p h t -> p (h t)"), SC[:, :], AF.Exp), r=[SC], w=[Pc])
                    A(k, lambda e: e.activation(Pp[:].rearrange("p h t -> p (h t)"), SP[:, :], AF.Exp), r=[SP], w=[Pp])
                    P(k, lambda e: e.affine_select(Pc[:], Pc[:], pattern=[[0, 4], [1, 128]], compare_op=ALU.is_ge,
                                                   fill=0.0, base=0, channel_multiplier=-1), r=[Pc], w=[Pc])
                    if blk > 0:
                        P(k, lambda e: e.affine_select(Pp[:], Pp[:], pattern=[[0, 4], [-1, 128]], compare_op=ALU.is_ge,
                                                       fill=0.0, base=0, channel_multiplier=1), r=[Pp], w=[Pp])
                    else:
                        P(k, lambda e: e.tensor_tensor(Pp[:], Pp[:], mpfx01[:].unsqueeze(1).broadcast_to([128, 4, 128]), ALU.mult),
                          r=[Pp, mpfx01], w=[Pp])

                def s2(ob=ob, Pc=Pc, Pp=Pp, vprev=vprev, vcur=vcur, qs=qs, g=g):
                    for h in range(4):
                        j, pbs = h // 2, 64 * (h % 2)
                        M(k, lambda e, h=h, j=j, pbs=pbs: e.matmul(ob[pbs:pbs + 64, j * 128:(j + 1) * 128], vt[:, vprev, h * 64:(h + 1) * 64],
                                                                  Pp[:, h, :], start=True, stop=False,
                                                                  tile_position=(0, pbs)), r=[vt, Pp], w=[ob])
                        M(k, lambda e, h=h, j=j, pbs=pbs: e.matmul(ob[pbs:pbs + 64, j * 128:(j + 1) * 128], vt[:, vcur, h * 64:(h + 1) * 64],
                                                                  Pc[:, h, :], start=False, stop=True,
                                                                  tile_position=(0, pbs)), r=[vt, Pc], w=[ob])
                    for h in range(4):
                        j, pbs = h // 2, 64 * (h % 2)
                        M(k, lambda e, h=h, j=j, pbs=pbs: e.matmul(ob[pbs:pbs + 64, 256 + j * 128:256 + (j + 1) * 128], ones_bf[:, 0:64],
                                                                  Pp[:, h, :], start=True, stop=False,
                                                                  tile_position=(0, pbs)), r=[ones_bf, Pp], w=[ob])
                        M(k, lambda e, h=h, j=j, pbs=pbs: e.matmul(ob[pbs:pbs + 64, 256 + j * 128:256 + (j + 1) * 128], ones_bf[:, 0:64],
                                                                  Pc[:, h, :], start=False, stop=True,
                                                                  tile_position=(0, pbs)), r=[ones_bf, Pc], w=[ob])
                    obv = ob[:, :].rearrange("p (s t) -> p s t", s=4)
                    if g == 0:
                        V(k, lambda e: e.tensor_copy(accB[:, :, qs], obv), r=[ob], w=[accB])
                    else:
                        V(k, lambda e: e.tensor_tensor(accB[:, :, qs], obv, accB[:, :, qs], ALU.add), r=[ob, accB], w=[accB])
                aitems.append((s1, (lambda: None), s2))
                ntile += 1
        pipeN(aitems)
    A(k, lambda e: e.activation(accB[:, 2:4, :], accB[:, 2:4, :], AF.Ln), r=[accB], w=[accB])
    A(k, lambda e: e.activation(accB[:, 2:4, :], accB[:, 2:4, :], AF.Exp, scale=-1.0), r=[accB], w=[accB])
    V(k, lambda e: e.tensor_tensor(hBT[:], accB[:, 0:2, :], accB[:, 2:4, :], ALU.mult), r=[accB], w=[hBT])
    k.pop()

    if "hB" in dbg:
        o = dout("d_hBT", [128, 2, T_OWN])
        k.push()
        t32 = k.sb("dbg32c", [128, 2, T_OWN], F32)
        V(k, lambda e: e.tensor_copy(t32[:], hBT[:]), r=[hBT], w=[t32])
        k.dma('sp', o[:, :, :], t32[:], reads=[t32])
        k.pop()
    if upto == "D":
        return finish()

    hCT = k.sb("hCT", [128, 4, T_OWN], BF16)
    k.push()
    gcolm = k.sb("gcolm", [128, 8], F32)
    k.dma('sp', gcolm[:], mem_norm_g.rearrange("(c p) -> p c", p=128), writes=[gcolm], allow_slow_non_contiguous=True)
    mnT = k.sb("mnT", [128, 8, 256], BF16)
    norm_T([(mem.rearrange("(t p) n -> p t n", p=128), 2)], mnT, gcolm, "m")
    wkv = k.sb("wkv", [128, 8, 1024], BF16)
    for c in range(8):
        k.dma('pool', wkv[:, c, :], w_mem_kv[c * 128:(c + 1) * 128, :], writes=[wkv])
    wxq = k.sb("wxq", [128, 8, 512], BF16)
    k.dma('pool', wxq[:], w_in[:, O_XQ:O_XQ + 512].rearrange("(c p) n -> p c n", p=128), writes=[wxq])
    gx = k.sb("gx", [128, 2], F32)
    k.dma('sp', gx[:, 0:1], x_q_g.rearrange("(p o) -> p o", o=1), writes=[gx])
    k.dma('sp', gx[:, 1:2], x_k_g.rearrange("(p o) -> p o", o=1), writes=[gx])
    V(k, lambda e: e.tensor_scalar(gx[:, 0:1], gx[:, 0:1], 128.0 ** -0.5, None, ALU.mult), r=[gx], w=[gx])
    kmT = k.sb("kmT", [128, 4, 256], BF16)
    vm = k.sb("vm", [128, 2, 512], BF16)
    xqT = k.sb("xqT", [128, 4, T_OWN], BF16)
    scrs = [(k.sb(f"sqe{i}", [128, 512], BF16), k.sb(f"mse{i}", [128, 512], F32), k.sb(f"rse{i}", [128, 512], F32))
            for i in range(3)]
    nfm = 0
    fitems = []
    for h in range(4):
        fitems.append(fm_norm(lambda c, h=h: wkv[:, c, h * 128:(h + 1) * 128], lambda c: mnT[:, c, :], 256, ones_bf[:], 1.0 / 128,
                              gx[:, 1:2], kmT[:, h, :], kmT, [mnT, wkv], scrs, gbuf=gx))
        nfm += 1
    pipeN(fitems)
    for mt in range(2):
        b = gb3()
        for c in range(8):
            M(k, lambda e, c=c: e.matmul(pb[b][:, :], mnT[:, c, mt * 128:(mt + 1) * 128], wkv[:, c, 512:1024],
                                         start=(c == 0), stop=(c == 7)), r=[mnT, wkv], w=[pb[b]])
        A(k, lambda e: e.activation(vm[:, mt, :], pb[b][:, :], AF.Copy), r=[pb[b]], w=[vm])
    fitems = []
    for tg in range(4):
        cs = slice(2048 + tg * 512, 2048 + (tg + 1) * 512)
        for h in range(4):
            fitems.append(fm_norm(lambda c, h=h: wxq[:, c, h * 128:(h + 1) * 128], lambda c, cs=cs: xnT[:, c, cs], 512, ones_bf[:], 1.0 / 128,
                                  gx[:, 0:1], xqT[:, h, tg * 512:(tg + 1) * 512], xqT, [xnT, wxq], scrs, gbuf=gx))
            nfm += 1
    pipeN(fitems)
    Pm = [[k.sb(f"Pm{i}{mt}", [128, 512], BF16) for mt in range(2)] for i in range(3)]
    rdn = [k.sb(f"rdn{i}", [128, 512], F32) for i in range(2)]
    it = 0
    eitems = []
    for tg in range(4):
        ts_ = slice(tg * 512, (tg + 1) * 512)
        for h in range(4):
            sbs = ((pb[4], pb[5]), (pb[2], pb[3]), (pb[0], pb[1]))[it % 3]
            Pmi = Pm[it % 3]
            rd = rdn[it % 2]

            def s1(h=h, ts_=ts_, sbs=sbs, Pmi=Pmi):
                for mt in range(2):
                    sb_ = sbs[mt]
                    M(k, lambda e: e.matmul(sb_[:, :], kmT[:, h, mt * 128:(mt + 1) * 128], xqT[:, h, ts_], start=True, stop=True),
                      r=[kmT, xqT], w=[sb_])
                    A(k, lambda e: e.activation(Pmi[mt][:], sb_[:, :], AF.Exp), r=[sb_], w=[Pmi[mt]])

            def s2(h=h, ts_=ts_, Pmi=Pmi, rd=rd):
                nb6, db7 = pb[6], pb[7]
                for mt in range(2):
                    M(k, lambda e: e.matmul(nb6[:, :], vm[:, mt, h * 128:(h + 1) * 128], Pmi[mt][:], start=(mt == 0), stop=(mt == 1)),
                      r=[vm, Pmi[mt]], w=[nb6])
                for mt in range(2):
                    M(k, lambda e: e.matmul(db7[:, :], ones_bf[:], Pmi[mt][:], start=(mt == 0), stop=(mt == 1)),
                      r=[ones_bf, Pmi[mt]], w=[db7])
                A(k, lambda e: e.activation(rd[:], db7[:, :], AF.Ln), r=[db7], w=[rd])
                A(k, lambda e: e.activation(rd[:], rd[:], AF.Exp, scale=-1.0), r=[rd], w=[rd])
                V(k, lambda e: e.tensor_tensor(hCT[:, h, ts_], nb6[:, :], rd[:], ALU.mult), r=[nb6, rd], w=[hCT])
            eitems.append((s1, (lambda: None), s2))
            it += 1
    pipeN(eitems)
    k.pop()

    if "hC" in dbg:
        o = dout("d_hCT", [128, 4, T_OWN])
        k.push()
        t32 = k.sb("dbg32d", [128, 4, T_OWN], F32)
        V(k, lambda e: e.tensor_copy(t32[:], hCT[:]), r=[hCT], w=[t32])
        k.dma('sp', o[:, :, :], t32[:], reads=[t32])
        k.pop()
    if upto == "E":
        return finish()

    MIXT_OFF = 195584
    OUT_OFF = 130048
    mixT = k.sb_at("mixT", [128, 8, T_OWN], BF16, MIXT_OFF)
    k.limit = MIXT_OFF
    rr8 = [0]

    def nbk():
        b = pb[rr8[0] % 8]
        rr8[0] += 1
        return b

    k.push()
    bg = k.sb("bg", [128, 3, 8], F32)
    for b in range(3):
        k.dma('sp', bg[:, b, :], b_gate[b * 1024:(b + 1) * 1024].rearrange("(j p) -> p j", p=128), writes=[bg],
              allow_slow_non_contiguous=True)
    wgj = [k.sb(f"wgj{i}", [128, 8, 3, 128], BF16) for i in range(3)]
    waj = [k.sb(f"waj{i}", [128, 4, 128], BF16) for i in range(3)]
    wbj = [k.sb(f"wbj{i}", [128, 2, 128], BF16) for i in range(3)]
    wcj = [k.sb(f"wcj{i}", [128, 4, 128], BF16) for i in range(3)]

    def loadF(j):
        w = j % 3
        for b in range(3):
            k.dma('pool', wgj[w][:, :, b, :], w_gate[:, b * 1024 + j * 128:b * 1024 + (j + 1) * 128].rearrange("(c p) n -> p c n", p=128),
                  writes=[wgj[w]])
        k.dma('pool', waj[w][:], w_a[:, j * 128:(j + 1) * 128].rearrange("(c p) n -> p c n", p=128), writes=[waj[w]])
        k.dma('pool', wbj[w][:], w_b[:, j * 128:(j + 1) * 128].rearrange("(c p) n -> p c n", p=128), writes=[wbj[w]])
        k.dma('pool', wcj[w][:], w_c[:, j * 128:(j + 1) * 128].rearrange("(c p) n -> p c n", p=128), writes=[wcj[w]])

    loadF(0)
    loadF(1)
    sig = [k.sb(f"sig{i}", [128, 3, 512], F32) for i in range(2)]
    mm_ = [k.sb(f"mm{i}", [128, 3, 512], F32) for i in range(2)]
    for j in range(8):
        w = j % 3
        wg_ = wgj[w]
        for tg in range(4):
            it = j * 4 + tg
            sg, m_ = sig[it % 2], mm_[it % 2]
            cs = slice(2048 + tg * 512, 2048 + (tg + 1) * 512)
            ts_ = slice(tg * 512, (tg + 1) * 512)
            for b in range(3):
                bank = nbk()
                for c in range(8):
                    M(k, lambda e, c=c: e.matmul(bank[:, :], wg_[:, c, b, :], xnT[:, c, cs],
                                                 start=(c == 0), stop=(c == 7)), r=[wg_, xnT], w=[bank])
                A(k, lambda e: e.activation(sg[:, b, :], bank[:, :], AF.Sigmoid, bias=bg[:, b, j:j + 1]), r=[bank, bg], w=[sg])
            for (b, wt, hT_, nk) in ((0, waj[w], hAT, 4), (1, wbj[w], hBT, 2), (2, wcj[w], hCT, 4)):
                bank = nbk()
                for c in range(nk):
                    M(k, lambda e, c=c: e.matmul(bank[:, :], wt[:, c, :], hT_[:, c, ts_], start=(c == 0), stop=(c == nk - 1)),
                      r=[wt, hT_], w=[bank])
                V(k, lambda e: e.tensor_tensor(m_[:, b, :], bank[:, :], sg[:, b, :], ALU.mult), r=[bank, sg], w=[m_])
            P(k, lambda e: e.tensor_tensor(m_[:, 0, :], m_[:, 0, :], m_[:, 1, :], ALU.add), r=[m_], w=[m_])
            P(k, lambda e: e.tensor_tensor(mixT[:, j, ts_], m_[:, 0, :], m_[:, 2, :], ALU.add), r=[m_], w=[mixT])
            if tg == 0 and j + 2 < 8:
                loadF(j + 2)
    k.pop()
    k.pop()

    out_acc = k.sb_at("out_acc", [128, 16, 1024], F32, OUT_OFF)
    k.limit = OUT_OFF
    k.push()
    oacc = [k.view(out_acc, f"oacc{i}") for i in range(16)]
    hnT = k.sb("hnT", [128, 8, T_OWN], BF16)
    combT = k.sb("combT", [32, T_OWN], BF16)
    k.push()
    wo = k.sb("wo", [128, 8, 1024], BF16)
    for c in range(8):
        k.dma('pool', wo[:, c, :], w_out[c * 128:(c + 1) * 128, :], writes=[wo])
    xrs = [k.sb(f"xr{i}", [128, 1024], F32) for i in range(3)]

    def outproj(i):
        xt = xrs[i % 3]
        k.dma('sp', xt[:], x_own[i * 128:(i + 1) * 128, :], writes=[xt])
        for hf in range(2):
            bank = nbk()
            for c in range(8):
                M(k, lambda e, c=c: e.matmul(bank[:, :], mixT[:, c, i * 128:(i + 1) * 128], wo[:, c, hf * 512:(hf + 1) * 512],
                                             start=(c == 0), stop=(c == 7)), r=[mixT, wo], w=[bank])
            V(k, lambda e: e.tensor_tensor(out_acc[:, i, hf * 512:(hf + 1) * 512], bank[:, :], xt[:, hf * 512:(hf + 1) * 512], ALU.add),
              r=[bank, xt], w=[oacc[i]])

    gcol2 = k.sb("gcol2", [128, 8], F32)
    k.dma('sp', gcol2[:], ln2_g.rearrange("(c p) -> p c", p=128), writes=[gcol2], allow_slow_non_contiguous=True)
    wr32 = k.sb("wr32", [128, 8, 36], F32)
    k.dma('sp', wr32[:, :, 0:4], w_grp.rearrange("(c p) n -> p c n", p=128), writes=[wr32])
    for g in range(4):
        k.dma('sp', wr32[:, :, 4 + 8 * g:12 + 8 * g], w_er[g, :, :].rearrange("(c p) n -> p c n", p=128), writes=[wr32])
    brow = k.sb("brow", [128, 36], F32)
    k.dma('sp', brow[:, 0:4], b_grp.partition_broadcast(128), writes=[brow])
    k.dma('sp', brow[:, 4:36], b_er.partition_broadcast(128), writes=[brow])
    LG = k.sb("LG", [128, 16, 36], F32)
    hss = [k.sb(f"hs{i}", [128, 1024], F32) for i in range(2)]
    hn32 = [k.sb(f"hn32T{i}", [128, 8, 128], F32) for i in range(2)]
    junk2 = k.sb("junk2", [128, 1024], BF16)
    gitems = []
    for i in range(16):
        hs, h32 = hss[i % 2], hn32[i % 2]
        ss = k.sb(f"ssg{i}", [128, 1], F32)
        rs = k.sb(f"rsg{i}", [128, 1], F32)

        def s1(i=i, hs=hs, ss=ss, rs=rs):
            A(k, lambda e: e.activation(junk2[:], out_acc[:, i, :], AF.Square, accum_out=ss[:]), r=[oacc[i]], w=[junk2, ss])
            V(k, lambda e: e.tensor_scalar(ss[:], ss[:], 1.0 / 1024, EPS, ALU.mult, ALU.add), r=[ss], w=[ss])
            P(k, lambda e: e.tensor_tensor(rs[:], ss[:], mhalf[:], ALU.pow), r=[ss, mhalf], w=[rs])
            A(k, lambda e: e.activation(hs[:], out_acc[:, i, :], AF.Copy, scale=rs[:, 0:1]), r=[oacc[i], rs], w=[hs])

        def s2(i=i, hs=hs, h32=h32):
            for q in range(2):
                bank = nbk()
                for c in range(4):
                    M(k, lambda e, c=c: e.transpose(bank[:, c * 128:(c + 1) * 128], hs[:, (4 * q + c) * 128:(4 * q + c + 1) * 128], identf[:]),
                      r=[hs, identf], w=[bank])
                bv = bank[:, :].rearrange("p (c t) -> p c t", c=4)
                gb_ = gcol2[:, 4 * q:4 * q + 4].unsqueeze(2).broadcast_to([128, 4, 128])
                V(k, lambda e: e.tensor_tensor(hnT[:, 4 * q:4 * q + 4, i * 128:(i + 1) * 128], bv, gb_, ALU.mult),
                  r=[bank, gcol2], w=[hnT])
                V(k, lambda e: e.tensor_tensor(h32[:, 4 * q:4 * q + 4, :], bv, gb_, ALU.mult), r=[bank, gcol2], w=[h32])

        def s3(i=i, h32=h32):
            bank = nbk()
            for c in range(8):
                M(k, lambda e, c=c: e.matmul(bank[:, 0:36], h32[:, c, :], wr32[:, c, :], start=(c == 0), stop=(c == 7)),
                  r=[h32, wr32], w=[bank])
            V(k, lambda e: e.tensor_tensor(LG[:, i, :], bank[:, 0:36], brow[:], ALU.add), r=[bank, brow], w=[LG])
        gitems.append((s1, s2, s3))
    outproj(0)
    outproj(1)
    for i in range(16 + 2):
        if i + 2 < 16:
            outproj(i + 2)
        if i < 16:
            gitems[i][0]()
        if 0 <= i - 1 < 16:
            gitems[i - 1][1]()
        if 0 <= i - 2 < 16:
            gitems[i - 2][2]()
    R = k.sb("R", [128, 16, 80], F32)
    T4 = k.sb("T4", [128, 16, 4, 8], F32)
    comb = k.sb("comb", [128, 16, 4, 8], F32)
    lg = LG[:, :, 0:4]
    le = LG[:, :, 4:36].rearrange("p t (g e) -> p t g e", g=4)
    gmax, gs, gw, v1, v2, e2, w1, w2 = (R[:, :, i] for i in range(8))
    oh = R[:, :, 8:12]
    ex = R[:, :, 12:16]
    sel = R[:, :, 16:24]
    m1 = R[:, :, 24:32]
    sel2 = R[:, :, 32:40]
    m2 = R[:, :, 40:48]
    wi = R[:, :, 48:56]
    wi2 = R[:, :, 56:64]

    def bc(ap, n):
        return ap.unsqueeze(2).broadcast_to([128, 16, n])

    def VR(fn):
        V(k, fn, r=[R, LG, T4], w=[R, T4])

    VR(lambda e: e.tensor_reduce(gmax, lg, AX.X, ALU.max))
    VR(lambda e: e.tensor_tensor(oh, lg, bc(gmax, 4), ALU.is_equal))
    VR(lambda e: e.tensor_tensor(ex, lg, bc(gmax, 4), ALU.subtract))
    A(k, lambda e: e.activation(ex, ex, AF.Exp), r=[R], w=[R])
    VR(lambda e: e.tensor_reduce(gs, ex, AX.X, ALU.add))
    VR(lambda e: e.reciprocal(gw, gs))
    VR(lambda e: e.tensor_tensor(T4[:], le, oh.unsqueeze(3).broadcast_to([128, 16, 4, 8]), ALU.mult))
    VR(lambda e: e.tensor_reduce(sel, T4[:].rearrange("p t g e -> p t e g"), AX.X, ALU.add))
    VR(lambda e: e.tensor_reduce(v1, sel, AX.X, ALU.max))
    VR(lambda e: e.tensor_tensor(m1, sel, bc(v1, 8), ALU.is_equal))
    VR(lambda e: e.scalar_tensor_tensor(sel2, m1, -1e30, sel, ALU.mult, ALU.add))
    VR(lambda e: e.tensor_reduce(v2, sel2, AX.X, ALU.max))
    VR(lambda e: e.tensor_tensor(m2, sel2, bc(v2, 8), ALU.is_equal))
    VR(lambda e: e.tensor_tensor(e2, v2, v1, ALU.subtract))
    A(k, lambda e: e.activation(e2, e2, AF.Exp), r=[R], w=[R])
    VR(lambda e: e.tensor_scalar(w2, e2, 1.0, None, ALU.add))
    VR(lambda e: e.reciprocal(w2, w2))
    VR(lambda e: e.tensor_tensor(w1, gw, w2, ALU.mult))
    VR(lambda e: e.tensor_tensor(w2, w1, e2, ALU.mult))
    VR(lambda e: e.tensor_tensor(wi, m1, bc(w1, 8), ALU.mult))
    VR(lambda e: e.tensor_tensor(wi2, m2, bc(w2, 8), ALU.mult))
    VR(lambda e: e.tensor_tensor(wi, wi, wi2, ALU.add))
    V(k, lambda e: e.tensor_tensor(comb[:], oh.unsqueeze(3).broadcast_to([128, 16, 4, 8]),
                                   wi.unsqueeze(2).broadcast_to([128, 16, 4, 8]), ALU.mult), r=[R], w=[comb])
    for i in range(16):
        bank = nbk()
        M(k, lambda e: e.transpose(bank[0:32, 0:128], comb[:, i, :, :].rearrange("p g e -> p (g e)"), identf[:]),
          r=[comb, identf], w=[bank])
        A(k, lambda e: e.activation(combT[0:32, i * 128:(i + 1) * 128], bank[0:32, 0:128], AF.Copy), r=[bank], w=[combT])
    if "comb" in dbg:
        o = dout("d_comb", [128, 16 * 32])
        k.dma('sp', o[:, :], comb[:].rearrange("p t g e -> p (t g e)"), reads=[comb])
    k.pop()
    if "h1" in dbg:
        o = dout("d_h1", [T_OWN, 1024])
        k.dma('sp', o.rearrange("(i p) n -> p i n", p=128), out_acc[:], reads=oacc)
    if upto in ("F", "G"):
        k.wait_all('sp', oacc)
        return finish()

    k.push()
    selall = k.sb("selall", [32, 32, 128], BF16)
    P(k, lambda e: e.memset(selall[:], 0.0), w=[selall])
    P(k, lambda e: e.affine_select(selall[:], selall[:], pattern=[[1, 32], [0, 128]], compare_op=ALU.not_equal,
                                   fill=1.0, base=0, channel_multiplier=-1), r=[selall], w=[selall])
    weg = [k.sb(f"weg{i}", [128, 8, 256], BF16) for i in range(3)]
    weu = [k.sb(f"weu{i}", [128, 8, 256], BF16) for i in range(3)]
    wed = [k.sb(f"wed{i}", [128, 2, 1024], BF16) for i in range(3)]

    def load_gu(ei):
        k.dma('pool', weg[ei % 3][:], w_eg[ei, :, :].rearrange("(c p) n -> p c n", p=128), writes=[weg[ei % 3]])
        k.dma('pool', weu[ei % 3][:], w_eu[ei, :, :].rearrange("(c p) n -> p c n", p=128), writes=[weu[ei % 3]])

    def load_d(ei):
        k.dma('pool', wed[ei % 3][:], w_ed[ei, :, :].rearrange("(c p) n -> p c n", p=128), writes=[wed[ei % 3]])

    load_gu(0)
    load_d(0)
    load_gu(1)
    load_d(1)
    sgs = [k.sb(f"sgs{i}", [128, 512], F32) for i in range(2)]
    cbs = [k.sb(f"cbs{i}", [128, 512], BF16) for i in range(2)]
    t1s = [k.sb(f"t1s{i}", [128, 512], BF16) for i in range(2)]
    hids = [k.sb(f"hid{i}", [128, 2, 512], BF16) for i in range(3)]
    evs = [k.sb(f"evs{i}", [128, 512], F32) for i in range(2)]
    pending = []
    nacc = [0]

    def emit_down(e_, tg, hid, w):
        for tt in range(4):
            ti = tg * 4 + tt
            for hf in range(2):
                bank = nbk()
                for f in range(2):
                    M(k, lambda e, f=f: e.matmul(bank[:, :], hid[:, f, tt * 128:(tt + 1) * 128], wed[w][:, f, hf * 512:(hf + 1) * 512],
                                                 start=(f == 0), stop=(f == 1)), r=[hid, wed[w]], w=[bank])
                dst = out_acc[:, ti, hf * 512:(hf + 1) * 512]
                if nacc[0] % 3 == 2:
                    ev = evs[(nacc[0] // 3) % 2]
                    A(k, lambda e: e.activation(ev[:], bank[:, :], AF.Copy), r=[bank], w=[ev])
                    P(k, lambda e: e.tensor_tensor(dst, dst, ev[:], ALU.add), r=[ev, oacc[ti]], w=[oacc[ti]])
                else:
                    V(k, lambda e: e.tensor_tensor(dst, bank[:, :], dst, ALU.add), r=[bank, oacc[ti]], w=[oacc[ti]])
                nacc[0] += 1
            if e_ == 31:
                k.dma('sp', out[ti * 128:(ti + 1) * 128, :], out_acc[:, ti, :], reads=[oacc[ti]])

    itn = 0
    for e_ in range(32):
        w = e_ % 3
        for tg in range(4):
            ts_ = slice(tg * 512, (tg + 1) * 512)
            hid = hids[itn % 3]
            cb = cbs[itn % 2]
            cbank = nbk()
            M(k, lambda e: e.matmul(cbank[:, :], selall[0:32, e_, :], combT[0:32, ts_], start=True, stop=True),
              r=[selall, combT], w=[cbank])
            A(k, lambda e: e.activation(cb[:], cbank[:, :], AF.Copy), r=[cbank], w=[cb])
            for f in range(2):
                gbank = nbk()
                for c in range(8):
                    M(k, lambda e, c=c: e.matmul(gbank[:, :], weg[w][:, c, f * 128:(f + 1) * 128], hnT[:, c, ts_],
                                                 start=(c == 0), stop=(c == 7)), r=[weg[w], hnT], w=[gbank])
                ubank = nbk()
                for c in range(8):
                    M(k, lambda e, c=c: e.matmul(ubank[:, :], weu[w][:, c, f * 128:(f + 1) * 128], hnT[:, c, ts_],
                                                 start=(c == 0), stop=(c == 7)), r=[weu[w], hnT], w=[ubank])
                sg = sgs[(itn * 2 + f) % 2]
                t1 = t1s[(itn * 2 + f) % 2]
                A(k, lambda e: e.activation(sg[:], gbank[:, :], AF.Silu), r=[gbank], w=[sg])
                V(k, lambda e: e.tensor_tensor(t1[:], ubank[:, :], sg[:], ALU.mult), r=[ubank, sg], w=[t1])
                P(k, lambda e: e.tensor_tensor(hid[:, f, :], t1[:], cb[:], ALU.mult), r=[t1, cb], w=[hid])
            if pending:
                emit_down(*pending.pop())
            pending.append((e_, tg, hid, w))
            if e_ + 2 < 32 and tg == 1:
                load_gu(e_ + 2)
            if e_ + 2 < 32 and tg == 2:
                load_d(e_ + 2)
            itn += 1
    emit_down(*pending.pop())
    k.wait_all('sp', oacc)
    k.pop()
    k.pop()
    return finish()


def make_in_maps(inputs):
    f = lambda a: np.ascontiguousarray(np.asarray(a, dtype=np.float32))
    x = f(inputs["x"])
    mem = f(inputs["mem"])
    shared = {
        "ln1_g": f(inputs["ln1_g"][0]), "w_in": f(inputs["w_in"][0]), "m_conv": f(inputs["m_conv"][0]),
        "m_i_b": f(inputs["m_i_b"][0]), "m_f_b": f(inputs["m_f_b"][0]), "m_norm_g": f(inputs["m_norm_g"][0]).reshape(512),
        "dil_q_g": f(inputs["dil_q_g"][0]), "dil_k_g": f(inputs["dil_k_g"][0]), "mem_norm_g": f(inputs["mem_norm_g"][0]),
        "w_mem_kv": f(inputs["w_mem_kv"][0]), "x_q_g": f(inputs["x_q_g"][0]), "x_k_g": f(inputs["x_k_g"][0]),
        "w_a": f(inputs["w_a"][0]), "w_b": f(inputs["w_b"][0]), "w_c": f(inputs["w_c"][0]),
        "w_gate": f(inputs["w_gate"][0]), "b_gate": f(inputs["b_gate"][0]), "w_out": f(inputs["w_out"][0]),
        "ln2_g": f(inputs["ln2_g"][0]), "w_grp": f(inputs["w_grp"][0]), "b_grp": f(inputs["b_grp"][0]),
        "w_er": f(inputs["w_er"][0]), "b_er": f(inputs["b_er"][0]).reshape(32),
        "w_eg": f(inputs["w_eg"][0]).reshape(32, 1024, 256), "w_eu": f(inputs["w_eu"][0]).reshape(32, 1024, 256),
        "w_ed": f(inputs["w_ed"][0]).reshape(32, 256, 1024),
    }
    s, t = np.meshgrid(np.arange(128), np.arange(128), indexing="ij")
    tri_prev = np.where(s >= t, 0.0, NEG).astype(np.float32)
    in_maps = []
    for core in range(8):
        b, half = core // 2, core % 2
        m = dict(shared)
        m["x_own"] = np.ascontiguousarray(x[b, half * T_OWN:(half + 1) * T_OWN])
        m["mem"] = np.ascontiguousarray(mem[b])
        pm = np.ones((4, T_ALL), np.float32)
        pa = np.zeros((4, T_ALL), np.float32)
        if half == 0:
            m["x_pre"] = np.zeros((T_OWN, 1024), np.float32)
            pm[:, :T_OWN] = 0.0
            pa[:, :T_OWN] = NEG
            m["mpfx"] = np.full((128, 128), NEG, np.float32)
        else:
            m["x_pre"] = np.ascontiguousarray(x[b, 0:T_OWN])
            m["mpfx"] = tri_prev
        m["pmul"] = pm
        m["padd"] = pa
        in_maps.append(m)
    return in_maps


_CACHE = {}


def kernel(**inputs):
    if "nc" not in _CACHE:
        _CACHE["nc"] = build_program()[0]
    nc = _CACHE["nc"]
    in_maps = make_in_maps(inputs)
    res = run_bass_kernel_spmd(nc, in_maps, core_ids=list(range(8)))
    outp = np.zeros((4, 4096, 1024), np.float32)
    for core in range(8):
        b, half = core // 2, core % 2
        outp[b, half * T_OWN:(half + 1) * T_OWN] = res.results[core]["out"]
    return outp
```
